# Optimizing a Trainium2 kernel written in Bass

```python
import math
import jax
import jax.numpy as jnp
from jax import lax
import numpy as np

D_MODEL = 1024
BATCH = 4
SEQ = 4096
DEPTH = 2

CTX_LEN = 256
GRID_W = 64
EPS = 1e-6
CHUNK = 16
F32 = jnp.float32

HG_HEADS = 4
HG_DK = 128
HG_DV = 128
HG_KW = HG_HEADS * HG_DK
HG_VW = HG_HEADS * HG_DV
GLA_HEADS = 4
GLA_DK = 64
GLA_DV = 128
GLA_KW = GLA_HEADS * GLA_DK
GLA_VW = GLA_HEADS * GLA_DV
GLA_RANK = 16
GLA_GATE_NORM = 16.0
S5_GROUP = 16
S5_GROUPS = 32
S5_WIDTH = S5_GROUP * S5_GROUPS
S5_STATE = 64
N_BRANCH = 3
BRANCH_W = 512
FFN_DIM = 3584
N_EXPERTS = 8
TOP_K = 2
N_DENSE = (DEPTH + 1) // 2
N_MOE = DEPTH // 2

IN_SPLITS = (HG_KW, HG_KW, HG_KW, HG_VW, HG_VW,
             GLA_KW, GLA_KW, GLA_VW, GLA_RANK, GLA_RANK, GLA_VW,
             S5_WIDTH, D_MODEL, D_MODEL, D_MODEL)
N_IN = 3 * HG_KW + 2 * HG_VW + 2 * GLA_KW + 2 * GLA_VW + 2 * GLA_RANK + S5_WIDTH + N_BRANCH * D_MODEL

kernel_name = 'hybrid_hgrn2_gla_s5_moe_dit'


def _rmsnorm(x, g):
    xf = x.astype(F32)
    y = xf * lax.rsqrt(jnp.mean(xf * xf, axis=-1, keepdims=True) + EPS)
    return (y * g.astype(F32)).astype(x.dtype)


def _modulate(xn, shift, scale):
    return xn * (1 + scale[:, None, :]) + shift[:, None, :]


def _grid_pos_embed(rows, dim, dtype):
    rr, cc = jnp.meshgrid(jnp.arange(rows, dtype=F32), jnp.arange(GRID_W, dtype=F32), indexing='ij')
    quarter = dim // 4
    omega = 1.0 / (10000.0 ** (jnp.arange(quarter, dtype=F32) / quarter))

    def emb(p):
        ang = p.reshape(-1)[:, None] * omega[None, :]
        return jnp.concatenate([jnp.sin(ang), jnp.cos(ang)], axis=-1)

    return jnp.concatenate([emb(rr), emb(cc)], axis=-1).astype(dtype)


def _split_cols(p):
    out = []
    start = 0
    for width in IN_SPLITS:
        out.append(p[..., start:start + width])
        start += width
    return out


def _heads(a, n_heads):
    bsz, t, w = a.shape
    return a.astype(F32).reshape(bsz, t, n_heads, w // n_heads).transpose(0, 2, 1, 3)


def _head_norm_gate(o, gain, gate):
    o = jnp.swapaxes(o, 1, 2)
    bsz, t, h, dv = o.shape
    o = o * lax.rsqrt(jnp.mean(o * o, axis=-1, keepdims=True) + EPS) * gain.astype(F32).reshape(h, dv)
    return o.reshape(bsz, t, h * dv) * jax.nn.silu(gate.astype(F32))


def _chunk_gated_linear(q, k, v, log_g, s0):
    bsz, h, l, dk = q.shape
    dv = v.shape[-1]
    n = l // CHUNK

    def chunks(a):
        return jnp.moveaxis(a.reshape(bsz, h, n, CHUNK, a.shape[-1]), 2, 0)

    lower = jnp.tril(jnp.ones((CHUNK, CHUNK), dtype=bool))[:, :, None]

    def step(s, inp):
        qc, kc, vc, gc = inp
        bc = jnp.cumsum(gc, axis=2)
        last = bc[:, :, -1:, :]
        rel = bc[:, :, :, None, :] - bc[:, :, None, :, :]
        decay = jnp.exp(jnp.where(lower, rel, -jnp.inf))
        scores = jnp.einsum('bhtd,bhsd,bhtsd->bhts', qc, kc, decay)
        o = (jnp.einsum('bhts,bhsv->bhtv', scores, vc)
             + jnp.einsum('bhtd,bhdv->bhtv', qc * jnp.exp(bc), s))
        s_new = (jnp.exp(last[:, :, 0, :])[..., None] * s
                 + jnp.einsum('bhsd,bhsv->bhdv', kc * jnp.exp(last - bc), vc))
        return s_new, o

    s_fin, o = lax.scan(step, s0, (chunks(q), chunks(k), chunks(v), chunks(log_g)))
    return jnp.moveaxis(o, 0, 2).reshape(bsz, h, l, dv), s_fin


def _bidir_prefix(q, k_f, k_b, v, g_f, g_b, n_ctx):
    bsz, h, _, dk = q.shape
    dv = v.shape[-1]
    zero = jnp.zeros((bsz, h, dk, dv), F32)

    def cx(a):
        return a[:, :, :n_ctx]

    def lt(a):
        return a[:, :, n_ctx:]

    def fl(a):
        return jnp.flip(a, axis=2)

    oc_f, sc_f = _chunk_gated_linear(cx(q), cx(k_f), cx(v), cx(g_f), zero)
    oc_b, sc_b = _chunk_gated_linear(fl(cx(q)), fl(cx(k_b)), fl(cx(v)), fl(cx(g_b)), zero)
    ol_f, _ = _chunk_gated_linear(lt(q), lt(k_f), lt(v), lt(g_f), sc_f)
    ol_b, _ = _chunk_gated_linear(fl(lt(q)), fl(lt(k_b)), fl(lt(v)), fl(lt(g_b)), sc_b)
    return jnp.concatenate([oc_f + fl(oc_b), ol_f + fl(ol_b)], axis=2)


def _hgrn2_branch(pq, pff, pfb, pi, pg, lb, norm_g, n_ctx):
    lb = lb.astype(F32)
    log_lb = jnp.log(lb)
    log_ub = jnp.log1p(-lb)

    def forget(z):
        z = z.astype(F32)
        log_f = jnp.logaddexp(log_lb, log_ub + jax.nn.log_sigmoid(z))
        one_minus_f = (1.0 - lb) * jax.nn.sigmoid(-z)
        return _heads(log_f, HG_HEADS), _heads(one_minus_f, HG_HEADS)

    g_f, k_f = forget(pff)
    g_b, k_b = forget(pfb)
    o = _bidir_prefix(_heads(pq, HG_HEADS), k_f, k_b, _heads(pi, HG_HEADS), g_f, g_b, n_ctx)
    return _head_norm_gate(o, norm_g, pg)


def _gla_branch(pq, pk, pv, pdf, pdb, pr, gate_up, gate_b, norm_g, n_ctx):
    up = gate_up.astype(F32)
    ub = gate_b.astype(F32)
    g_f = jax.nn.log_sigmoid(pdf.astype(F32) @ up[0] + ub[0]) / GLA_GATE_NORM
    g_b = jax.nn.log_sigmoid(pdb.astype(F32) @ up[1] + ub[1]) / GLA_GATE_NORM
    q = _heads(pq, GLA_HEADS) * (GLA_DK ** -0.5)
    k = _heads(pk, GLA_HEADS)
    o = _bidir_prefix(q, k, k, _heads(pv, GLA_HEADS), _heads(g_f, GLA_HEADS), _heads(g_b, GLA_HEADS), n_ctx)
    return _head_norm_gate(o, norm_g, pr)


def _complex_affine_combine(e1, e2):
    a1r, a1i, b1r, b1i = e1
    a2r, a2i, b2r, b2i = e2
    return (a2r * a1r - a2i * a1i,
            a2r * a1i + a2i * a1r,
            a2r * b1r - a2i * b1i + b2r,
            a2r * b1i + a2i * b1r + b2i)


def _s5_scan(abar_re, abar_im, bu_re, bu_im, s0_re, s0_im, reverse):
    if reverse:
        bu_re = jnp.flip(bu_re, axis=1)
        bu_im = jnp.flip(bu_im, axis=1)
    bu_re = bu_re.at[:, 0].add(abar_re * s0_re - abar_im * s0_im)
    bu_im = bu_im.at[:, 0].add(abar_re * s0_im + abar_im * s0_re)
    a_re = jnp.broadcast_to(abar_re, bu_re.shape)
    a_im = jnp.broadcast_to(abar_im, bu_im.shape)
    _, _, s_re, s_im = lax.associative_scan(_complex_affine_combine, (a_re, a_im, bu_re, bu_im), axis=1)
    fin_re, fin_im = s_re[:, -1], s_im[:, -1]
    if reverse:
        s_re = jnp.flip(s_re, axis=1)
        s_im = jnp.flip(s_im, axis=1)
    return s_re, s_im, fin_re, fin_im


def _s5_branch(u, n_ctx, a_re, a_im, log_step, b_re, b_im, c_re, c_im, d_skip, glu_w):
    uf = u.astype(F32)
    bsz, t, _ = uf.shape
    ug = uf.reshape(bsz, t, S5_GROUPS, S5_GROUP)
    y = uf * d_skip.astype(F32)
    zero = jnp.zeros((bsz, S5_GROUPS, S5_STATE), F32)
    for direction in range(2):
        lam_re = a_re[direction].astype(F32)
        lam_im = a_im[direction].astype(F32)
        step = jnp.exp(log_step[direction].astype(F32))[:, None]
        mag = jnp.exp(lam_re * step)
        abar_re = mag * jnp.cos(lam_im * step)
        abar_im = mag * jnp.sin(lam_im * step)
        den = lam_re * lam_re + lam_im * lam_im
        z_re = ((abar_re - 1.0) * lam_re + abar_im * lam_im) / den
        z_im = (abar_im * lam_re - (abar_re - 1.0) * lam_im) / den
        br = b_re[direction].astype(F32)
        bi = b_im[direction].astype(F32)
        bbar_re = z_re[..., None] * br - z_im[..., None] * bi
        bbar_im = z_re[..., None] * bi + z_im[..., None] * br
        bu_re = jnp.einsum('btgp,gnp->btgn', ug, bbar_re)
        bu_im = jnp.einsum('btgp,gnp->btgn', ug, bbar_im)
        rev = direction == 1
        sc_re, sc_im, f_re, f_im = _s5_scan(abar_re, abar_im, bu_re[:, :n_ctx], bu_im[:, :n_ctx], zero, zero, rev)
        sl_re, sl_im, _, _ = _s5_scan(abar_re, abar_im, bu_re[:, n_ctx:], bu_im[:, n_ctx:], f_re, f_im, rev)
        s_re = jnp.concatenate([sc_re, sl_re], axis=1)
        s_im = jnp.concatenate([sc_im, sl_im], axis=1)
        cr = c_re[direction].astype(F32)
        ci = c_im[direction].astype(F32)
        y = y + (jnp.einsum('btgn,gpn->btgp', s_re, cr)
                 - jnp.einsum('btgn,gpn->btgp', s_im, ci)).reshape(bsz, t, S5_WIDTH)
    y = jax.nn.gelu(y)
    return y * jax.nn.sigmoid(y @ glu_w.astype(F32))


def _mixer(h, n_ctx, w_in, lb, hg_norm_g, gla_gate_up, gla_gate_b, gla_norm_g, s5_a_re, s5_a_im,
           s5_log_step, s5_b_re, s5_b_im, s5_c_re, s5_c_im, s5_d, s5_glu_w, branch_proj, w_out):
    (hg_q, hg_ff, hg_fb, hg_i, hg_g, gl_q, gl_k, gl_v, gl_df, gl_db, gl_r, s5_u,
     gate_a, gate_b, gate_c) = _split_cols(h @ w_in)
    y_a = _hgrn2_branch(hg_q, hg_ff, hg_fb, hg_i, hg_g, lb, hg_norm_g, n_ctx)
    y_b = _gla_branch(gl_q, gl_k, gl_v, gl_df, gl_db, gl_r, gla_gate_up, gla_gate_b, gla_norm_g, n_ctx)
    y_c = _s5_branch(s5_u, n_ctx, s5_a_re, s5_a_im, s5_log_step, s5_b_re, s5_b_im, s5_c_re, s5_c_im,
                     s5_d, s5_glu_w)
    merged = jnp.zeros(h.shape, F32)
    for idx, (y_k, gate_k) in enumerate(((y_a, gate_a), (y_b, gate_b), (y_c, gate_c))):
        merged = merged + jax.nn.sigmoid(gate_k.astype(F32)) * (y_k.astype(h.dtype) @ branch_proj[idx]).astype(F32)
    return merged.astype(h.dtype) @ w_out


def _swiglu(h, w1, w3, w2):
    return (jax.nn.silu(h @ w1) * (h @ w3)) @ w2


def _moe(h, router_w, w1, w3, w2):
    probs = jax.nn.softmax((h @ router_w).astype(F32), axis=-1)
    top_p, top_i = lax.top_k(probs, TOP_K)
    top_p = top_p / jnp.sum(top_p, axis=-1, keepdims=True)
    combine = jnp.sum(jax.nn.one_hot(top_i, N_EXPERTS, dtype=F32) * top_p[..., None], axis=-2)
    out = jnp.zeros(h.shape, F32)
    for e in range(N_EXPERTS):
        out = out + combine[..., e:e + 1] * _swiglu(h, w1[e], w3[e], w2[e]).astype(F32)
    return out.astype(h.dtype)


def setup_inputs(seed: int = 0) -> dict:
    key = jax.random.key(seed)
    keys = iter(jax.random.split(key, 48))

    def nrm(shape, scale):
        return scale * jax.random.normal(next(keys), shape, F32)

    def gain(shape):
        return 1.0 + nrm(shape, 0.01)

    d = D_MODEL
    state_idx = jnp.arange(S5_STATE, dtype=F32)
    s5_shape = (DEPTH, 2, S5_GROUPS, S5_STATE)
    return {
        'x': nrm((BATCH, SEQ, d), 1.0),
        'c': nrm((BATCH, d), 1.0),
        'ctx': nrm((BATCH, CTX_LEN, d), 1.0),
        'c_ctx': nrm((d,), 1.0),
        'norm1_g': gain((DEPTH, d)),
        'norm2_g': gain((DEPTH, d)),
        'ada_w': nrm((DEPTH, d, 6 * d), 0.5 * d ** -0.5),
        'ada_b': nrm((DEPTH, 6 * d), 0.01),
        'w_in': nrm((DEPTH, d, N_IN), d ** -0.5),
        'hg_lb_logits': nrm((DEPTH, HG_KW), 0.1),
        'hg_norm_g': gain((DEPTH, HG_VW)),
        'gla_gate_up': nrm((DEPTH, 2, GLA_RANK, GLA_KW), GLA_RANK ** -0.5),
        'gla_gate_b': nrm((DEPTH, 2, GLA_KW), 0.01),
        'gla_norm_g': gain((DEPTH, GLA_VW)),
        's5_a_re': -0.5 + nrm(s5_shape, 0.01),
        's5_a_im': math.pi * state_idx + nrm(s5_shape, 0.01),
        's5_log_step': jax.random.uniform(next(keys), (DEPTH, 2, S5_GROUPS), F32, math.log(1e-3), math.log(1e-1)),
        's5_b_re': nrm((DEPTH, 2, S5_GROUPS, S5_STATE, S5_GROUP), (2 * S5_GROUP) ** -0.5),
        's5_b_im': nrm((DEPTH, 2, S5_GROUPS, S5_STATE, S5_GROUP), (2 * S5_GROUP) ** -0.5),
        's5_c_re': nrm((DEPTH, 2, S5_GROUPS, S5_GROUP, S5_STATE), S5_STATE ** -0.5),
        's5_c_im': nrm((DEPTH, 2, S5_GROUPS, S5_GROUP, S5_STATE), S5_STATE ** -0.5),
        's5_d': nrm((DEPTH, S5_WIDTH), 1.0),
        's5_glu_w': nrm((DEPTH, S5_WIDTH, S5_WIDTH), S5_WIDTH ** -0.5),
        'branch_proj': nrm((DEPTH, N_BRANCH, BRANCH_W, d), BRANCH_W ** -0.5),
        'w_out': nrm((DEPTH, d, d), d ** -0.5),
        'ffn_w1': nrm((N_DENSE, d, FFN_DIM), d ** -0.5),
        'ffn_w3': nrm((N_DENSE, d, FFN_DIM), d ** -0.5),
        'ffn_w2': nrm((N_DENSE, FFN_DIM, d), FFN_DIM ** -0.5),
        'router_w': nrm((N_MOE, d, N_EXPERTS), d ** -0.5),
        'moe_w1': nrm((N_MOE, N_EXPERTS, d, FFN_DIM), d ** -0.5),
        'moe_w3': nrm((N_MOE, N_EXPERTS, d, FFN_DIM), d ** -0.5),
        'moe_w2': nrm((N_MOE, N_EXPERTS, FFN_DIM, d), FFN_DIM ** -0.5),
        'final_norm_g': gain((d,)),
    }


def reference(x, c, ctx, c_ctx, norm1_g, norm2_g, ada_w, ada_b, w_in, hg_lb_logits, hg_norm_g,
              gla_gate_up, gla_gate_b, gla_norm_g, s5_a_re, s5_a_im, s5_log_step, s5_b_re, s5_b_im,
              s5_c_re, s5_c_im, s5_d, s5_glu_w, branch_proj, w_out, ffn_w1, ffn_w3, ffn_w2,
              router_w, moe_w1, moe_w3, moe_w2, final_norm_g):
    n_tok = x.shape[1]
    n_ctx = ctx.shape[1]
    rows = n_tok // GRID_W
    x = x + _grid_pos_embed(rows, D_MODEL, x.dtype)[None]
    lb_cum = jnp.cumsum(jax.nn.softmax(hg_lb_logits.astype(F32), axis=0), axis=0)
    lower_bounds = lb_cum - lb_cum[0:1]
    cond = jax.nn.silu(c)
    cond_ctx = jax.nn.silu(c_ctx)[None]
    for layer in range(DEPTH):
        last = layer == DEPTH - 1
        mod = cond @ ada_w[layer] + ada_b[layer]
        mod_c = cond_ctx @ ada_w[layer] + ada_b[layer]
        sh1, sc1, g1, sh2, sc2, g2 = jnp.split(mod, 6, axis=-1)
        csh1, csc1, cg1, csh2, csc2, cg2 = jnp.split(mod_c, 6, axis=-1)
        h = jnp.concatenate([_modulate(_rmsnorm(ctx, norm1_g[layer]), csh1, csc1),
                             _modulate(_rmsnorm(x, norm1_g[layer]), sh1, sc1)], axis=1)
        y = _mixer(h, n_ctx, w_in[layer], lower_bounds[layer], hg_norm_g[layer], gla_gate_up[layer],
                   gla_gate_b[layer], gla_norm_g[layer], s5_a_re[layer], s5_a_im[layer], s5_log_step[layer],
                   s5_b_re[layer], s5_b_im[layer], s5_c_re[layer], s5_c_im[layer], s5_d[layer],
                   s5_glu_w[layer], branch_proj[layer], w_out[layer])
        x = x + g1[:, None, :] * y[:, n_ctx:]
        if not last:
            ctx = ctx + cg1[:, None, :] * y[:, n_ctx * 0:n_ctx]
        h_lat = _modulate(_rmsnorm(x, norm2_g[layer]), sh2, sc2)
        if last:
            h = h_lat
        else:
            h = jnp.concatenate([_modulate(_rmsnorm(ctx, norm2_g[layer]), csh2, csc2), h_lat], axis=1)
        if layer % 2 == 0:
            f = _swiglu(h, ffn_w1[layer // 2], ffn_w3[layer // 2], ffn_w2[layer // 2])
        else:
            f = _moe(h, router_w[layer // 2], moe_w1[layer // 2], moe_w3[layer // 2], moe_w2[layer // 2])
        if last:
            x = x + g2[:, None, :] * f
        else:
            x = x + g2[:, None, :] * f[:, n_ctx:]
            ctx = ctx + cg2[:, None, :] * f[:, :n_ctx]
    return _rmsnorm(x, final_norm_g)
```

```python
import math
import numpy as np
from contextlib import ExitStack, contextmanager
import concourse.bass as bass
import concourse.mybir as mybir
from concourse.bass_utils import run_bass_kernel_spmd


def pbc(ap):
    b = ap.partition_broadcast(128)
    if len(b.shape) == 3 and b.shape[1] == 1:
        return b[:, 0, :]
    return b


F32 = mybir.dt.float32
BF16 = mybir.dt.bfloat16
I32 = mybir.dt.int32
AF = mybir.ActivationFunctionType
ALU = mybir.AluOpType
AX = mybir.AxisListType

ENGS = ("pe", "act", "dve", "pool", "sp")
EPOCH = 50000
NDMASEM = 4
DEPOCH = 2000


class Buf:
    __slots__ = ("name", "w", "r")

    def __init__(self, name):
        self.name = name
        self.w = None
        self.r = []


class Sched:
    def __init__(self, nc, stack):
        self.nc = nc
        self.stack = stack
        self.eng = {"pe": nc.tensor, "act": nc.scalar, "dve": nc.vector, "pool": nc.gpsimd, "sp": nc.sync}
        self.q = {e: [] for e in ENGS}
        self.cnt = {e: 0 for e in ENGS}
        self.sems = {e: [] for e in ENGS}
        self.dcnt = {e: 0 for e in ENGS}
        self.dsems = {e: None for e in ENGS}
        self.seen = {e: {} for e in ENGS}
        self.bufs = {}

    def buf(self, name):
        b = self.bufs.get(name)
        if b is None:
            b = self.bufs[name] = Buf(name)
        return b

    def _sem(self, name):
        return self.stack.enter_context(self.nc.semaphore(name))

    def _esem(self, e, epoch):
        while len(self.sems[e]) <= epoch:
            self.sems[e].append(self._sem(f"s_{e}_{len(self.sems[e])}"))
        return self.sems[e][epoch]

    def _dsem(self, e, n):
        if self.dsems[e] is None:
            self.dsems[e] = []
        setsz = NDMASEM * DEPOCH
        si, r = divmod(n, setsz)
        while len(self.dsems[e]) <= si:
            k = len(self.dsems[e])
            self.dsems[e].append([self._sem(f"d_{e}_{k}_{j}") for j in range(NDMASEM)])
        return self.dsems[e][si][r % NDMASEM], 16 * (r // NDMASEM + 1)

    def _wait(self, e, ev):
        if ev is None:
            return
        kind, src, n = ev
        if kind == "c":
            if src == e and e == "pe":
                return
            key = ("c", src)
            if self.seen[e].get(key, 0) >= n:
                return
            self.seen[e][key] = n
            ep, loc = divmod(n - 1, EPOCH)
            sem = self._esem(src, ep)
            val = loc + 1
        else:
            key = ("d", src, n % NDMASEM)
            if self.seen[e].get(key, -1) >= n:
                return
            self.seen[e][key] = n
            sem, val = self._dsem(src, n)
        self.q[e].append(lambda eng, sem=sem, val=val: eng.wait_ge(sem, val))

    def _deps(self, e, reads, writes):
        for b in reads:
            if b.w is not None:
                self._wait(e, b.w)
        for b in writes:
            if b.w is not None:
                self._wait(e, b.w)
            for ev in b.r:
                self._wait(e, ev)

    def _commit(self, ev, reads, writes):
        for b in reads:
            b.r.append(ev)
            if len(b.r) > 64:
                b.r = b.r[-64:] if False else b.r
        for b in writes:
            b.w = ev
            b.r = []

    def op(self, e, fn, reads=(), writes=()):
        reads = [self.buf(b) if isinstance(b, str) else b for b in reads]
        writes = [self.buf(b) if isinstance(b, str) else b for b in writes]
        self._deps(e, reads, writes)
        self.cnt[e] += 1
        n = self.cnt[e]
        ep, loc = divmod(n - 1, EPOCH)
        sem = self._esem(e, ep)
        import sys as _sys
        fr = _sys._getframe(1)
        tag = []
        while fr is not None and len(tag) < 4:
            tag.append(f"{fr.f_code.co_name}:{fr.f_lineno}")
            fr = fr.f_back

        def _emit(eng, fn=fn, sem=sem, tag=tag):
            try:
                fn(eng).then_inc(sem, 1)
            except Exception:
                print("EMIT FAILED at", tag, flush=True)
                raise
        self.q[e].append(_emit)
        self._commit(("c", e, n), reads, writes)

    def dma(self, e, out, in_, reads=(), writes=(), **kw):
        reads = [self.buf(b) if isinstance(b, str) else b for b in reads]
        writes = [self.buf(b) if isinstance(b, str) else b for b in writes]
        self._deps(e, reads, writes)
        n = self.dcnt[e]
        self.dcnt[e] += 1
        if n >= NDMASEM:
            self._wait(e, ("d", e, n - NDMASEM))
        sem, _v = self._dsem(e, n)
        self.q[e].append(lambda eng, out=out, in_=in_, sem=sem, kw=kw: eng.dma_start(out=out, in_=in_, **kw).then_inc(sem, 16))
        self._commit(("d", e, n), reads, writes)

    def dma_gather(self, out, in_, idx_ap, reads=(), writes=()):
        e = "pool"
        reads = [self.buf(b) if isinstance(b, str) else b for b in reads]
        writes = [self.buf(b) if isinstance(b, str) else b for b in writes]
        self._deps(e, reads, writes)
        n = self.dcnt[e]
        self.dcnt[e] += 1
        if n >= NDMASEM:
            self._wait(e, ("d", e, n - NDMASEM))
        sem, _v = self._dsem(e, n)
        self.q[e].append(lambda eng, out=out, in_=in_, idx_ap=idx_ap, sem=sem: eng.indirect_dma_start(
            out=out, out_offset=None, in_=in_, in_offset=bass.IndirectOffsetOnAxis(ap=idx_ap, axis=0)).then_inc(sem, 16))
        self._commit(("d", e, n), reads, writes)

    def finish(self, final_bufs):
        for b in final_bufs:
            b = self.buf(b) if isinstance(b, str) else b
            self._wait("sp", b.w)
        for e in ENGS:
            for n in range(max(0, self.dcnt[e] - NDMASEM), self.dcnt[e]):
                self._wait("sp", ("d", e, n))
        for e in ENGS:
            if e != "sp" and self.cnt[e] > 0:
                self._wait("sp", ("c", e, self.cnt[e]))

    def barrier(self):
        for e in ENGS:
            for f in ENGS:
                if f != e and self.cnt[f] > 0:
                    self._wait(e, ("c", f, self.cnt[f]))
            for f in ENGS:
                for n in range(max(0, self.dcnt[f] - NDMASEM), self.dcnt[f]):
                    self._wait(e, ("d", f, n))

    def emit(self):
        nc = self.nc
        if not any(self.q[e] for e in ENGS):
            return
        with nc.Block() as block:
            @block.tensor
            def _(eng):
                for f in self.q["pe"]:
                    f(eng)

            @block.scalar
            def _(eng):
                for f in self.q["act"]:
                    f(eng)

            @block.vector
            def _(eng):
                for f in self.q["dve"]:
                    f(eng)

            @block.gpsimd
            def _(eng):
                for f in self.q["pool"]:
                    f(eng)

            @block.sync
            def _(eng):
                for f in self.q["sp"]:
                    f(eng)
        self.q = {e: [] for e in ENGS}


D = 1024
FF = 3584
NE = 8
NIN = 7712
EPS = 1e-6
OFF = dict(hg_q=0, hg_ff=512, hg_fb=1024, hg_i=1536, hg_g=2048, gl_q=2560, gl_k=2816, gl_v=3072,
           gl_df=3584, gl_db=3600, gl_r=3616, s5_u=4128, ga=4640, gb=5664, gc=6688)
TWO_PI = 6.283185307179586
PI_SAFE = 3.1415925

WSPECS = [("norm1_g", [2, D]), ("norm2_g", [2, D]), ("ada_w", [2, D, 6 * D]), ("ada_b", [2, 6 * D]),
          ("w_in", [2, D, NIN]), ("hg_lb_logits", [2, 512]), ("hg_norm_g", [2, 512]),
          ("gla_gate_up", [2, 2, 16, 256]), ("gla_gate_b", [2, 2, 256]), ("gla_norm_g", [2, 512]),
          ("s5_a_re", [2, 2, 32, 64]), ("s5_a_im", [2, 2, 32, 64]), ("s5_log_step", [2, 2, 32]),
          ("s5_b_re", [2, 2, 32, 64, 16]), ("s5_b_im", [2, 2, 32, 64, 16]),
          ("s5_c_re", [2, 2, 32, 16, 64]), ("s5_c_im", [2, 2, 32, 16, 64]),
          ("s5_d", [2, 512]), ("s5_glu_w", [2, 512, 512]), ("branch_proj", [2, 3, 512, D]),
          ("w_out", [2, D, D]), ("ffn_w1", [1, D, FF]), ("ffn_w3", [1, D, FF]), ("ffn_w2", [1, FF, D]),
          ("router_w", [1, D, NE]), ("moe_w1", [1, NE, D, FF]), ("moe_w3", [1, NE, D, FF]),
          ("moe_w2", [1, NE, FF, D]), ("final_norm_g", [D])]


class KB:
    def __init__(self, nct=2, nlt=32, dumps=(), stop=None):
        self.nct, self.nlt = nct, nlt
        self.NT = nct + nlt
        self.T = self.NT * 128
        self.dumps = set(dumps)
        self.stop = stop
        self.nc = nc = bass.Bass("TRN2", target_bir_lowering=False)
        self.st = ExitStack()
        self.S = Sched(nc, self.st)
        self.t = {}
        din = lambda name, shape: nc.dram_tensor(name, list(shape), F32, kind="ExternalInput").ap()
        self.xin = din("xin", [self.T, D])
        self.ccol = din("ccol", [128, 8])
        self.cctxcol = din("cctxcol", [128, 8])
        self.w = {name: din(name, shape) for name, shape in WSPECS}
        self.NLOC = nlt // 2
        self.idx_in = nc.dram_tensor("idx", [128, self.NLOC], I32, kind="ExternalInput").ap()
        self.out = nc.dram_tensor("out", [self.NLOC * 128, D], F32, kind="ExternalOutput").ap()
        self.scr = {}

    def dscr(self, name, shape, dt=F32):
        kind = "ExternalOutput" if name in self.dumps else "Internal"
        ap = self.nc.dram_tensor(name, list(shape), dt, kind=kind).ap()
        self.scr[name] = ap
        return ap

    def sb(self, stack, name, shape, dt=F32):
        self._uid = getattr(self, "_uid", 0) + 1
        h = stack.enter_context(self.nc.sbuf_tensor(f"{name}_u{self._uid}", list(shape), dt))
        self.t[name] = h
        if hasattr(stack, "_names"):
            stack._names.append(name)
        return h

    @contextmanager
    def phase(self):
        with ExitStack() as ph:
            ph._names = []
            yield ph
            self.phase_end()
            for n in ph._names:
                self.t.pop(n, None)

    def mm(self, out, lhsT, rhs, r, w, start=True, stop=True):
        self.S.op("pe", lambda e: e.matmul(out, lhsT=lhsT, rhs=rhs, start=start, stop=stop), r, w)

    def tr(self, out, in_, r, w):
        ident = self.t["ident"]
        self.S.op("pe", lambda e: e.transpose(out, in_, ident[:]), list(r) + ["ident"], w)

    def act(self, out, in_, func, r, w, bias=None, scale=None, accum=None):
        kw = {}
        if bias is not None:
            kw["bias"] = bias
        if scale is not None:
            kw["scale"] = scale
        if accum is not None:
            kw["accum_out"] = accum
        self.S.op("act", lambda e: e.activation(out=out, in_=in_, func=func, **kw), r, w)

    def tt(self, eng, out, in0, in1, op, r, w):
        self.S.op(eng, lambda e: e.tensor_tensor(out=out, in0=in0, in1=in1, op=op), r, w)

    def ts(self, eng, out, in0, s1, op0, r, w, s2=None, op1=None):
        if op1 is None:
            self.S.op(eng, lambda e: e.tensor_scalar(out=out, in0=in0, scalar1=s1, scalar2=None, op0=op0), r, w)
        else:
            self.S.op(eng, lambda e: e.tensor_scalar(out=out, in0=in0, scalar1=s1, scalar2=s2, op0=op0, op1=op1), r, w)

    def stt(self, out, in0, scalar, in1, op0, op1, r, w, accum=None):
        if accum is None:
            self.S.op("dve", lambda e: e.scalar_tensor_tensor(out=out, in0=in0, scalar=scalar, in1=in1, op0=op0, op1=op1), r, w)
        else:
            self.S.op("dve", lambda e: e.scalar_tensor_tensor(out=out, in0=in0, scalar=scalar, in1=in1, op0=op0, op1=op1, accum_out=accum), r, w)

    def cp(self, eng, out, in_, r, w):
        if eng == "act":
            self.S.op("act", lambda e: e.activation(out=out, in_=in_, func=AF.Identity), r, w)
        else:
            self.S.op(eng, lambda e: e.tensor_copy(out=out, in_=in_), r, w)

    def memset(self, eng, ap, val, w):
        self.S.op(eng, lambda e: e.memset(ap, val), (), w)

    def dma(self, q, out, in_, r, w, **kw):
        self.S.dma(q, out, in_, r, w, **kw)

    def phase_end(self):
        self.S.barrier()
        self.S.emit()

    def is_ctx(self, ti):
        return ti < self.nct

    def setup(self):
        S, t, st = self.S, self.t, self.st
        sb = lambda name, shape, dt=F32: self.sb(st, name, shape, dt)
        for i in range(8):
            h = st.enter_context(self.nc.psum_tensor(f"P{i}", [128, 512], F32))
            t[f"P{i}"] = h
        self.dscr("X", [self.T, D])
        di = sb("c_di", [128, 128], I32)
        df = sb("c_df", [128, 128])
        S.op("pool", lambda e: e.iota(di[:], pattern=[[1, 128]], base=0, channel_multiplier=-1), (), ["c_di"])
        self.cp("dve", df[:], di[:], ["c_di"], ["c_df"])
        for name, op in (("ident", ALU.is_equal), ("minc0", ALU.is_ge), ("minc1", ALU.is_le),
                         ("mstr0", ALU.is_lt), ("mstr1", ALU.is_gt)):
            h = sb(name, [128, 128])
            S.op("dve", lambda e, h=h, op=op: e.tensor_single_scalar(out=h[:], in_=df[:], scalar=0.0, op=op), ["c_df"], [name])
        for d in (0, 1):
            h = sb(f"mincb{d}", [128, 128], BF16)
            self.cp("dve", h[:], t[f"minc{d}"][:], [f"minc{d}"], [f"mincb{d}"])
        for hh in (0, 1):
            hm = sb(f"hm{hh}", [128, 1])
            self.memset("pool", hm[:], 0.0, [f"hm{hh}"])
            self.memset("pool", hm[hh * 64:(hh + 1) * 64, :], 1.0, [f"hm{hh}"])
        ones = sb("ones", [128, 128])
        self.memset("pool", ones[:], 1.0, ["ones"])
        for k in range(4):
            m = sb(f"mask{k}", [128, 128])
            self.memset("pool", m[:], 0.0, [f"mask{k}"])
            self.memset("pool", m[0:64, (2 * k) * 16:(2 * k + 1) * 16], 1.0, [f"mask{k}"])
            self.memset("pool", m[64:128, (2 * k + 1) * 16:(2 * k + 2) * 16], 1.0, [f"mask{k}"])
        lg = self.w["hg_lb_logits"]
        for nm, shape in (("fm", [128, 4]), ("tm", [128, 512])):
            for l in (0, 1):
                sb(f"lb_{nm}{l}", shape)
                sb(f"oml_{nm}{l}", shape)
        with self.phase() as ph0:
            lfm = self.sb(ph0, "lb_lfm", [128, 2, 4])
            self.dma("sp", lfm[:], lg.rearrange("l (h p) -> p l h", p=128), (), ["lb_lfm"], allow_slow_non_contiguous=True)
            ltm = self.sb(ph0, "lb_ltm", [128, 2, 512])
            self.dma("sp", ltm[:], lg.partition_broadcast(128), (), ["lb_ltm"])
            for nm, shape, src in (("fm", [128, 4], lfm), ("tm", [128, 512], ltm)):
                self.memset("pool", t[f"lb_{nm}0"][:], 0.0, [f"lb_{nm}0"])
                self.memset("pool", t[f"oml_{nm}0"][:], 1.0, [f"oml_{nm}0"])
                dtile = self.sb(ph0, f"lb_d{nm}", shape)
                self.tt("dve", dtile[:], src[:, 1, :], src[:, 0, :], ALU.subtract, [f"lb_l{nm}"], [f"lb_d{nm}"])
                self.act(t[f"lb_{nm}1"][:], dtile[:], AF.Sigmoid, [f"lb_d{nm}"], [f"lb_{nm}1"])
                self.act(t[f"oml_{nm}1"][:], dtile[:], AF.Sigmoid, [f"lb_d{nm}"], [f"oml_{nm}1"], scale=-1.0)
        with self.phase() as ph:
            psb = lambda name, shape, dt=F32: self.sb(ph, name, shape, dt)
            ji = psb("pe_ji", [128, 256], I32)
            om2 = psb("pe_om2", [128, 512])
            ph2 = psb("pe_ph", [128, 512])
            S.op("pool", lambda e: e.iota(ji[:], pattern=[[1, 256]], base=0, channel_multiplier=0), (), ["pe_ji"])
            self.cp("dve", om2[:, 0:256], ji[:], ["pe_ji"], ["pe_om2"])
            self.act(om2[:, 0:256], om2[:, 0:256], AF.Exp, ["pe_om2"], ["pe_om2"], scale=-math.log(10000.0) / 256.0)
            self.cp("dve", om2[:, 256:512], om2[:, 0:256], ["pe_om2"], ["pe_om2"])
            self.memset("pool", ph2[:, 0:256], 0.0, ["pe_ph"])
            self.memset("pool", ph2[:, 256:512], math.pi / 2, ["pe_ph"])
            pi_ = psb("pe_pi", [128, 1], I32)
            pf = psb("pe_pf", [128, 1])
            hi = psb("pe_hi", [128, 1])
            cpos = psb("pe_c", [128, 1])
            S.op("pool", lambda e: e.iota(pi_[:], pattern=[[0, 1]], base=0, channel_multiplier=1), (), ["pe_pi"])
            self.cp("dve", pf[:], pi_[:], ["pe_pi"], ["pe_pf"])
            self.ts("dve", hi[:], pf[:], 64.0, ALU.is_ge, ["pe_pf"], ["pe_hi"])
            self.stt(cpos[:], hi[:], -64.0, pf[:], ALU.mult, ALU.add, ["pe_hi", "pe_pf"], ["pe_c"])

            def sincos(dst, scal, tag):
                a = t["pe_a"]
                b = t["pe_b"]
                bi = t["pe_bi"]
                self.stt(a[:], om2[:], scal, ph2[:], ALU.mult, ALU.add, ["pe_om2", "pe_ph", tag], ["pe_a"])
                self.ts("dve", b[:], a[:], 1.0 / TWO_PI, ALU.mult, ["pe_a"], ["pe_b"])
                self.cp("dve", bi[:], b[:], ["pe_b"], ["pe_bi"])
                self.cp("dve", b[:], bi[:], ["pe_bi"], ["pe_b"])
                self.stt(a[:], b[:], -TWO_PI, a[:], ALU.mult, ALU.add, ["pe_b", "pe_a"], ["pe_a"])
                self.ts("dve", a[:], a[:], PI_SAFE, ALU.min, ["pe_a"], ["pe_a"], s2=-PI_SAFE, op1=ALU.max)
                self.act(dst, a[:], AF.Sin, ["pe_a"], [tag + "_o"])

            psb("pe_a", [128, 512])
            psb("pe_b", [128, 512])
            psb("pe_bi", [128, 512], I32)
            colp = psb("pe_col", [128, 512])
            S.buf("pe_c_o")
            sincos(colp[:], cpos[:], "pe_c")
            rsc = psb("pe_r", [128, 1])
            for ti in range(self.NT):
                xt = psb(f"pe_x{ti % 2}", [128, D]) if ti < 2 else t[f"pe_x{ti % 2}"]
                nm = f"pe_x{ti % 2}"
                self.dma("sp", xt[:], self.xin[ti * 128:(ti + 1) * 128, :], (), [nm])
                if not self.is_ctx(ti):
                    li = ti - self.nct
                    rowp = psb(f"pe_row{ti % 2}", [128, 512]) if f"pe_row{ti % 2}" not in t else t[f"pe_row{ti % 2}"]
                    self.ts("dve", rsc[:], hi[:], float(2 * li), ALU.add, ["pe_hi"], ["pe_r"])
                    S.buf("pe_r_o")
                    sincos(rowp[:], rsc[:], "pe_r")
                    self.tt("dve", xt[:, 0:512], xt[:, 0:512], rowp[:], ALU.add, [nm, "pe_r_o"], [nm])
                    self.tt("pool", xt[:, 512:1024], xt[:, 512:1024], colp[:], ALU.add, [nm, "pe_c_o"], [nm])
                self.dma("pool", self.scr["X"][ti * 128:(ti + 1) * 128, :], xt[:], [nm], [f"X@{ti}"])

    def mod_phase(self, l):
        S, t = self.S, self.t
        if "MODS" not in self.scr:
            self.dscr("MODS", [2, 2, 6 * D])
        with self.phase() as ph:
            psb = lambda name, shape, dt=F32: self.sb(ph, name, shape, dt)
            cb = {}
            for v, src in ((0, self.ccol), (1, self.cctxcol)):
                c = psb(f"md_c{v}", [128, 8])
                self.dma("sp", c[:], src, (), [f"md_c{v}"])
                self.act(c[:], c[:], AF.Silu, [f"md_c{v}"], [f"md_c{v}"])
                cbt = psb(f"md_cb{v}", [128, 8, 128])
                self.cp("dve", cbt[:], c[:].unsqueeze(2).to_broadcast([128, 8, 128]), [f"md_c{v}"], [f"md_cb{v}"])
                cb[v] = cbt
            bb = psb("md_b", [128, 6 * D])
            self.dma("sp", bb[:], pbc(self.w["ada_b"][l:l + 1, :]), (), ["md_b"])
            wsrc = self.w["ada_w"][l].rearrange("(kc p) n -> p kc n", p=128)
            for cbk in range(12):
                wn = f"md_w{cbk % 2}"
                wt = psb(wn, [128, 8, 512]) if wn not in t else t[wn]
                self.dma("sp", wt[:], wsrc[:, :, cbk * 512:(cbk + 1) * 512], (), [wn])
                for v in (0, 1):
                    pn = f"P{v}"
                    for kc in range(8):
                        self.mm(t[pn][:], cb[v][:, kc, :], wt[:, kc, :], [f"md_cb{v}", wn], [pn], start=(kc == 0), stop=(kc == 7))
                    on = f"md_o{v}"
                    ot = psb(on, [128, 512]) if on not in t else t[on]
                    self.tt("dve", ot[:], t[pn][:], bb[:, cbk * 512:(cbk + 1) * 512], ALU.add, [pn, "md_b"], [on])
                    self.dma("pool", self.scr["MODS"][l, v:v + 1, cbk * 512:(cbk + 1) * 512], ot[0:1, :], [on], [f"MODS@{l}"])

    def load_mod(self, stack, l, which, tag):
        for v in (0, 1):
            h = self.sb(stack, f"{tag}{v}", [128, D])
            self.dma("sp", h[:], pbc(self.scr["MODS"][l, v:v + 1, which * D:(which + 1) * D]),
                     [f"MODS@{l}"], [f"{tag}{v}"])

    def norm_phase(self, stack, l, second, tiles, router=False, local=None):
        hT = self.sb(stack, "hT", [128, 8, self.T if local is None else self.NLOC * 128], BF16)
        with self.phase() as inner:
            self._norm_phase(inner, l, second, tiles, router, hT, local)
        return hT

    def _norm_phase(self, stack, l, second, tiles, router, hT, local=None):
        S, t = self.S, self.t
        psb = lambda name, shape, dt=F32: self.sb(stack, name, shape, dt)
        pre = "n2" if second else "n1"
        self.load_mod(stack, l, 4 if second else 1, pre + "sc")
        self.load_mod(stack, l, 3 if second else 0, pre + "sh")
        gn = psb(pre + "g", [128, D])
        gsrc = self.w["norm2_g" if second else "norm1_g"]
        self.dma("sp", gn[:], pbc(gsrc[l:l + 1, :]), (), [pre + "g"])
        for v in (0, 1):
            self.stt(t[f"{pre}sc{v}"][:], t[f"{pre}sc{v}"][:], 1.0, gn[:], ALU.add, ALU.mult, [f"{pre}sc{v}", pre + "g"], [f"{pre}sc{v}"])
        _sk = ""
        if router:
            wr = psb("rt_w", [128, 8, 128])
            self.memset("pool", wr[:], 0.0, ["rt_w"])
            if "noW" not in _sk:
                self.dma("sp", wr[:, :, 0:NE], self.w["router_w"][0].rearrange("(kc p) e -> p kc e", p=128), (), ["rt_w"])
            pass
        if local is not None:
            idxt = psb("n_idx", [128, self.NLOC], I32)
            self.dma("sp", idxt[:], self.idx_in, (), ["n_idx"])
        for ti in tiles:
            v = 1 if (local is None and self.is_ctx(ti)) else 0
            xn = f"nx{ti % 2}"
            xt = psb(xn, [128, D]) if xn not in t else t[xn]
            if local is None:
                self.dma("sp", xt[:], self.scr["X"][ti * 128:(ti + 1) * 128, :], [f"X@{ti}"], [xn])
            else:
                self.S.dma_gather(xt[:], self.scr["X"], idxt[:, ti:ti + 1], ["n_idx"] + [f"X@{k}" for k in range(self.NT)], [xn])
                self.dma("sp", self.scr["XL"][ti * 128:(ti + 1) * 128, :], xt[:], [xn], [f"XL@{ti}"])
            jn = f"njunk{ti % 2}"
            junk = psb(jn, [128, D]) if jn not in t else t[jn]
            sn = f"nss{ti % 2}"
            ss = psb(sn, [128, 1]) if sn not in t else t[sn]
            self.act(junk[:], xt[:], AF.Square, [xn], [jn, sn], accum=ss[:])
            self.ts("dve", ss[:], ss[:], 1.0 / D, ALU.mult, [sn], [sn], s2=EPS, op1=ALU.add)
            self.act(ss[:], ss[:], AF.Sqrt, [sn], [sn])
            S.op("dve", lambda e, ss=ss: e.reciprocal(out=ss[:], in_=ss[:]), [sn], [sn])
            self.stt(junk[:], xt[:], ss[:], t[f"{pre}sc{v}"][:], ALU.mult, ALU.mult, [xn, sn, f"{pre}sc{v}"], [jn])
            self.tt("pool", junk[:], junk[:], t[f"{pre}sh{v}"][:], ALU.add, [jn, f"{pre}sh{v}"], [jn])
            for half in range(2):
                pn = f"P{half}"
                for k4 in range(4):
                    kc = half * 4 + k4
                    self.tr(t[pn][:, k4 * 128:(k4 + 1) * 128], junk[:, kc * 128:(kc + 1) * 128], [jn], [pn])
                eng = "act" if half == 0 else "dve"
                if router:
                    hn = f"rt_h{half}"
                    hf = psb(hn, [128, 4, 128]) if hn not in t else t[hn]
                    self.cp(eng, hf[:], t[pn][:].rearrange("p (k c) -> p k c", k=4), [pn], [hn])
                    self.cp("pool", hT[:, half * 4:(half + 1) * 4, ti * 128:(ti + 1) * 128], hf[:], [hn], ["hT"])
                else:
                    self.cp(eng, hT[:, half * 4:(half + 1) * 4, ti * 128:(ti + 1) * 128],
                            t[pn][:].rearrange("p (k c) -> p k c", k=4), [pn], ["hT"])
            if router and "rtA" in "":
                continue
            if router:
                for kc in range(8):
                    self.mm(t["P2"][:, 0:128], t[f"rt_h{kc // 4}"][:, kc % 4, :], t["rt_w"][:, kc, :],
                            [f"rt_h{kc // 4}", "rt_w"], ["P2"], start=(kc == 0), stop=(kc == 7))
                self.router_post(stack, ti, local)
        return hT

    def router_post(self, stack, ti, cmbt):
        S, t = self.S, self.t
        import os as _os
        _sk = ""
        psb = lambda name, shape, dt=F32: (self.sb(stack, name, shape, dt) if name not in t else t[name])
        lgt = psb("rt_lg", [128, NE])
        m1 = psb("rt_m1", [128, 1])
        m2 = psb("rt_m2", [128, 1])
        eq = psb("rt_eq", [128, NE])
        ex = psb("rt_ex", [128, NE])
        den = psb("rt_den", [128, 1])
        self.cp("dve", lgt[:], t["P2"][:, 0:NE], ["P2"], ["rt_lg"])
        if "rt1" in _sk:
            return
        S.op("dve", lambda e: e.reduce_max(out=m1[:], in_=lgt[:], axis=AX.X), ["rt_lg"], ["rt_m1"])
        self.ts("dve", eq[:], lgt[:], m1[:], ALU.is_equal, ["rt_lg", "rt_m1"], ["rt_eq"])
        self.stt(eq[:], eq[:], -1e30, lgt[:], ALU.mult, ALU.add, ["rt_eq", "rt_lg"], ["rt_eq"])
        S.op("dve", lambda e: e.reduce_max(out=m2[:], in_=eq[:], axis=AX.X), ["rt_eq"], ["rt_m2"])
        self.ts("dve", eq[:], lgt[:], m2[:], ALU.is_ge, ["rt_lg", "rt_m2"], ["rt_eq"])
        self.ts("dve", m1[:], m1[:], -1.0, ALU.mult, ["rt_m1"], ["rt_m1"])
        self.act(ex[:], lgt[:], AF.Exp, ["rt_lg", "rt_m1"], ["rt_ex"], bias=m1[:])
        self.tt("dve", ex[:], ex[:], eq[:], ALU.mult, ["rt_ex", "rt_eq"], ["rt_ex"])
        S.op("dve", lambda e: e.reduce_sum(out=den[:], in_=ex[:], axis=AX.X), ["rt_ex"], ["rt_den"])
        S.op("dve", lambda e: e.reciprocal(out=den[:], in_=den[:]), ["rt_den"], ["rt_den"])
        self.ts("dve", cmbt[:, ti, :], ex[:], den[:], ALU.mult, ["rt_ex", "rt_den"], ["ff_cmbt"])


class KB2(KB):
    def proj_phase(self, stack, l):
        S, t = self.S, self.t
        T, NT = self.T, self.NT
        psb = lambda name, shape, dt=F32: (self.sb(stack, name, shape, dt) if name not in t else t[name])
        hT = t["hT"]
        for name, rows in (("QH", 512), ("KHF", 512), ("KHB", 512), ("GH", 512), ("QG", 256), ("KG", 256),
                           ("RG", 512), ("U5", 512), ("GA", D), ("GB", D), ("GC", D)):
            if name not in self.scr:
                self.dscr(name, [rows, T])
        for name, cols, dt in (("GHF", 512, F32), ("GHB", 512, F32), ("KHFt", 512, F32), ("KHBt", 512, F32),
                               ("VH", 512, BF16), ("KGt", 256, F32), ("VG", 512, BF16), ("GG", 512, F32)):
            if name not in self.scr:
                self.dscr(name, [T, cols], dt)
        wsrc = self.w["w_in"][l].rearrange("(kc p) n -> p kc n", p=128)
        tbs = [(s, min(512, T - s)) for s in range(0, T, 512)]
        self._wcnt = getattr(self, "_wcnt", 0)

        def load_w(c0, ncol):
            i = self._wcnt % 2
            self._wcnt += 1
            wf = psb(f"pw_f{i}", [128, 8, 512])
            wb = psb(f"pw_b{i}", [128, 8, 512], BF16)
            self.dma("sp", wf[:, :, 0:ncol], wsrc[:, :, c0:c0 + ncol], (), [f"pw_f{i}"])
            self.cp("pool", wb[:, :, 0:ncol], wf[:, :, 0:ncol], [f"pw_f{i}"], [f"pw_b{i}"])
            return wb, f"pw_b{i}"

        self._pcnt = getattr(self, "_pcnt", 0)

        def next_p():
            i = self._pcnt % 4
            self._pcnt += 1
            return t[f"P{i}"], f"P{i}"

        self._ocnt = getattr(self, "_ocnt", 0)

        def next_o(dt=F32):
            i = self._ocnt % 3
            self._ocnt += 1
            nm = f"po_{'b' if dt == BF16 else 'f'}{i}"
            return psb(nm, [128, 512], dt), nm

        lb, oml = t[f"lb_fm{l}"], t[f"oml_fm{l}"]
        lbt, omlt = t[f"lb_tm{l}"], t[f"oml_tm{l}"]

        fm = [(OFF["hg_q"], 512, "QH", "copy"), (OFF["hg_ff"], 512, "KHF", "hk"), (OFF["hg_fb"], 512, "KHB", "hk"),
              (OFF["hg_g"], 512, "GH", "silu"), (OFF["gl_q"], 256, "QG", "q8"), (OFF["gl_k"], 256, "KG", "copy"),
              (OFF["gl_r"], 512, "RG", "silu"), (OFF["s5_u"], 512, "U5", "copy"),
              (OFF["ga"], D, "GA", "sig"), (OFF["gb"], D, "GB", "sig"), (OFF["gc"], D, "GC", "sig")]
        for c0, ncols, sname, kind in fm:
            for cb0 in range(0, ncols, 512):
                nb = min(512, ncols - cb0)
                wb, wn = load_w(c0 + cb0, nb)
                for sub in range(nb // 128):
                    row0 = cb0 + sub * 128
                    for (s0, sl) in tbs:
                        P, pn = next_p()
                        for kc in range(8):
                            self.mm(P[:, 0:sl], wb[:, kc, sub * 128:(sub + 1) * 128], hT[:, kc, s0:s0 + sl],
                                    [wn, "hT"], [pn], start=(kc == 0), stop=(kc == 7))
                        o, on = next_o()
                        if kind == "copy":
                            self.cp("act", o[:, 0:sl], P[:, 0:sl], [pn], [on])
                        elif kind == "q8":
                            self.act(o[:, 0:sl], P[:, 0:sl], AF.Identity, [pn], [on], scale=0.125)
                        elif kind == "silu":
                            self.act(o[:, 0:sl], P[:, 0:sl], AF.Silu, [pn], [on])
                        elif kind == "sig":
                            self.act(o[:, 0:sl], P[:, 0:sl], AF.Sigmoid, [pn], [on])
                        elif kind == "hk":
                            h = row0 // 128
                            self.act(o[:, 0:sl], P[:, 0:sl], AF.Sigmoid, [pn], [on], scale=-1.0)
                            self.ts("dve", o[:, 0:sl], o[:, 0:sl], oml[:, h:h + 1], ALU.mult, [on, f"oml_fm{l}"], [on])
                        self.dma("sp", self.scr[sname][row0:row0 + 128, s0:s0 + sl], o[:, 0:sl], [on],
                                 [f"{sname}@{s0 // 128 + k}" for k in range(sl // 128)])
        tm = [(OFF["hg_ff"], 512, "hgf", ("GHF", "KHFt")), (OFF["hg_fb"], 512, "hgf", ("GHB", "KHBt")),
              (OFF["hg_i"], 512, "vbf", ("VH",)), (OFF["gl_k"], 256, "copy", ("KGt",)), (OFF["gl_v"], 512, "vbf", ("VG",))]
        for c0, ncols, kind, snames in tm:
            wb, wn = load_w(c0, ncols)
            for ti in range(NT):
                P, pn = next_p()
                for kc in range(8):
                    self.mm(P[:, 0:ncols], hT[:, kc, ti * 128:(ti + 1) * 128], wb[:, kc, 0:ncols], [wn, "hT"], [pn],
                            start=(kc == 0), stop=(kc == 7))
                rows = slice(ti * 128, (ti + 1) * 128)
                if kind == "copy":
                    o, on = next_o()
                    self.cp("act", o[:, 0:ncols], P[:, 0:ncols], [pn], [on])
                    self.dma("sp", self.scr[snames[0]][rows, :], o[:, 0:ncols], [on], [f"{snames[0]}@{ti}"])
                elif kind == "vbf":
                    o, on = next_o(BF16)
                    self.cp("act", o[:, 0:ncols], P[:, 0:ncols], [pn], [on])
                    self.dma("sp", self.scr[snames[0]][rows, :], o[:, 0:ncols], [on], [f"{snames[0]}@{ti}"])
                else:
                    o, on = next_o()
                    o2, on2 = next_o()
                    self.act(o[:], P[:], AF.Sigmoid, [pn], [on])
                    self.tt("dve", o[:], o[:], omlt[:], ALU.mult, [on, f"oml_tm{l}"], [on])
                    self.tt("dve", o[:], o[:], lbt[:], ALU.add, [on, f"lb_tm{l}"], [on])
                    self.act(o[:], o[:], AF.Ln, [on], [on])
                    self.dma("sp", self.scr[snames[0]][rows, :], o[:], [on], [f"{snames[0]}@{ti}"])
                    self.act(o2[:], P[:], AF.Sigmoid, [pn], [on2], scale=-1.0)
                    self.tt("dve", o2[:], o2[:], omlt[:], ALU.mult, [on2, f"oml_tm{l}"], [on2])
                    self.dma("sp", self.scr[snames[1]][rows, :], o2[:], [on2], [f"{snames[1]}@{ti}"])
        upf = psb("pg_upf", [33, 512])
        upb = psb("pg_upb", [33, 512], BF16)
        self.memset("pool", upf[:], 0.0, ["pg_upf"])
        self.dma("sp", upf[0:16, 0:256], self.w["gla_gate_up"][l, 0], (), ["pg_upf"])
        self.dma("sp", upf[16:32, 256:512], self.w["gla_gate_up"][l, 1], (), ["pg_upf"])
        self.dma("sp", upf[32:33, :], self.w["gla_gate_b"][l:l + 1].rearrange("o d k -> o (d k)"), (), ["pg_upf"])
        self.cp("dve", upb[:], upf[:], ["pg_upf"], ["pg_upb"])
        wb, wn = load_w(OFF["gl_df"], 32)
        for (s0, sl) in tbs:
            P, pn = next_p()
            for kc in range(8):
                self.mm(P[0:32, 0:sl], wb[:, kc, 0:32], hT[:, kc, s0:s0 + sl], [wn, "hT"], [pn], start=(kc == 0), stop=(kc == 7))
            i = (s0 // 512) % 2
            pdt = psb(f"pg_pd{i}", [33, 512], BF16)
            if s0 < 1024:
                self.memset("pool", pdt[32:33, :], 1.0, [f"pg_pd{i}"])
            self.cp("act", pdt[0:32, 0:sl], P[0:32, 0:sl], [pn], [f"pg_pd{i}"])
            for k in range(sl // 128):
                ti = s0 // 128 + k
                P2, pn2 = next_p()
                self.mm(P2[:], pdt[0:33, k * 128:(k + 1) * 128], upb[0:33, :], [f"pg_pd{i}", "pg_upb"], [pn2])
                o, on = next_o()
                self.act(o[:], P2[:], AF.Sigmoid, [pn2], [on])
                self.act(o[:], o[:], AF.Ln, [on], [on])
                self.ts("dve", o[:], o[:], 1.0 / 16.0, ALU.mult, [on], [on])
                self.dma("sp", self.scr["GG"][ti * 128:(ti + 1) * 128, :], o[:], [on], [f"GG@{ti}"])

    def s5_setup(self, stack, l, d):
        S, t = self.S, self.t
        psb = lambda name, shape, dt=F32: (self.sb(stack, name, shape, dt) if name not in t else t[name])
        W = psb("s5_W", [128, 16, 2, 128])
        V = psb("s5_V", [128, 16, 2, 128])
        BT = psb("s5_BT", [128, 16, 256], BF16)
        CT = psb("s5_CT", [128, 16, 2, 128], BF16)
        with self.phase() as tmp:
            tsb = lambda name, shape, dt=F32: self.sb(tmp, name, shape, dt)
            lre = tsb("s5t_lre", [128, 16])
            lim = tsb("s5t_lim", [128, 16])
            lst = tsb("s5t_lst", [128, 16])
            self.dma("sp", lre[:], self.w["s5_a_re"][l, d].rearrange("g n -> (g n)").rearrange("(j p) -> p j", p=128), (), ["s5t_lre"], allow_slow_non_contiguous=True)
            self.dma("sp", lim[:], self.w["s5_a_im"][l, d].rearrange("g n -> (g n)").rearrange("(j p) -> p j", p=128), (), ["s5t_lim"], allow_slow_non_contiguous=True)
            ls = self.w["s5_log_step"]
            base = ls[l, d, 0:1]
            for g2 in (0, 1):
                src = bass.AP(ls.tensor, base.offset + g2, [[0, 64], [2, 16]])
                self.dma("sp", lst[g2 * 64:(g2 + 1) * 64, :], src, (), ["s5t_lst"], allow_slow_non_contiguous=True)
            self.act(lst[:], lst[:], AF.Exp, ["s5t_lst"], ["s5t_lst"])
            lr = tsb("s5t_lr", [128, 16])
            li = tsb("s5t_li", [128, 16])
            self.tt("dve", lr[:], lre[:], lst[:], ALU.mult, ["s5t_lre", "s5t_lst"], ["s5t_lr"])
            self.tt("dve", li[:], lim[:], lst[:], ALU.mult, ["s5t_lim", "s5t_lst"], ["s5t_li"])
            ti_ = tsb("s5t_ti", [128, 128], I32)
            tau = tsb("s5t_tau", [128, 128])
            if d == 0:
                S.op("pool", lambda e: e.iota(ti_[:], pattern=[[1, 128]], base=1, channel_multiplier=0), (), ["s5t_ti"])
            else:
                S.op("pool", lambda e: e.iota(ti_[:], pattern=[[-1, 128]], base=128, channel_multiplier=0), (), ["s5t_ti"])
            self.cp("dve", tau[:], ti_[:], ["s5t_ti"], ["s5t_tau"])
            mag = tsb("s5t_mag", [128, 16, 128])
            ang = tsb("s5t_ang", [128, 16, 128])
            a2 = tsb("s5t_a2", [128, 16, 128])
            bi = tsb("s5t_bi", [128, 16, 128], I32)
            sn = tsb("s5t_sin", [128, 16, 128])
            cs = tsb("s5t_cos", [128, 16, 128])
            for j in range(16):
                self.act(mag[:, j, :], tau[:], AF.Exp, ["s5t_tau", "s5t_lr"], ["s5t_mag"], scale=lr[:, j:j + 1])
                self.ts("dve", ang[:, j, :], tau[:], li[:, j:j + 1], ALU.mult, ["s5t_tau", "s5t_li"], ["s5t_ang"])
            for dst, phase in ((sn, 0.0), (cs, math.pi / 2)):
                dn = "s5t_sin" if phase == 0.0 else "s5t_cos"
                self.ts("dve", a2[:], ang[:], phase, ALU.add, ["s5t_ang"], ["s5t_a2"])
                self.ts("dve", dst[:], a2[:], 1.0 / TWO_PI, ALU.mult, ["s5t_a2"], [dn])
                self.cp("dve", bi[:], dst[:], [dn], ["s5t_bi"])
                self.cp("dve", dst[:], bi[:], ["s5t_bi"], [dn])
                self.stt(a2[:], dst[:], -TWO_PI, a2[:], ALU.mult, ALU.add, [dn, "s5t_a2"], ["s5t_a2"])
                self.ts("dve", a2[:], a2[:], PI_SAFE, ALU.min, ["s5t_a2"], ["s5t_a2"], s2=-PI_SAFE, op1=ALU.max)
                self.act(dst[:], a2[:], AF.Sin, ["s5t_a2"], [dn])
            self.tt("dve", V[:, :, 0, :], mag[:], cs[:], ALU.mult, ["s5t_mag", "s5t_cos"], ["s5_V"])
            self.tt("dve", V[:, :, 1, :], mag[:], sn[:], ALU.mult, ["s5t_mag", "s5t_sin"], ["s5_V"])
            S.op("dve", lambda e: e.reciprocal(out=mag[:], in_=mag[:]), ["s5t_mag"], ["s5t_mag"])
            wfm = tsb("s5t_wfm", [128, 16, 2, 128])
            self.tt("dve", wfm[:, :, 0, :], mag[:], cs[:], ALU.mult, ["s5t_mag", "s5t_cos"], ["s5t_wfm"])
            self.stt(wfm[:, :, 1, :], mag[:], -1.0, sn[:], ALU.mult, ALU.mult, ["s5t_mag", "s5t_sin"], ["s5t_wfm"])
            k = 0
            for j in range(16):
                for c in range(2):
                    pn = f"P{k % 4}"
                    k += 1
                    self.tr(t[pn][:, 0:128], wfm[:, j, c, :], ["s5t_wfm"], [pn])
                    self.cp("act" if k % 2 else "dve", W[:, j, c, :], t[pn][:, 0:128], [pn], ["s5_W"])
            c1 = 0 if d == 0 else 127
            are = V[:, :, 0, c1]
            aim = V[:, :, 1, c1]
            den = tsb("s5t_den", [128, 16])
            tmp1 = tsb("s5t_t1", [128, 16])
            tmp2 = tsb("s5t_t2", [128, 16])
            ar1 = tsb("s5t_ar1", [128, 16])
            zre = tsb("s5t_zre", [128, 16])
            zim = tsb("s5t_zim", [128, 16])
            self.tt("dve", den[:], lre[:], lre[:], ALU.mult, ["s5t_lre"], ["s5t_den"])
            self.tt("dve", tmp1[:], lim[:], lim[:], ALU.mult, ["s5t_lim"], ["s5t_t1"])
            self.tt("dve", den[:], den[:], tmp1[:], ALU.add, ["s5t_den", "s5t_t1"], ["s5t_den"])
            S.op("dve", lambda e: e.reciprocal(out=den[:], in_=den[:]), ["s5t_den"], ["s5t_den"])
            self.ts("dve", ar1[:], are, -1.0, ALU.add, ["s5_V"], ["s5t_ar1"])
            self.tt("dve", tmp1[:], ar1[:], lre[:], ALU.mult, ["s5t_ar1", "s5t_lre"], ["s5t_t1"])
            self.tt("dve", tmp2[:], aim, lim[:], ALU.mult, ["s5_V", "s5t_lim"], ["s5t_t2"])
            self.tt("dve", tmp1[:], tmp1[:], tmp2[:], ALU.add, ["s5t_t1", "s5t_t2"], ["s5t_t1"])
            self.tt("dve", zre[:], tmp1[:], den[:], ALU.mult, ["s5t_t1", "s5t_den"], ["s5t_zre"])
            self.tt("dve", tmp1[:], aim, lre[:], ALU.mult, ["s5_V", "s5t_lre"], ["s5t_t1"])
            self.tt("dve", tmp2[:], ar1[:], lim[:], ALU.mult, ["s5t_ar1", "s5t_lim"], ["s5t_t2"])
            self.tt("dve", tmp1[:], tmp1[:], tmp2[:], ALU.subtract, ["s5t_t1", "s5t_t2"], ["s5t_t1"])
            self.tt("dve", zim[:], tmp1[:], den[:], ALU.mult, ["s5t_t1", "s5t_den"], ["s5t_zim"])
            bre = tsb("s5t_bre", [128, 16, 16])
            bim = tsb("s5t_bim", [128, 16, 16])
            self.dma("sp", bre[:], self.w["s5_b_re"][l, d].rearrange("g n q -> (g n) q").rearrange("(j p) q -> p j q", p=128), (), ["s5t_bre"])
            self.dma("sp", bim[:], self.w["s5_b_im"][l, d].rearrange("g n q -> (g n) q").rearrange("(j p) q -> p j q", p=128), (), ["s5t_bim"])
            bbr = tsb("s5t_bbr", [128, 16, 16])
            bbi = tsb("s5t_bbi", [128, 16, 16])
            u1 = tsb("s5t_u1", [128, 16, 16])
            zr_b = zre[:].unsqueeze(2).to_broadcast([128, 16, 16])
            zi_b = zim[:].unsqueeze(2).to_broadcast([128, 16, 16])
            self.tt("dve", bbr[:], bre[:], zr_b, ALU.mult, ["s5t_bre", "s5t_zre"], ["s5t_bbr"])
            self.tt("dve", u1[:], bim[:], zi_b, ALU.mult, ["s5t_bim", "s5t_zim"], ["s5t_u1"])
            self.tt("dve", bbr[:], bbr[:], u1[:], ALU.subtract, ["s5t_bbr", "s5t_u1"], ["s5t_bbr"])
            self.tt("dve", bbi[:], bim[:], zr_b, ALU.mult, ["s5t_bim", "s5t_zre"], ["s5t_bbi"])
            self.tt("dve", u1[:], bre[:], zi_b, ALU.mult, ["s5t_bre", "s5t_zim"], ["s5t_u1"])
            self.tt("dve", bbi[:], bbi[:], u1[:], ALU.add, ["s5t_bbi", "s5t_u1"], ["s5t_bbi"])
            bm = tsb("s5t_bm", [128, 16, 2, 128])
            for j in range(16):
                mk = t[f"mask{j % 4}"]
                for c, src, sname in ((0, bbr, "s5t_bbr"), (1, bbi, "s5t_bbi")):
                    self.tt("dve", bm[:, j, c, :].rearrange("p (r q) -> p r q", r=8),
                            src[:, j, :].unsqueeze(1).to_broadcast([128, 8, 16]),
                            mk[:].rearrange("p (r q) -> p r q", r=8), ALU.mult, [sname, f"mask{j % 4}"], ["s5t_bm"])
            for j in range(16):
                for c in range(2):
                    pn = f"P{k % 4}"
                    k += 1
                    self.tr(t[pn][:, 0:128], bm[:, j, c, :], ["s5t_bm"], [pn])
                    self.cp("act" if k % 2 else "dve", BT[:, j, c * 128:(c + 1) * 128], t[pn][:, 0:128], [pn], ["s5_BT"])
            for c, cname in ((0, "s5_c_re"), (1, "s5_c_im")):
                ctm = tsb(f"s5t_ctm{c}", [128, 4, 2, 64])
                src = self.w[cname][l, d].rearrange("g p n -> (g p) n").rearrange("(b rp) n -> rp b n", rp=128)
                for dup in range(2):
                    self.dma("sp", ctm[:, :, dup, :], src, (), [f"s5t_ctm{c}"])
                for b in range(4):
                    pn = f"P{k % 4}"
                    k += 1
                    self.tr(t[pn][:, 0:128], ctm[:, b, :, :].rearrange("p a n -> p (a n)"), [f"s5t_ctm{c}"], [pn])
                    for kk in range(4):
                        j = 4 * b + kk
                        self.stt(CT[:, j, c, :], t[pn][:, 0:128], 1.0 if c == 0 else -1.0, t[f"mask{kk}"][:], ALU.mult, ALU.mult,
                                 [pn, f"mask{kk}"], ["s5_CT"])


FAMS = {
    "hg": dict(G=4, W=512, g=("GHF", "GHB"), kt=("KHFt", "KHBt"), v="VH", q="QH", kf=("KHF", "KHB"),
               heads=[(h, 0, 128) for h in range(4)], orow=0),
    "gl": dict(G=2, W=256, g=("GG", "GG"), kt=("KGt", "KGt"), v="VG", q="QG", kf=("KG", "KG"),
               heads=[(h // 2, (h % 2) * 64, 64) for h in range(4)], orow=512),
}


class KB3(KB2):
    def rec_pass(self, l, d):
        S, t = self.S, self.t
        T, NT, nct = self.T, self.NT, self.nct
        for nm, rows in ((f"O{d}", 1024), (f"Y5{d}", 512)):
            if nm not in self.scr:
                self.dscr(nm, [rows, T])
        order = list(range(NT)) if d == 0 else (list(range(nct - 1, -1, -1)) + list(range(NT - 1, nct - 1, -1)))
        last = 127 if d == 0 else 0
        with self.phase() as ph:
            psb = lambda name, shape, dt=F32: (self.sb(ph, name, shape, dt) if name not in t else t[name])
            self.s5_setup(ph, l, d)
            minc, mstr, mincb = t[f"minc{d}"], t[f"mstr{d}"], t[f"mincb{d}"]
            mincn, mstrn, mincbn = f"minc{d}", f"mstr{d}", f"mincb{d}"
            W5, V5, BT5, CT5 = t["s5_W"], t["s5_V"], t["s5_BT"], t["s5_CT"]
            for f in ("hg", "gl"):
                psb(f"{f}_Sf", [128, 4, 128])
                psb(f"{f}_Sb", [128, 4, 128], BF16)
                self.memset("pool", t[f"{f}_Sf"][:], 0.0, [f"{f}_Sf"])
                self.memset("pool", t[f"{f}_Sb"][:], 0.0, [f"{f}_Sb"])
            carry = psb("s5_carry", [128, 16, 2])
            self.memset("pool", carry[:], 0.0, ["s5_carry"])
            dbuf = psb("r_D", [128, 2, 8, 128])

            def loads(ti, par):
                rows = slice(ti * 128, (ti + 1) * 128)
                cols = slice(ti * 128, (ti + 1) * 128)
                for f, F_ in FAMS.items():
                    G, W = F_["G"], F_["W"]
                    gt = psb(f"{f}_g{par}", [128, W])
                    gsrc = self.scr[F_["g"][d]]
                    gs = gsrc[rows, :] if f == "hg" else gsrc[rows, d * 256:(d + 1) * 256]
                    self.dma("sp", gt[:], gs, [f"{F_['g'][d]}@{ti}"], [f"{f}_g{par}"])
                    kt = psb(f"{f}_kt{par}", [128, W])
                    self.dma("sp", kt[:], self.scr[F_["kt"][d]][rows, :], [f"{F_['kt'][d]}@{ti}"], [f"{f}_kt{par}"])
                    vt = psb(f"{f}_v{par}", [128, 512], BF16)
                    self.dma("sp", vt[:], self.scr[F_["v"]][rows, :], [f"{F_['v']}@{ti}"], [f"{f}_v{par}"])
                    qf = psb(f"{f}_q{par}", [128, G, 128])
                    self.dma("sp", qf[:], self.scr[F_["q"]].rearrange("(g p) t -> p g t", p=128)[:, :, cols], [f"{F_['q']}@{ti}"], [f"{f}_q{par}"])
                    kf = psb(f"{f}_kf{par}", [128, G, 128])
                    self.dma("sp", kf[:], self.scr[F_["kf"][d]].rearrange("(g p) t -> p g t", p=128)[:, :, cols], [f"{F_['kf'][d]}@{ti}"], [f"{f}_kf{par}"])
                u = psb(f"s5_u{par}", [128, 4, 128])
                self.dma("sp", u[:], self.scr["U5"].rearrange("(g p) t -> p g t", p=128)[:, :, cols], [f"U5@{ti}"], [f"s5_u{par}"])

            def fam_tile(f, ti, par):
                F_ = FAMS[f]
                G, W, heads = F_["G"], F_["W"], F_["heads"]
                gt, kt, vt, qf, kf = (t[f"{f}_{x}{par}"] for x in ("g", "kt", "v", "q", "kf"))
                gn, ktn, vn, qn, kfn = (f"{f}_{x}{par}" for x in ("g", "kt", "v", "q", "kf"))
                Sf, Sb = t[f"{f}_Sf"], t[f"{f}_Sb"]
                PE_, PB, PS, PO, PU = (t[f"P{i}"] for i in range(5))
                ex = psb(f"{f}_ex", [128, W])
                if f == "hg":
                    khat = psb(f"{f}_khat", [128, 4, 128], BF16)
                else:
                    fresh = "gl_khat" not in t
                    khat = psb("gl_khat", [128, 4, 128], BF16)
                    if fresh:
                        self.memset("pool", khat[:], 0.0, ["gl_khat"])
                bt = psb(f"{f}_bt", [128, G, 128])
                eb = psb(f"{f}_eb", [128, G, 128])
                qbar = psb(f"{f}_qbar", [128, G, 128], BF16)
                bprev = psb(f"{f}_bprev", [128, G, 8])
                bq = psb(f"{f}_bq", [128, G, 128])
                qt = psb(f"{f}_qt", [128, G, 128], BF16)
                pt = psb(f"{f}_pt", [128, 4, 128], BF16)
                osb = psb(f"{f}_osb", [128, 4, 128])
                ktb = psb(f"{f}_Kt", [128, 4, 8, 128], BF16)
                self.mm(PE_[:, 0:W], mstr[:], gt[:], [mstrn, gn], ["P0"])
                self.act(ex[:], PE_[:, 0:W], AF.Exp, ["P0"], [f"{f}_ex"])
                if f == "hg":
                    self.tt("dve", khat[:].rearrange("p h c -> p (h c)"), ex[:], kt[:], ALU.mult, [f"{f}_ex", ktn], [f"{f}_khat"])
                else:
                    kb_ = khat[:]
                    kout = bass.AP(kb_.tensor, kb_.offset, [list(kb_.ap[0]), [256, 2], [192, 2], [1, 64]])
                    self.tt("dve", kout, ex[:].rearrange("p (g a c) -> p g a c", g=2, a=2), kt[:].rearrange("p (g a c) -> p g a c", g=2, a=2),
                            ALU.mult, [f"{f}_ex", ktn], [f"{f}_khat"])
                yield
                for g in range(G):
                    self.mm(PB[:, g * 128:(g + 1) * 128], gt[:, g * 128:(g + 1) * 128], minc[:], [gn, mincn], ["P1"])
                pb3 = PB[:, 0:G * 128].rearrange("p (g c) -> p g c", g=G)
                self.cp("act", bt[:], pb3, ["P1"], [f"{f}_bt"])
                self.act(eb[:], pb3, AF.Exp, ["P1"], [f"{f}_eb"])
                self.tt("dve", qbar[:], qf[:], eb[:], ALU.mult, [qn, f"{f}_eb"], [f"{f}_qbar"])
                yield
                if d == 0:
                    self.memset("pool", bprev[:, :, 0:1], 0.0, [f"{f}_bprev"])
                    self.cp("pool", bprev[:, :, 1:8], bt[:, :, 15:127:16], [f"{f}_bt"], [f"{f}_bprev"])
                else:
                    self.memset("pool", bprev[:, :, 7:8], 0.0, [f"{f}_bprev"])
                    self.cp("pool", bprev[:, :, 0:7], bt[:, :, 16:128:16], [f"{f}_bt"], [f"{f}_bprev"])
                self.tt("dve", bq[:].rearrange("p g (i j) -> p g i j", i=8), bt[:].rearrange("p g (i j) -> p g i j", i=8),
                        bprev[:].unsqueeze(3).to_broadcast([128, G, 8, 16]), ALU.subtract, [f"{f}_bt", f"{f}_bprev"], [f"{f}_bq"])
                self.act(bq[:], bq[:], AF.Exp, [f"{f}_bq"], [f"{f}_bq"])
                self.tt("dve", qt[:], bq[:], qf[:], ALU.mult, [f"{f}_bq", qn], [f"{f}_qt"])
                yield
                if f == "gl":
                    kfm = psb("gl_kfm", [128, 4, 128])
                    for h, (g, p0, ps_) in enumerate(heads):
                        self.ts("dve", kfm[:, h, :], kf[:, g, :], t[f"hm{h % 2}"][:, 0:1], ALU.mult, [kfn, f"hm{h % 2}"], ["gl_kfm"])
                for g0 in range(0, G, 2):
                    dv = dbuf[:, 0:2, :, :]
                    self.tt("dve", dv, bprev[:, g0:g0 + 2, :].unsqueeze(3).to_broadcast([128, 2, 8, 128]),
                            bt[:, g0:g0 + 2, :].unsqueeze(2).to_broadcast([128, 2, 8, 128]), ALU.subtract, [f"{f}_bprev", f"{f}_bt"], ["r_D"])
                    self.ts("pool", dv, dv, 60.0, ALU.min, ["r_D"], ["r_D"])
                    self.act(dv, dv, AF.Exp, ["r_D"], ["r_D"])
                    if f == "hg":
                        self.tt("dve", ktb[:, g0:g0 + 2, :, :], dv, kf[:, g0:g0 + 2, :].unsqueeze(2).to_broadcast([128, 2, 8, 128]), ALU.mult, ["r_D", kfn], [f"{f}_Kt"])
                    else:
                        for h, (g, p0, ps_) in enumerate(heads):
                            self.tt("dve", ktb[:, h, :, :], dbuf[:, g, :, :], kfm[:, h, :].unsqueeze(1).to_broadcast([128, 8, 128]), ALU.mult,
                                    ["r_D", "gl_kfm"], [f"{f}_Kt"])
                yield
                for h, (g, p0, ps_) in enumerate(heads):
                    for i in range(8):
                        self.mm(PS[:, h * 128 + 16 * i:h * 128 + 16 * i + 16], ktb[:, h, i, :],
                                qt[:, g, 16 * i:16 * i + 16], [f"{f}_Kt", f"{f}_qt"], ["P2"])
                self.tt("dve", pt[:], PS[:].rearrange("p (h c) -> p h c", h=4), minc[:].unsqueeze(1).to_broadcast([128, 4, 128]),
                        ALU.mult, ["P2", mincn], [f"{f}_pt"])
                yield
                for h, (g, p0, ps_) in enumerate(heads):
                    self.mm(PO[:, h * 128:(h + 1) * 128], vt[:, h * 128:(h + 1) * 128], pt[:, h, :], [vn, f"{f}_pt"], ["P3"], start=True, stop=False)
                    self.mm(PO[:, h * 128:(h + 1) * 128], Sb[:, h, :], qbar[:, g, :], [f"{f}_Sb", f"{f}_qbar"], ["P3"], start=False, stop=True)
                self.cp("act", osb[:], PO[:].rearrange("p (h c) -> p h c", h=4), ["P3"], [f"{f}_osb"])
                orow = F_["orow"]
                self.dma("act", self.scr[f"O{d}"][orow:orow + 512, :].rearrange("(h p) t -> p h t", p=128)[:, :, ti * 128:(ti + 1) * 128],
                         osb[:], [f"{f}_osb"], [f"O{d}{f}@{ti}"])
                yield
                for h, (g, p0, ps_) in enumerate(heads):
                    self.mm(PU[:, h * 128:(h + 1) * 128], khat[:, h, :], vt[:, h * 128:(h + 1) * 128], [f"{f}_khat", vn], ["P4"])
                for h, (g, p0, ps_) in enumerate(heads):
                    self.stt(Sf[:, h, :], Sf[:, h, :], eb[:, g, last:last + 1], PU[:, h * 128:(h + 1) * 128], ALU.mult, ALU.add,
                             [f"{f}_Sf", f"{f}_eb", "P4"], [f"{f}_Sf"])
                self.cp("act", Sb[:], Sf[:], [f"{f}_Sf"], [f"{f}_Sb"])
                yield

            def s5_tile(ti, par):
                u = t[f"s5_u{par}"]
                un = f"s5_u{par}"
                ub = psb("s5_ub", [128, 4, 128], BF16)
                self.cp("pool", ub[:], u[:], [un], ["s5_ub"])
                PBU, PZ, PY = t["P5"], t["P6"], t["P7"]
                sbf = psb("s5_sbf", [128, 16, 2, 128], BF16)
                for r in range(8):
                    for jj in range(2):
                        j = 2 * r + jj
                        self.mm(PBU[:, jj * 256:(jj + 1) * 256], ub[:, j // 4, :], BT5[:, j, :], ["s5_ub", "s5_BT"], ["P5"])
                    yield
                    pbu = PBU[:].rearrange("p (a c n) -> p a c n", a=2, c=2)
                    Wv = W5[:, 2 * r:2 * r + 2, :, :]
                    t1 = psb(f"s5_t1_{r % 2}", [128, 2, 128])
                    t2 = psb(f"s5_t2_{r % 2}", [128, 2, 128])
                    x = psb(f"s5_x{r % 2}", [128, 2, 2, 128], BF16)
                    xn = f"s5_x{r % 2}"
                    self.tt("dve", t1[:], pbu[:, :, 0, :], Wv[:, :, 0, :], ALU.mult, ["P5", "s5_W"], [f"s5_t1_{r % 2}"])
                    self.tt("dve", t2[:], pbu[:, :, 1, :], Wv[:, :, 1, :], ALU.mult, ["P5", "s5_W"], [f"s5_t2_{r % 2}"])
                    self.tt("dve", x[:, :, 0, :], t1[:], t2[:], ALU.subtract, [f"s5_t1_{r % 2}", f"s5_t2_{r % 2}"], [xn])
                    self.tt("dve", t1[:], pbu[:, :, 0, :], Wv[:, :, 1, :], ALU.mult, ["P5", "s5_W"], [f"s5_t1_{r % 2}"])
                    self.tt("dve", t2[:], pbu[:, :, 1, :], Wv[:, :, 0, :], ALU.mult, ["P5", "s5_W"], [f"s5_t2_{r % 2}"])
                    self.tt("dve", x[:, :, 1, :], t1[:], t2[:], ALU.add, [f"s5_t1_{r % 2}", f"s5_t2_{r % 2}"], [xn])
                    yield
                    zs = psb(f"s5_zs_{r % 2}", [128, 2, 2, 128])
                    for jj in range(2):
                        for c in range(2):
                            col = (jj * 2 + c) * 128
                            self.mm(PZ[:, col:col + 128], x[:, jj, c, :], mincb[:], [xn, mincbn], ["P6"])
                    for jj in range(2):
                        for c in range(2):
                            col = (jj * 2 + c) * 128
                            self.act(zs[:, jj, c, :], PZ[:, col:col + 128], AF.Identity, ["P6", "s5_carry"], [f"s5_zs_{r % 2}"],
                                     bias=carry[:, 2 * r + jj, c:c + 1])
                    yield
                    Vv = V5[:, 2 * r:2 * r + 2, :, :]
                    sre = psb(f"s5_sre_{r % 2}", [128, 2, 128])
                    sim = psb(f"s5_sim_{r % 2}", [128, 2, 128])
                    u1 = psb(f"s5_m1_{r % 2}", [128, 2, 128])
                    u2 = psb(f"s5_m2_{r % 2}", [128, 2, 128])
                    self.tt("dve", u1[:], zs[:, :, 0, :], Vv[:, :, 0, :], ALU.mult, [f"s5_zs_{r % 2}", "s5_V"], [f"s5_m1_{r % 2}"])
                    self.tt("pool", u2[:], zs[:, :, 1, :], Vv[:, :, 1, :], ALU.mult, [f"s5_zs_{r % 2}", "s5_V"], [f"s5_m2_{r % 2}"])
                    self.tt("dve", sre[:], u1[:], u2[:], ALU.subtract, [f"s5_m1_{r % 2}", f"s5_m2_{r % 2}"], [f"s5_sre_{r % 2}"])
                    self.tt("dve", u1[:], zs[:, :, 0, :], Vv[:, :, 1, :], ALU.mult, [f"s5_zs_{r % 2}", "s5_V"], [f"s5_m1_{r % 2}"])
                    self.tt("pool", u2[:], zs[:, :, 1, :], Vv[:, :, 0, :], ALU.mult, [f"s5_zs_{r % 2}", "s5_V"], [f"s5_m2_{r % 2}"])
                    self.tt("dve", sim[:], u1[:], u2[:], ALU.add, [f"s5_m1_{r % 2}", f"s5_m2_{r % 2}"], [f"s5_sim_{r % 2}"])
                    yield
                    self.cp("pool", sbf[:, 2 * r:2 * r + 2, 0, :], sre[:], [f"s5_sre_{r % 2}"], ["s5_sbf"])
                    self.cp("pool", sbf[:, 2 * r:2 * r + 2, 1, :], sim[:], [f"s5_sim_{r % 2}"], ["s5_sbf"])
                    self.cp("pool", carry[:, 2 * r:2 * r + 2, 0], sre[:, :, last], [f"s5_sre_{r % 2}"], ["s5_carry"])
                    self.cp("pool", carry[:, 2 * r:2 * r + 2, 1], sim[:, :, last], [f"s5_sim_{r % 2}"], ["s5_carry"])
                yield
                for b in range(4):
                    for kk in range(4):
                        j = 4 * b + kk
                        self.mm(PY[:, b * 128:(b + 1) * 128], CT5[:, j, 0, :], sbf[:, j, 0, :], ["s5_CT", "s5_sbf"], ["P7"], start=(kk == 0), stop=False)
                        self.mm(PY[:, b * 128:(b + 1) * 128], CT5[:, j, 1, :], sbf[:, j, 1, :], ["s5_CT", "s5_sbf"], ["P7"], start=False, stop=(kk == 3))
                ysb = psb("s5_ysb", [128, 4, 128])
                self.cp("act", ysb[:], PY[:].rearrange("p (b c) -> p b c", b=4), ["P7"], ["s5_ysb"])
                self.dma("act", self.scr[f"Y5{d}"].rearrange("(b p) t -> p b t", p=128)[:, :, ti * 128:(ti + 1) * 128], ysb[:],
                         ["s5_ysb"], [f"Y5{d}@{ti}"])
                yield

            import os as _os
            skip = ""
            if "all" in skip:
                return
            loads(order[0], 0)
            for n, ti in enumerate(order):
                par = n % 2
                if n + 1 < len(order):
                    loads(order[n + 1], 1 - par)
                gens = []
                if "hg" not in skip:
                    gens.append(fam_tile("hg", ti, par))
                if "gl" not in skip:
                    gens.append(fam_tile("gl", ti, par))
                if "s5" not in skip:
                    gens.append(s5_tile(ti, par))
                while gens:
                    for g_ in list(gens):
                        try:
                            next(g_)
                        except StopIteration:
                            gens.remove(g_)


class KB4(KB3):
    def load_cast(self, stack, dst, dst_name, src_ap, nk, ncols, kstep):
        t = self.t
        for k0 in range(0, nk, kstep):
            kn = min(kstep, nk - k0)
            sn = f"lc_st{kstep}_{self._lc % 2}"
            self._lc += 1
            stg = self.sb(stack, sn, [128, kstep, 1024]) if sn not in t else t[sn]
            self.dma("sp", stg[:, 0:kn, 0:ncols], src_ap[:, k0:k0 + kn, :], (), [sn])
            self.cp("pool", dst[:, k0:k0 + kn, 0:ncols], stg[:, 0:kn, 0:ncols], [sn], [dst_name])

    def merge_phase(self, l, last):
        S, t = self.S, self.t
        T, NT = self.T, self.NT
        self._lc = 0
        with self.phase() as ph:
            psb = lambda name, shape, dt=F32: (self.sb(ph, name, shape, dt) if name not in t else t[name])
            bp = psb("mg_bp", [128, 12, D], BF16)
            wo = psb("mg_wo", [128, 8, D], BF16)
            glu = psb("mg_glu", [128, 4, 512], BF16)
            with self.phase() as tmpw:
                self.load_cast(tmpw, bp, "mg_bp", self.w["branch_proj"][l].rearrange("b (kc p) n -> p (b kc) n", p=128), 12, D, 4)
                self.load_cast(tmpw, wo, "mg_wo", self.w["w_out"][l].rearrange("(kc p) n -> p kc n", p=128), 8, D, 4)
                self.load_cast(tmpw, glu, "mg_glu", self.w["s5_glu_w"][l].rearrange("(kc p) n -> p kc n", p=128), 4, 512, 4)
            gains = {}
            for nm, src in (("hgn", "hg_norm_g"), ("gln", "gla_norm_g"), ("s5d", "s5_d")):
                g = psb("mg_" + nm, [128, 4])
                self.dma("sp", g[:], self.w[src][l].rearrange("(h p) -> p h", p=128), (), ["mg_" + nm], allow_slow_non_contiguous=True)
                gains[nm] = g
            self.load_mod(ph, l, 2, "mg_g1")
            epsT = psb("mg_eps", [128, 1])
            self.memset("pool", epsT[:], EPS, ["mg_eps"])
            tiles = [ti for ti in range(NT) if not (last and self.is_ctx(ti))]

            def ld(name, par, src_ap, shape, deps, dt=F32):
                nm = f"mg_{name}{par}"
                h = psb(nm, shape, dt)
                self.dma("sp", h[:], src_ap, deps, [nm])
                return h

            def loads(ti, par):
                cols = slice(ti * 128, (ti + 1) * 128)
                fmv = lambda ap: ap.rearrange("(h p) t -> p h t", p=128)[:, :, cols]
                for d in (0, 1):
                    ld(f"ohg{d}", par, fmv(self.scr[f"O{d}"][0:512, :]), [128, 4, 128], [f"O{d}hg@{ti}"])
                    ld(f"ogl{d}", par, fmv(self.scr[f"O{d}"][512:1024, :]), [128, 4, 128], [f"O{d}gl@{ti}"])
                    ld(f"y5{d}", par, fmv(self.scr[f"Y5{d}"]), [128, 4, 128], [f"Y5{d}@{ti}"])
                ld("u5", par, fmv(self.scr["U5"]), [128, 4, 128], [f"U5@{ti}"])
                ld("gh", par, fmv(self.scr["GH"]), [128, 4, 128], [f"GH@{ti}"])
                ld("rg", par, fmv(self.scr["RG"]), [128, 4, 128], [f"RG@{ti}"])
                for nm in ("GA", "GB", "GC"):
                    ld(nm, par, fmv(self.scr[nm]), [128, 8, 128], [f"{nm}@{ti}"])
                ld("x", par, self.scr["X"][ti * 128:(ti + 1) * 128, :], [128, D], [f"X@{ti}"])

            def compute(ti, par):
                v = 1 if self.is_ctx(ti) else 0
                g = lambda name: (t[f"mg_{name}{par}"], f"mg_{name}{par}")
                ybr = {}
                for bi, (fam, gate, gain) in enumerate((("hg", "gh", "hgn"), ("gl", "rg", "gln"))):
                    o0, o0n = g(f"o{fam}0")
                    o1, o1n = g(f"o{fam}1")
                    gt, gtn = g(gate)
                    o = psb(f"mg_o{bi}", [128, 4, 128])
                    osq = psb(f"mg_osq{bi}", [128, 4, 128])
                    rs = psb(f"mg_rs{bi}", [128, 4, 128])
                    yb = psb(f"mg_y{bi}", [128, 4, 128], BF16)
                    on, sqn, rsn, ybn = f"mg_o{bi}", f"mg_osq{bi}", f"mg_rs{bi}", f"mg_y{bi}"
                    self.tt("dve", o[:], o0[:], o1[:], ALU.add, [o0n, o1n], [on])
                    self.tt("pool", osq[:], o[:], o[:], ALU.mult, [on], [sqn])
                    self.mm(t["P0"][:], t["ones"][:], osq[:].rearrange("p h c -> p (h c)"), ["ones", sqn], ["P0"])
                    self.act(rs[:].rearrange("p h c -> p (h c)"), t["P0"][:], AF.Sqrt, ["P0", "mg_eps"], [rsn], bias=epsT[:], scale=1.0 / 128.0)
                    S.op("dve", lambda e, rs=rs: e.reciprocal(out=rs[:], in_=rs[:]), [rsn], [rsn])
                    self.tt("dve", o[:], o[:], rs[:], ALU.mult, [on, rsn], [on])
                    for h in range(4):
                        self.stt(yb[:, h, :], o[:, h, :], gains[gain][:, h:h + 1], gt[:, h, :], ALU.mult, ALU.mult,
                                 [on, "mg_" + gain, gtn], [ybn])
                    ybr[bi] = (yb, ybn)
                u5, u5n = g("u5")
                y50, y50n = g("y50")
                y51, y51n = g("y51")
                yy = psb("mg_yy", [128, 4, 128])
                tq = psb("mg_tq", [128, 4, 128])
                for k in range(4):
                    self.stt(yy[:, k, :], u5[:, k, :], gains["s5d"][:, k:k + 1], y50[:, k, :], ALU.mult, ALU.add, [u5n, "mg_s5d", y50n], ["mg_yy"])
                self.tt("dve", yy[:], yy[:], y51[:], ALU.add, ["mg_yy", y51n], ["mg_yy"])
                self.tt("pool", tq[:], yy[:], yy[:], ALU.mult, ["mg_yy"], ["mg_tq"])
                self.ts("pool", tq[:], tq[:], 0.044715, ALU.mult, ["mg_tq"], ["mg_tq"], s2=1.0, op1=ALU.add)
                self.tt("pool", tq[:], tq[:], yy[:], ALU.mult, ["mg_tq", "mg_yy"], ["mg_tq"])
                self.act(tq[:], tq[:], AF.Tanh, ["mg_tq"], ["mg_tq"], scale=math.sqrt(2.0 / math.pi))
                self.ts("dve", tq[:], tq[:], 1.0, ALU.add, ["mg_tq"], ["mg_tq"], s2=0.5, op1=ALU.mult)
                self.tt("dve", yy[:], yy[:], tq[:], ALU.mult, ["mg_yy", "mg_tq"], ["mg_yy"])
                ygb = psb("mg_ygb", [128, 4, 128], BF16)
                self.cp("pool", ygb[:], yy[:], ["mg_yy"], ["mg_ygb"])
                for oc in range(4):
                    for kc in range(4):
                        self.mm(t["P1"][:, oc * 128:(oc + 1) * 128], glu[:, kc, oc * 128:(oc + 1) * 128], ygb[:, kc, :],
                                ["mg_glu", "mg_ygb"], ["P1"], start=(kc == 0), stop=(kc == 3))
                self.act(tq[:].rearrange("p h c -> p (h c)"), t["P1"][:], AF.Sigmoid, ["P1"], ["mg_tq"])
                yc = psb("mg_y2", [128, 4, 128], BF16)
                self.tt("dve", yc[:], yy[:], tq[:], ALU.mult, ["mg_yy", "mg_tq"], ["mg_y2"])
                ybr[2] = (yc, "mg_y2")
                mg = psb("mg_m", [128, 8, 128])
                mt = psb("mg_mt", [128, 8, 128])
                for bi, gname in enumerate(("GA", "GB", "GC")):
                    yb, ybn = ybr[bi]
                    gt, gtn = g(gname)
                    for oc in range(8):
                        pn = f"P{2 + oc // 4}"
                        for kc in range(4):
                            self.mm(t[pn][:, (oc % 4) * 128:(oc % 4 + 1) * 128], bp[:, bi * 4 + kc, oc * 128:(oc + 1) * 128], yb[:, kc, :],
                                    ["mg_bp", ybn], [pn], start=(kc == 0), stop=(kc == 3))
                    for half in range(2):
                        pn = f"P{2 + half}"
                        pv = t[pn][:].rearrange("p (o c) -> p o c", o=4)
                        dst = mg if bi == 0 else mt
                        dn = "mg_m" if bi == 0 else "mg_mt"
                        self.tt("dve", dst[:, half * 4:(half + 1) * 4, :], pv, gt[:, half * 4:(half + 1) * 4, :], ALU.mult, [pn, gtn], [dn])
                    if bi > 0:
                        self.tt("pool", mg[:], mg[:], mt[:], ALU.add, ["mg_m", "mg_mt"], ["mg_m"])
                mgb = psb("mg_mb", [128, 8, 128], BF16)
                self.cp("pool", mgb[:], mg[:], ["mg_m"], ["mg_mb"])
                x, xn = g("x")
                xt_ = psb("mg_xt", [128, D])
                for half in range(2):
                    pn = f"P{4 + half}"
                    for kc in range(8):
                        self.mm(t[pn][:], mgb[:, kc, :], wo[:, kc, half * 512:(half + 1) * 512], ["mg_mb", "mg_wo"], [pn],
                                start=(kc == 0), stop=(kc == 7))
                    self.tt("dve", xt_[:, half * 512:(half + 1) * 512], t[pn][:], t[f"mg_g1{v}"][:, half * 512:(half + 1) * 512], ALU.mult,
                            [pn, f"mg_g1{v}"], ["mg_xt"])
                self.tt("pool", x[:], x[:], xt_[:], ALU.add, [xn, "mg_xt"], [xn])
                self.dma("act", self.scr["X"][ti * 128:(ti + 1) * 128, :], x[:], [xn], [f"X@{ti}"])

            loads(tiles[0], 0)
            for n, ti in enumerate(tiles):
                if n + 1 < len(tiles):
                    loads(tiles[n + 1], (n + 1) % 2)
                compute(ti, n % 2)

    def ffn_phase(self, l, moe):
        S, t = self.S, self.t
        T, NT = self.T, self.NT
        self._lc = 0
        GF = 4
        if moe:
            NL = self.NLOC
            tiles = list(range(NL))
            TT_ = NL * 128
            if "XL" not in self.scr:
                self.dscr("XL", [TT_, D])
            xs, xtag = self.scr["XL"], "XL"
        else:
            tiles = list(range(NT))
            TT_ = T
            xs, xtag = self.scr["X"], "X"
        tbs = [(s_, min(512, TT_ - s_)) for s_ in range(0, TT_, 512)]
        with self.phase() as ph:
            psb = lambda name, shape, dt=F32: (self.sb(ph, name, shape, dt) if name not in t else t[name])
            cmbt = psb("ff_cmbt", [128, self.NLOC, NE]) if moe else None
            hT = self.norm_phase(ph, l, True, tiles, router=moe, local=cmbt)
            self.load_mod(ph, l, 5, "ff_g2")
            ag = psb("ff_a", [128, GF, TT_], BF16)
            w2g = psb("ff_w2", [128, GF, D], BF16)
            experts = list(range(NE)) if moe else [None]
            cnt = 0
            for e in experts:
                if moe:
                    w1s, w3s, w2s = self.w["moe_w1"][0, e], self.w["moe_w3"][0, e], self.w["moe_w2"][0, e]
                else:
                    w1s, w3s, w2s = self.w["ffn_w1"][0], self.w["ffn_w3"][0], self.w["ffn_w2"][0]
                w1v = w1s.rearrange("(kc p) f -> p kc f", p=128)
                w3v = w3s.rearrange("(kc p) f -> p kc f", p=128)
                w2v = w2s.rearrange("(fc p) n -> p fc n", p=128)
                for grp in range(FF // 128 // GF):
                    for fi in range(GF):
                        fc = grp * GF + fi
                        i = cnt % 2
                        cnt += 1
                        wf = psb(f"ff_wf{i}", [128, 2, 8, 128])
                        wb = psb(f"ff_wb{i}", [128, 2, 8, 128], BF16)
                        self.dma("sp", wf[:, 0, :, :], w1v[:, :, fc * 128:(fc + 1) * 128], (), [f"ff_wf{i}"])
                        self.dma("sp", wf[:, 1, :, :], w3v[:, :, fc * 128:(fc + 1) * 128], (), [f"ff_wf{i}"])
                        self.cp("pool", wb[:], wf[:], [f"ff_wf{i}"], [f"ff_wb{i}"])
                        for bi_, (s0, sl) in enumerate(tbs):
                            pa, pb_ = f"P{(bi_ % 2) * 2}", f"P{(bi_ % 2) * 2 + 1}"
                            for kc in range(8):
                                self.mm(t[pa][:, 0:sl], wb[:, 0, kc, :], hT[:, kc, s0:s0 + sl], [f"ff_wb{i}", "hT"], [pa], start=(kc == 0), stop=(kc == 7))
                            for kc in range(8):
                                self.mm(t[pb_][:, 0:sl], wb[:, 1, kc, :], hT[:, kc, s0:s0 + sl], [f"ff_wb{i}", "hT"], [pb_], start=(kc == 0), stop=(kc == 7))
                            sg = psb(f"ff_s{bi_ % 2}", [128, 512])
                            sgn = f"ff_s{bi_ % 2}"
                            self.act(sg[:, 0:sl], t[pa][:, 0:sl], AF.Silu, [pa], [sgn])
                            self.tt("dve", ag[:, fi, s0:s0 + sl], sg[:, 0:sl], t[pb_][:, 0:sl], ALU.mult, [sgn, pb_], ["ff_a"])
                    self.load_cast(ph, w2g, "ff_w2", w2v[:, grp * GF:(grp + 1) * GF, :], GF, D, 1)
                    for n, ti in enumerate(tiles):
                        v = 1 if (not moe and self.is_ctx(ti)) else 0
                        xn = f"ff_x{n % 2}"
                        x = psb(xn, [128, D])
                        self.dma("sp", x[:], xs[ti * 128:(ti + 1) * 128, :], [f"{xtag}@{ti}"], [xn])
                        xt_ = psb("ff_xt", [128, D])
                        for half in range(2):
                            pn = f"P{4 + (n % 2) * 2 + half}"
                            for fi in range(GF):
                                self.mm(t[pn][:], ag[:, fi, ti * 128:(ti + 1) * 128], w2g[:, fi, half * 512:(half + 1) * 512], ["ff_a", "ff_w2"], [pn],
                                        start=(fi == 0), stop=(fi == GF - 1))
                            g2 = t[f"ff_g2{v}"][:, half * 512:(half + 1) * 512]
                            if moe:
                                self.stt(xt_[:, half * 512:(half + 1) * 512], t[pn][:], cmbt[:, ti, e:e + 1], g2, ALU.mult, ALU.mult,
                                         [pn, f"ff_g2{v}", "ff_cmbt"], ["ff_xt"])
                            else:
                                self.tt("dve", xt_[:, half * 512:(half + 1) * 512], t[pn][:], g2, ALU.mult, [pn, f"ff_g2{v}"], ["ff_xt"])
                        self.tt("dve", x[:], x[:], xt_[:], ALU.add, [xn, "ff_xt"], [xn])
                        self.dma("pool", xs[ti * 128:(ti + 1) * 128, :], x[:], [xn], [f"{xtag}@{ti}"])

    def final_phase(self):
        S, t = self.S, self.t
        with self.phase() as ph:
            psb = lambda name, shape, dt=F32: (self.sb(ph, name, shape, dt) if name not in t else t[name])
            g = psb("fn_g", [128, D])
            self.dma("sp", g[:], pbc(self.w["final_norm_g"].rearrange("(o d) -> o d", o=1)), (), ["fn_g"])
            if "XL" not in self.scr:
                self.dscr("XL", [self.NLOC * 128, D])
                idxt = psb("fn_idx", [128, self.NLOC], I32)
                self.dma("sp", idxt[:], self.idx_in, (), ["fn_idx"])
                for lt in range(self.NLOC):
                    g_ = psb(f"fn_g{lt % 2}", [128, D])
                    self.S.dma_gather(g_[:], self.scr["X"], idxt[:, lt:lt + 1], ["fn_idx"] + [f"X@{k}" for k in range(self.NT)], [f"fn_g{lt % 2}"])
                    self.dma("sp", self.scr["XL"][lt * 128:(lt + 1) * 128, :], g_[:], [f"fn_g{lt % 2}"], [f"XL@{lt}"])
            for n, ti in enumerate(range(self.NLOC)):
                xn, jn, sn = f"fn_x{n % 2}", f"fn_j{n % 2}", f"fn_s{n % 2}"
                x, junk, ss = psb(xn, [128, D]), psb(jn, [128, D]), psb(sn, [128, 1])
                self.dma("sp", x[:], self.scr["XL"][ti * 128:(ti + 1) * 128, :], [f"XL@{ti}"], [xn])
                self.act(junk[:], x[:], AF.Square, [xn], [jn, sn], accum=ss[:])
                self.ts("dve", ss[:], ss[:], 1.0 / D, ALU.mult, [sn], [sn], s2=EPS, op1=ALU.add)
                self.act(ss[:], ss[:], AF.Sqrt, [sn], [sn])
                S.op("dve", lambda e, ss=ss: e.reciprocal(out=ss[:], in_=ss[:]), [sn], [sn])
                self.stt(junk[:], x[:], ss[:], g[:], ALU.mult, ALU.mult, [xn, sn, "fn_g"], [jn])
                self.dma("pool", self.out[ti * 128:(ti + 1) * 128, :], junk[:], [jn], ["out"])
            self.S.finish(["out"])

    def mixer(self, l, sub=None):
        with self.phase() as ph:
            self.norm_phase(ph, l, False, list(range(self.NT)))
            if sub == "norm":
                return
            self.proj_phase(ph, l)
        if sub == "proj":
            return
        self.rec_pass(l, 0)
        if sub == "rec0":
            return
        self.rec_pass(l, 1)
        if sub == "rec1":
            return
        self.merge_phase(l, last=(l == 1))

    def build_all(self, upto="all"):
        stages = ["setup", "mod0", "mix0", "ffn0", "mod1", "mix1", "ffn1"]
        self.setup()
        if upto != "setup":
            self.mod_phase(0)
            if upto != "mod0":
                self.mixer(0, upto[4:] if upto.startswith("sub0") else None)
                if upto != "mix0" and not upto.startswith("sub0"):
                    self.ffn_phase(0, moe=False)
                    if upto != "ffn0":
                        self.mod_phase(1)
                        self.mixer(1)
                        if upto != "mix1":
                            self.ffn_phase(1, moe=True)
        self.final_phase()
        self.st.close()
        return self.nc


_NC_CACHE = {}


def _build_full():
    if "nc" not in _NC_CACHE:
        k = KB4(nct=2, nlt=32)
        _NC_CACHE["nc"] = k.build_all("all")
    return _NC_CACHE["nc"]


def kernel(**inputs):
    x = np.asarray(inputs["x"], dtype=np.float32)
    c = np.asarray(inputs["c"], dtype=np.float32)
    ctx = np.asarray(inputs["ctx"], dtype=np.float32)
    c_ctx = np.asarray(inputs["c_ctx"], dtype=np.float32)
    nb = x.shape[0]
    shared = {}
    for name, shape in WSPECS:
        shared[name] = np.ascontiguousarray(np.asarray(inputs[name], dtype=np.float32)).reshape(shape)
    cctxcol = np.ascontiguousarray(c_ctx.reshape(8, 128).T)
    in_maps = []
    nloc = 16
    for core in range(8):
        b, half = core % nb, core // nb
        m = dict(shared)
        m["xin"] = np.ascontiguousarray(np.concatenate([ctx[b], x[b]], axis=0))
        m["ccol"] = np.ascontiguousarray(c[b].reshape(8, 128).T)
        m["cctxcol"] = cctxcol
        m["idx"] = (256 + half * nloc * 128 + np.arange(nloc)[None, :] * 128 + np.arange(128)[:, None]).astype(np.int32)
        in_maps.append(m)
    nc = _build_full()
    res = run_bass_kernel_spmd(nc, in_maps, core_ids=list(range(8)))
    out = np.stack([np.concatenate([np.asarray(res.results[b + nb * h]["out"], dtype=np.float32) for h in range(2)], axis=0)
                    for b in range(nb)], axis=0)
    return out
```

```python
import math
import numpy as np
from contextlib import ExitStack, contextmanager
import concourse.bass as bass
import concourse.mybir as mybir
from concourse.bass_utils import run_bass_kernel_spmd


def pbc(ap):
    b = ap.partition_broadcast(128)
    if len(b.shape) == 3 and b.shape[1] == 1:
        return b[:, 0, :]
    return b


F32 = mybir.dt.float32
BF16 = mybir.dt.bfloat16
I32 = mybir.dt.int32
AF = mybir.ActivationFunctionType
ALU = mybir.AluOpType
AX = mybir.AxisListType

ENGS = ("pe", "act", "dve", "pool", "sp")
EPOCH = 50000
NDMASEM = 4
DEPOCH = 2000


class Buf:
    __slots__ = ("name", "w", "r")

    def __init__(self, name):
        self.name = name
        self.w = None
        self.r = []


class Sched:
    def __init__(self, nc, stack):
        self.nc = nc
        self.stack = stack
        self.eng = {"pe": nc.tensor, "act": nc.scalar, "dve": nc.vector, "pool": nc.gpsimd, "sp": nc.sync}
        self.q = {e: [] for e in ENGS}
        self.cnt = {e: 0 for e in ENGS}
        self.sems = {e: [] for e in ENGS}
        self.dcnt = {e: 0 for e in ENGS}
        self.dsems = {e: None for e in ENGS}
        self.seen = {e: {} for e in ENGS}
        self.bufs = {}

    def buf(self, name):
        b = self.bufs.get(name)
        if b is None:
            b = self.bufs[name] = Buf(name)
        return b

    def _sem(self, name):
        return self.stack.enter_context(self.nc.semaphore(name))

    def _esem(self, e, epoch):
        while len(self.sems[e]) <= epoch:
            self.sems[e].append(self._sem(f"s_{e}_{len(self.sems[e])}"))
        return self.sems[e][epoch]

    def _dsem(self, e, n):
        if self.dsems[e] is None:
            self.dsems[e] = []
        setsz = NDMASEM * DEPOCH
        si, r = divmod(n, setsz)
        while len(self.dsems[e]) <= si:
            k = len(self.dsems[e])
            self.dsems[e].append([self._sem(f"d_{e}_{k}_{j}") for j in range(NDMASEM)])
        return self.dsems[e][si][r % NDMASEM], 16 * (r // NDMASEM + 1)

    def _wait(self, e, ev):
        if ev is None:
            return
        kind, src, n = ev
        if kind == "c":
            if src == e and e == "pe":
                return
            key = ("c", src)
            if self.seen[e].get(key, 0) >= n:
                return
            self.seen[e][key] = n
            ep, loc = divmod(n - 1, EPOCH)
            sem = self._esem(src, ep)
            val = loc + 1
        else:
            key = ("d", src, n % NDMASEM)
            if self.seen[e].get(key, -1) >= n:
                return
            self.seen[e][key] = n
            sem, val = self._dsem(src, n)
        self.q[e].append(lambda eng, sem=sem, val=val: eng.wait_ge(sem, val))

    def _deps(self, e, reads, writes):
        for b in reads:
            if b.w is not None:
                self._wait(e, b.w)
        for b in writes:
            if b.w is not None:
                self._wait(e, b.w)
            for ev in b.r:
                self._wait(e, ev)

    def _commit(self, ev, reads, writes):
        for b in reads:
            b.r.append(ev)
            if len(b.r) > 64:
                b.r = b.r[-64:] if False else b.r
        for b in writes:
            b.w = ev
            b.r = []

    def op(self, e, fn, reads=(), writes=()):
        reads = [self.buf(b) if isinstance(b, str) else b for b in reads]
        writes = [self.buf(b) if isinstance(b, str) else b for b in writes]
        self._deps(e, reads, writes)
        self.cnt[e] += 1
        n = self.cnt[e]
        ep, loc = divmod(n - 1, EPOCH)
        sem = self._esem(e, ep)
        import sys as _sys
        fr = _sys._getframe(1)
        tag = []
        while fr is not None and len(tag) < 4:
            tag.append(f"{fr.f_code.co_name}:{fr.f_lineno}")
            fr = fr.f_back

        def _emit(eng, fn=fn, sem=sem, tag=tag):
            try:
                fn(eng).then_inc(sem, 1)
            except Exception:
                print("EMIT FAILED at", tag, flush=True)
                raise
        self.q[e].append(_emit)
        self._commit(("c", e, n), reads, writes)

    def dma(self, e, out, in_, reads=(), writes=(), **kw):
        reads = [self.buf(b) if isinstance(b, str) else b for b in reads]
        writes = [self.buf(b) if isinstance(b, str) else b for b in writes]
        self._deps(e, reads, writes)
        n = self.dcnt[e]
        self.dcnt[e] += 1
        if n >= NDMASEM:
            self._wait(e, ("d", e, n - NDMASEM))
        sem, _v = self._dsem(e, n)
        self.q[e].append(lambda eng, out=out, in_=in_, sem=sem, kw=kw: eng.dma_start(out=out, in_=in_, **kw).then_inc(sem, 16))
        self._commit(("d", e, n), reads, writes)

    def dma_gather(self, out, in_, idx_ap, reads=(), writes=()):
        e = "pool"
        reads = [self.buf(b) if isinstance(b, str) else b for b in reads]
        writes = [self.buf(b) if isinstance(b, str) else b for b in writes]
        self._deps(e, reads, writes)
        n = self.dcnt[e]
        self.dcnt[e] += 1
        if n >= NDMASEM:
            self._wait(e, ("d", e, n - NDMASEM))
        sem, _v = self._dsem(e, n)
        self.q[e].append(lambda eng, out=out, in_=in_, idx_ap=idx_ap, sem=sem: eng.indirect_dma_start(
            out=out, out_offset=None, in_=in_, in_offset=bass.IndirectOffsetOnAxis(ap=idx_ap, axis=0)).then_inc(sem, 16))
        self._commit(("d", e, n), reads, writes)

    def finish(self, final_bufs):
        for b in final_bufs:
            b = self.buf(b) if isinstance(b, str) else b
            self._wait("sp", b.w)
        for e in ENGS:
            for n in range(max(0, self.dcnt[e] - NDMASEM), self.dcnt[e]):
                self._wait("sp", ("d", e, n))
        for e in ENGS:
            if e != "sp" and self.cnt[e] > 0:
                self._wait("sp", ("c", e, self.cnt[e]))

    def barrier(self):
        for e in ENGS:
            for f in ENGS:
                if f != e and self.cnt[f] > 0:
                    self._wait(e, ("c", f, self.cnt[f]))
            for f in ENGS:
                for n in range(max(0, self.dcnt[f] - NDMASEM), self.dcnt[f]):
                    self._wait(e, ("d", f, n))

    def emit(self):
        nc = self.nc
        if not any(self.q[e] for e in ENGS):
            return
        with nc.Block() as block:
            @block.tensor
            def _(eng):
                for f in self.q["pe"]:
                    f(eng)

            @block.scalar
            def _(eng):
                for f in self.q["act"]:
                    f(eng)

            @block.vector
            def _(eng):
                for f in self.q["dve"]:
                    f(eng)

            @block.gpsimd
            def _(eng):
                for f in self.q["pool"]:
                    f(eng)

            @block.sync
            def _(eng):
                for f in self.q["sp"]:
                    f(eng)
        self.q = {e: [] for e in ENGS}


D = 1024
FF = 3584
NE = 8
NIN = 7712
EPS = 1e-6
OFF = dict(hg_q=0, hg_ff=512, hg_fb=1024, hg_i=1536, hg_g=2048, gl_q=2560, gl_k=2816, gl_v=3072,
           gl_df=3584, gl_db=3600, gl_r=3616, s5_u=4128, ga=4640, gb=5664, gc=6688)
TWO_PI = 6.283185307179586
PI_SAFE = 3.1415925

WSPECS = [("norm1_g", [2, D]), ("norm2_g", [2, D]), ("ada_w", [2, D, 6 * D]), ("ada_b", [2, 6 * D]),
          ("w_in", [2, D, NIN]), ("hg_lb_logits", [2, 512]), ("hg_norm_g", [2, 512]),
          ("gla_gate_up", [2, 2, 16, 256]), ("gla_gate_b", [2, 2, 256]), ("gla_norm_g", [2, 512]),
          ("s5_a_re", [2, 2, 32, 64]), ("s5_a_im", [2, 2, 32, 64]), ("s5_log_step", [2, 2, 32]),
          ("s5_b_re", [2, 2, 32, 64, 16]), ("s5_b_im", [2, 2, 32, 64, 16]),
          ("s5_c_re", [2, 2, 32, 16, 64]), ("s5_c_im", [2, 2, 32, 16, 64]),
          ("s5_d", [2, 512]), ("s5_glu_w", [2, 512, 512]), ("branch_proj", [2, 3, 512, D]),
          ("w_out", [2, D, D]), ("ffn_w1", [1, D, FF]), ("ffn_w3", [1, D, FF]), ("ffn_w2", [1, FF, D]),
          ("router_w", [1, D, NE]), ("moe_w1", [1, NE, D, FF]), ("moe_w3", [1, NE, D, FF]),
          ("moe_w2", [1, NE, FF, D]), ("final_norm_g", [D])]


class KB:
    def __init__(self, nct=2, nlt=32, dumps=(), stop=None):
        self.nct, self.nlt = nct, nlt
        self.NT = nct + nlt
        self.T = self.NT * 128
        self.dumps = set(dumps)
        self.stop = stop
        self.nc = nc = bass.Bass("TRN2", target_bir_lowering=False)
        self.st = ExitStack()
        self.S = Sched(nc, self.st)
        self.t = {}
        din = lambda name, shape: nc.dram_tensor(name, list(shape), F32, kind="ExternalInput").ap()
        self.xin = din("xin", [self.T, D])
        self.ccol = din("ccol", [128, 8])
        self.cctxcol = din("cctxcol", [128, 8])
        self.w = {name: din(name, shape) for name, shape in WSPECS}
        self.NLOC = nlt // 2
        self.idx_in = nc.dram_tensor("idx", [128, self.NLOC], I32, kind="ExternalInput").ap()
        self.out = nc.dram_tensor("out", [self.NLOC * 128, D], F32, kind="ExternalOutput").ap()
        self.scr = {}

    def dscr(self, name, shape, dt=F32):
        kind = "ExternalOutput" if name in self.dumps else "Internal"
        ap = self.nc.dram_tensor(name, list(shape), dt, kind=kind).ap()
        self.scr[name] = ap
        return ap

    def sb(self, stack, name, shape, dt=F32):
        self._uid = getattr(self, "_uid", 0) + 1
        h = stack.enter_context(self.nc.sbuf_tensor(f"{name}_u{self._uid}", list(shape), dt))
        self.t[name] = h
        if hasattr(stack, "_names"):
            stack._names.append(name)
        return h

    @contextmanager
    def phase(self):
        with ExitStack() as ph:
            ph._names = []
            yield ph
            self.phase_end()
            for n in ph._names:
                self.t.pop(n, None)

    def mm(self, out, lhsT, rhs, r, w, start=True, stop=True):
        self.S.op("pe", lambda e: e.matmul(out, lhsT=lhsT, rhs=rhs, start=start, stop=stop), r, w)

    def tr(self, out, in_, r, w):
        ident = self.t["ident"]
        self.S.op("pe", lambda e: e.transpose(out, in_, ident[:]), list(r) + ["ident"], w)

    def act(self, out, in_, func, r, w, bias=None, scale=None, accum=None):
        kw = {}
        if bias is not None:
            kw["bias"] = bias
        if scale is not None:
            kw["scale"] = scale
        if accum is not None:
            kw["accum_out"] = accum
        self.S.op("act", lambda e: e.activation(out=out, in_=in_, func=func, **kw), r, w)

    def tt(self, eng, out, in0, in1, op, r, w):
        self.S.op(eng, lambda e: e.tensor_tensor(out=out, in0=in0, in1=in1, op=op), r, w)

    def ts(self, eng, out, in0, s1, op0, r, w, s2=None, op1=None):
        if op1 is None:
            self.S.op(eng, lambda e: e.tensor_scalar(out=out, in0=in0, scalar1=s1, scalar2=None, op0=op0), r, w)
        else:
            self.S.op(eng, lambda e: e.tensor_scalar(out=out, in0=in0, scalar1=s1, scalar2=s2, op0=op0, op1=op1), r, w)

    def stt(self, out, in0, scalar, in1, op0, op1, r, w, accum=None):
        if accum is None:
            self.S.op("dve", lambda e: e.scalar_tensor_tensor(out=out, in0=in0, scalar=scalar, in1=in1, op0=op0, op1=op1), r, w)
        else:
            self.S.op("dve", lambda e: e.scalar_tensor_tensor(out=out, in0=in0, scalar=scalar, in1=in1, op0=op0, op1=op1, accum_out=accum), r, w)

    def cp(self, eng, out, in_, r, w):
        if eng == "act":
            self.S.op("act", lambda e: e.activation(out=out, in_=in_, func=AF.Identity), r, w)
        else:
            self.S.op(eng, lambda e: e.tensor_copy(out=out, in_=in_), r, w)

    def memset(self, eng, ap, val, w):
        self.S.op(eng, lambda e: e.memset(ap, val), (), w)

    def dma(self, q, out, in_, r, w, **kw):
        self.S.dma(q, out, in_, r, w, **kw)

    def phase_end(self):
        self.S.barrier()
        self.S.emit()

    def is_ctx(self, ti):
        return ti < self.nct

    def setup(self):
        S, t, st = self.S, self.t, self.st
        sb = lambda name, shape, dt=F32: self.sb(st, name, shape, dt)
        for i in range(8):
            h = st.enter_context(self.nc.psum_tensor(f"P{i}", [128, 512], F32))
            t[f"P{i}"] = h
        self.dscr("X", [self.T, D])
        di = sb("c_di", [128, 128], I32)
        df = sb("c_df", [128, 128])
        S.op("pool", lambda e: e.iota(di[:], pattern=[[1, 128]], base=0, channel_multiplier=-1), (), ["c_di"])
        self.cp("dve", df[:], di[:], ["c_di"], ["c_df"])
        for name, op in (("ident", ALU.is_equal), ("minc0", ALU.is_ge), ("minc1", ALU.is_le),
                         ("mstr0", ALU.is_lt), ("mstr1", ALU.is_gt)):
            h = sb(name, [128, 128])
            S.op("dve", lambda e, h=h, op=op: e.tensor_single_scalar(out=h[:], in_=df[:], scalar=0.0, op=op), ["c_df"], [name])
        for d in (0, 1):
            h = sb(f"mincb{d}", [128, 128], BF16)
            self.cp("dve", h[:], t[f"minc{d}"][:], [f"minc{d}"], [f"mincb{d}"])
        for hh in (0, 1):
            hm = sb(f"hm{hh}", [128, 1])
            self.memset("pool", hm[:], 0.0, [f"hm{hh}"])
            self.memset("pool", hm[hh * 64:(hh + 1) * 64, :], 1.0, [f"hm{hh}"])
        ones = sb("ones", [128, 128])
        self.memset("pool", ones[:], 1.0, ["ones"])
        for k in range(4):
            m = sb(f"mask{k}", [128, 128])
            self.memset("pool", m[:], 0.0, [f"mask{k}"])
            self.memset("pool", m[0:64, (2 * k) * 16:(2 * k + 1) * 16], 1.0, [f"mask{k}"])
            self.memset("pool", m[64:128, (2 * k + 1) * 16:(2 * k + 2) * 16], 1.0, [f"mask{k}"])
        lg = self.w["hg_lb_logits"]
        for nm, shape in (("fm", [128, 4]), ("tm", [128, 512])):
            for l in (0, 1):
                sb(f"lb_{nm}{l}", shape)
                sb(f"oml_{nm}{l}", shape)
        with self.phase() as ph0:
            lfm = self.sb(ph0, "lb_lfm", [128, 2, 4])
            self.dma("sp", lfm[:], lg.rearrange("l (h p) -> p l h", p=128), (), ["lb_lfm"], allow_slow_non_contiguous=True)
            ltm = self.sb(ph0, "lb_ltm", [128, 2, 512])
            self.dma("sp", ltm[:], lg.partition_broadcast(128), (), ["lb_ltm"])
            for nm, shape, src in (("fm", [128, 4], lfm), ("tm", [128, 512], ltm)):
                self.memset("pool", t[f"lb_{nm}0"][:], 0.0, [f"lb_{nm}0"])
                self.memset("pool", t[f"oml_{nm}0"][:], 1.0, [f"oml_{nm}0"])
                dtile = self.sb(ph0, f"lb_d{nm}", shape)
                self.tt("dve", dtile[:], src[:, 1, :], src[:, 0, :], ALU.subtract, [f"lb_l{nm}"], [f"lb_d{nm}"])
                self.act(t[f"lb_{nm}1"][:], dtile[:], AF.Sigmoid, [f"lb_d{nm}"], [f"lb_{nm}1"])
                self.act(t[f"oml_{nm}1"][:], dtile[:], AF.Sigmoid, [f"lb_d{nm}"], [f"oml_{nm}1"], scale=-1.0)
        with self.phase() as ph:
            psb = lambda name, shape, dt=F32: self.sb(ph, name, shape, dt)
            ji = psb("pe_ji", [128, 256], I32)
            om2 = psb("pe_om2", [128, 512])
            ph2 = psb("pe_ph", [128, 512])
            S.op("pool", lambda e: e.iota(ji[:], pattern=[[1, 256]], base=0, channel_multiplier=0), (), ["pe_ji"])
            self.cp("dve", om2[:, 0:256], ji[:], ["pe_ji"], ["pe_om2"])
            self.act(om2[:, 0:256], om2[:, 0:256], AF.Exp, ["pe_om2"], ["pe_om2"], scale=-math.log(10000.0) / 256.0)
            self.cp("dve", om2[:, 256:512], om2[:, 0:256], ["pe_om2"], ["pe_om2"])
            self.memset("pool", ph2[:, 0:256], 0.0, ["pe_ph"])
            self.memset("pool", ph2[:, 256:512], math.pi / 2, ["pe_ph"])
            pi_ = psb("pe_pi", [128, 1], I32)
            pf = psb("pe_pf", [128, 1])
            hi = psb("pe_hi", [128, 1])
            cpos = psb("pe_c", [128, 1])
            S.op("pool", lambda e: e.iota(pi_[:], pattern=[[0, 1]], base=0, channel_multiplier=1), (), ["pe_pi"])
            self.cp("dve", pf[:], pi_[:], ["pe_pi"], ["pe_pf"])
            self.ts("dve", hi[:], pf[:], 64.0, ALU.is_ge, ["pe_pf"], ["pe_hi"])
            self.stt(cpos[:], hi[:], -64.0, pf[:], ALU.mult, ALU.add, ["pe_hi", "pe_pf"], ["pe_c"])

            def sincos(dst, scal, tag):
                a = t["pe_a"]
                b = t["pe_b"]
                bi = t["pe_bi"]
                self.stt(a[:], om2[:], scal, ph2[:], ALU.mult, ALU.add, ["pe_om2", "pe_ph", tag], ["pe_a"])
                self.ts("dve", b[:], a[:], 1.0 / TWO_PI, ALU.mult, ["pe_a"], ["pe_b"])
                self.cp("dve", bi[:], b[:], ["pe_b"], ["pe_bi"])
                self.cp("dve", b[:], bi[:], ["pe_bi"], ["pe_b"])
                self.stt(a[:], b[:], -TWO_PI, a[:], ALU.mult, ALU.add, ["pe_b", "pe_a"], ["pe_a"])
                self.ts("dve", a[:], a[:], PI_SAFE, ALU.min, ["pe_a"], ["pe_a"], s2=-PI_SAFE, op1=ALU.max)
                self.act(dst, a[:], AF.Sin, ["pe_a"], [tag + "_o"])

            psb("pe_a", [128, 512])
            psb("pe_b", [128, 512])
            psb("pe_bi", [128, 512], I32)
            colp = psb("pe_col", [128, 512])
            S.buf("pe_c_o")
            sincos(colp[:], cpos[:], "pe_c")
            rsc = psb("pe_r", [128, 1])
            for ti in range(self.NT):
                xt = psb(f"pe_x{ti % 2}", [128, D]) if ti < 2 else t[f"pe_x{ti % 2}"]
                nm = f"pe_x{ti % 2}"
                self.dma("sp", xt[:], self.xin[ti * 128:(ti + 1) * 128, :], (), [nm])
                if not self.is_ctx(ti):
                    li = ti - self.nct
                    rowp = psb(f"pe_row{ti % 2}", [128, 512]) if f"pe_row{ti % 2}" not in t else t[f"pe_row{ti % 2}"]
                    self.ts("dve", rsc[:], hi[:], float(2 * li), ALU.add, ["pe_hi"], ["pe_r"])
                    S.buf("pe_r_o")
                    sincos(rowp[:], rsc[:], "pe_r")
                    self.tt("dve", xt[:, 0:512], xt[:, 0:512], rowp[:], ALU.add, [nm, "pe_r_o"], [nm])
                    self.tt("pool", xt[:, 512:1024], xt[:, 512:1024], colp[:], ALU.add, [nm, "pe_c_o"], [nm])
                self.dma("pool", self.scr["X"][ti * 128:(ti + 1) * 128, :], xt[:], [nm], [f"X@{ti}"])

    def mod_phase(self, l):
        S, t = self.S, self.t
        if "MODS" not in self.scr:
            self.dscr("MODS", [2, 2, 6 * D])
        with self.phase() as ph:
            psb = lambda name, shape, dt=F32: self.sb(ph, name, shape, dt)
            cb = {}
            for v, src in ((0, self.ccol), (1, self.cctxcol)):
                c = psb(f"md_c{v}", [128, 8])
                self.dma("sp", c[:], src, (), [f"md_c{v}"])
                self.act(c[:], c[:], AF.Silu, [f"md_c{v}"], [f"md_c{v}"])
                cbt = psb(f"md_cb{v}", [128, 8, 128])
                self.cp("dve", cbt[:], c[:].unsqueeze(2).to_broadcast([128, 8, 128]), [f"md_c{v}"], [f"md_cb{v}"])
                cb[v] = cbt
            bb = psb("md_b", [128, 6 * D])
            self.dma("sp", bb[:], pbc(self.w["ada_b"][l:l + 1, :]), (), ["md_b"])
            wsrc = self.w["ada_w"][l].rearrange("(kc p) n -> p kc n", p=128)
            for cbk in range(12):
                wn = f"md_w{cbk % 2}"
                wt = psb(wn, [128, 8, 512]) if wn not in t else t[wn]
                self.dma("sp", wt[:], wsrc[:, :, cbk * 512:(cbk + 1) * 512], (), [wn])
                for v in (0, 1):
                    pn = f"P{v}"
                    for kc in range(8):
                        self.mm(t[pn][:], cb[v][:, kc, :], wt[:, kc, :], [f"md_cb{v}", wn], [pn], start=(kc == 0), stop=(kc == 7))
                    on = f"md_o{v}"
                    ot = psb(on, [128, 512]) if on not in t else t[on]
                    self.tt("dve", ot[:], t[pn][:], bb[:, cbk * 512:(cbk + 1) * 512], ALU.add, [pn, "md_b"], [on])
                    self.dma("pool", self.scr["MODS"][l, v:v + 1, cbk * 512:(cbk + 1) * 512], ot[0:1, :], [on], [f"MODS@{l}"])

    def load_mod(self, stack, l, which, tag):
        for v in (0, 1):
            h = self.sb(stack, f"{tag}{v}", [128, D])
            self.dma("sp", h[:], pbc(self.scr["MODS"][l, v:v + 1, which * D:(which + 1) * D]),
                     [f"MODS@{l}"], [f"{tag}{v}"])

    def norm_phase(self, stack, l, second, tiles, router=False, local=None):
        hT = self.sb(stack, "hT", [128, 8, self.T if local is None else self.NLOC * 128], BF16)
        with self.phase() as inner:
            self._norm_phase(inner, l, second, tiles, router, hT, local)
        return hT

    def _norm_phase(self, stack, l, second, tiles, router, hT, local=None):
        S, t = self.S, self.t
        psb = lambda name, shape, dt=F32: self.sb(stack, name, shape, dt)
        pre = "n2" if second else "n1"
        self.load_mod(stack, l, 4 if second else 1, pre + "sc")
        self.load_mod(stack, l, 3 if second else 0, pre + "sh")
        gn = psb(pre + "g", [128, D])
        gsrc = self.w["norm2_g" if second else "norm1_g"]
        self.dma("sp", gn[:], pbc(gsrc[l:l + 1, :]), (), [pre + "g"])
        for v in (0, 1):
            self.stt(t[f"{pre}sc{v}"][:], t[f"{pre}sc{v}"][:], 1.0, gn[:], ALU.add, ALU.mult, [f"{pre}sc{v}", pre + "g"], [f"{pre}sc{v}"])
        _sk = ""
        if router:
            wr = psb("rt_w", [128, 8, 128])
            self.memset("pool", wr[:], 0.0, ["rt_w"])
            if "noW" not in _sk:
                self.dma("sp", wr[:, :, 0:NE], self.w["router_w"][0].rearrange("(kc p) e -> p kc e", p=128), (), ["rt_w"])
            pass
        if local is not None:
            idxt = psb("n_idx", [128, self.NLOC], I32)
            self.dma("sp", idxt[:], self.idx_in, (), ["n_idx"])
        for ti in tiles:
            v = 1 if (local is None and self.is_ctx(ti)) else 0
            xn = f"nx{ti % 2}"
            xt = psb(xn, [128, D]) if xn not in t else t[xn]
            if local is None:
                self.dma("sp", xt[:], self.scr["X"][ti * 128:(ti + 1) * 128, :], [f"X@{ti}"], [xn])
            else:
                self.S.dma_gather(xt[:], self.scr["X"], idxt[:, ti:ti + 1], ["n_idx"] + [f"X@{k}" for k in range(self.NT)], [xn])
                self.dma("sp", self.scr["XL"][ti * 128:(ti + 1) * 128, :], xt[:], [xn], [f"XL@{ti}"])
            jn = f"njunk{ti % 2}"
            junk = psb(jn, [128, D]) if jn not in t else t[jn]
            sn = f"nss{ti % 2}"
            ss = psb(sn, [128, 1]) if sn not in t else t[sn]
            self.act(junk[:], xt[:], AF.Square, [xn], [jn, sn], accum=ss[:])
            self.ts("dve", ss[:], ss[:], 1.0 / D, ALU.mult, [sn], [sn], s2=EPS, op1=ALU.add)
            self.act(ss[:], ss[:], AF.Sqrt, [sn], [sn])
            S.op("dve", lambda e, ss=ss: e.reciprocal(out=ss[:], in_=ss[:]), [sn], [sn])
            self.stt(junk[:], xt[:], ss[:], t[f"{pre}sc{v}"][:], ALU.mult, ALU.mult, [xn, sn, f"{pre}sc{v}"], [jn])
            self.tt("pool", junk[:], junk[:], t[f"{pre}sh{v}"][:], ALU.add, [jn, f"{pre}sh{v}"], [jn])
            for half in range(2):
                pn = f"P{half}"
                for k4 in range(4):
                    kc = half * 4 + k4
                    self.tr(t[pn][:, k4 * 128:(k4 + 1) * 128], junk[:, kc * 128:(kc + 1) * 128], [jn], [pn])
                eng = "act" if half == 0 else "dve"
                if router:
                    hn = f"rt_h{half}"
                    hf = psb(hn, [128, 4, 128]) if hn not in t else t[hn]
                    self.cp(eng, hf[:], t[pn][:].rearrange("p (k c) -> p k c", k=4), [pn], [hn])
                    self.cp("pool", hT[:, half * 4:(half + 1) * 4, ti * 128:(ti + 1) * 128], hf[:], [hn], ["hT"])
                else:
                    self.cp(eng, hT[:, half * 4:(half + 1) * 4, ti * 128:(ti + 1) * 128],
                            t[pn][:].rearrange("p (k c) -> p k c", k=4), [pn], ["hT"])
            if router and "rtA" in "":
                continue
            if router:
                for kc in range(8):
                    self.mm(t["P2"][:, 0:128], t[f"rt_h{kc // 4}"][:, kc % 4, :], t["rt_w"][:, kc, :],
                            [f"rt_h{kc // 4}", "rt_w"], ["P2"], start=(kc == 0), stop=(kc == 7))
                self.router_post(stack, ti, local)
        return hT

    def router_post(self, stack, ti, cmbt):
        S, t = self.S, self.t
        import os as _os
        _sk = ""
        psb = lambda name, shape, dt=F32: (self.sb(stack, name, shape, dt) if name not in t else t[name])
        lgt = psb("rt_lg", [128, NE])
        m1 = psb("rt_m1", [128, 1])
        m2 = psb("rt_m2", [128, 1])
        eq = psb("rt_eq", [128, NE])
        ex = psb("rt_ex", [128, NE])
        den = psb("rt_den", [128, 1])
        self.cp("dve", lgt[:], t["P2"][:, 0:NE], ["P2"], ["rt_lg"])
        if "rt1" in _sk:
            return
        S.op("dve", lambda e: e.reduce_max(out=m1[:], in_=lgt[:], axis=AX.X), ["rt_lg"], ["rt_m1"])
        self.ts("dve", eq[:], lgt[:], m1[:], ALU.is_equal, ["rt_lg", "rt_m1"], ["rt_eq"])
        self.stt(eq[:], eq[:], -1e30, lgt[:], ALU.mult, ALU.add, ["rt_eq", "rt_lg"], ["rt_eq"])
        S.op("dve", lambda e: e.reduce_max(out=m2[:], in_=eq[:], axis=AX.X), ["rt_eq"], ["rt_m2"])
        self.ts("dve", eq[:], lgt[:], m2[:], ALU.is_ge, ["rt_lg", "rt_m2"], ["rt_eq"])
        self.ts("dve", m1[:], m1[:], -1.0, ALU.mult, ["rt_m1"], ["rt_m1"])
        self.act(ex[:], lgt[:], AF.Exp, ["rt_lg", "rt_m1"], ["rt_ex"], bias=m1[:])
        self.tt("dve", ex[:], ex[:], eq[:], ALU.mult, ["rt_ex", "rt_eq"], ["rt_ex"])
        S.op("dve", lambda e: e.reduce_sum(out=den[:], in_=ex[:], axis=AX.X), ["rt_ex"], ["rt_den"])
        S.op("dve", lambda e: e.reciprocal(out=den[:], in_=den[:]), ["rt_den"], ["rt_den"])
        self.ts("dve", cmbt[:, ti, :], ex[:], den[:], ALU.mult, ["rt_ex", "rt_den"], ["ff_cmbt"])


class KB2(KB):
    def proj_phase(self, stack, l):
        S, t = self.S, self.t
        T, NT = self.T, self.NT
        psb = lambda name, shape, dt=F32: (self.sb(stack, name, shape, dt) if name not in t else t[name])
        hT = t["hT"]
        for name, rows in (("QH", 512), ("KHF", 512), ("KHB", 512), ("GH", 512), ("QG", 256), ("KG", 256),
                           ("RG", 512), ("U5", 512), ("GA", D), ("GB", D), ("GC", D)):
            if name not in self.scr:
                self.dscr(name, [rows, T])
        for name, cols, dt in (("GHF", 512, F32), ("GHB", 512, F32), ("KHFt", 512, F32), ("KHBt", 512, F32),
                               ("VH", 512, BF16), ("KGt", 256, F32), ("VG", 512, BF16), ("GG", 512, F32)):
            if name not in self.scr:
                self.dscr(name, [T, cols], dt)
        wsrc = self.w["w_in"][l].rearrange("(kc p) n -> p kc n", p=128)
        tbs = [(s, min(512, T - s)) for s in range(0, T, 512)]
        self._wcnt = getattr(self, "_wcnt", 0)

        def load_w(c0, ncol):
            i = self._wcnt % 2
            self._wcnt += 1
            wf = psb(f"pw_f{i}", [128, 8, 512])
            wb = psb(f"pw_b{i}", [128, 8, 512], BF16)
            self.dma("sp", wf[:, :, 0:ncol], wsrc[:, :, c0:c0 + ncol], (), [f"pw_f{i}"])
            self.cp("pool", wb[:, :, 0:ncol], wf[:, :, 0:ncol], [f"pw_f{i}"], [f"pw_b{i}"])
            return wb, f"pw_b{i}"

        self._pcnt = getattr(self, "_pcnt", 0)

        def next_p():
            i = self._pcnt % 4
            self._pcnt += 1
            return t[f"P{i}"], f"P{i}"

        self._ocnt = getattr(self, "_ocnt", 0)

        def next_o(dt=F32):
            i = self._ocnt % 3
            self._ocnt += 1
            nm = f"po_{'b' if dt == BF16 else 'f'}{i}"
            return psb(nm, [128, 512], dt), nm

        lb, oml = t[f"lb_fm{l}"], t[f"oml_fm{l}"]
        lbt, omlt = t[f"lb_tm{l}"], t[f"oml_tm{l}"]

        fm = [(OFF["hg_q"], 512, "QH", "copy"), (OFF["hg_ff"], 512, "KHF", "hk"), (OFF["hg_fb"], 512, "KHB", "hk"),
              (OFF["hg_g"], 512, "GH", "silu"), (OFF["gl_q"], 256, "QG", "q8"), (OFF["gl_k"], 256, "KG", "copy"),
              (OFF["gl_r"], 512, "RG", "silu"), (OFF["s5_u"], 512, "U5", "copy"),
              (OFF["ga"], D, "GA", "sig"), (OFF["gb"], D, "GB", "sig"), (OFF["gc"], D, "GC", "sig")]
        for c0, ncols, sname, kind in fm:
            for cb0 in range(0, ncols, 512):
                nb = min(512, ncols - cb0)
                wb, wn = load_w(c0 + cb0, nb)
                for sub in range(nb // 128):
                    row0 = cb0 + sub * 128
                    for (s0, sl) in tbs:
                        P, pn = next_p()
                        for kc in range(8):
                            self.mm(P[:, 0:sl], wb[:, kc, sub * 128:(sub + 1) * 128], hT[:, kc, s0:s0 + sl],
                                    [wn, "hT"], [pn], start=(kc == 0), stop=(kc == 7))
                        o, on = next_o()
                        if kind == "copy":
                            self.cp("act", o[:, 0:sl], P[:, 0:sl], [pn], [on])
                        elif kind == "q8":
                            self.act(o[:, 0:sl], P[:, 0:sl], AF.Identity, [pn], [on], scale=0.125)
                        elif kind == "silu":
                            self.act(o[:, 0:sl], P[:, 0:sl], AF.Silu, [pn], [on])
                        elif kind == "sig":
                            self.act(o[:, 0:sl], P[:, 0:sl], AF.Sigmoid, [pn], [on])
                        elif kind == "hk":
                            h = row0 // 128
                            self.act(o[:, 0:sl], P[:, 0:sl], AF.Sigmoid, [pn], [on], scale=-1.0)
                            self.ts("dve", o[:, 0:sl], o[:, 0:sl], oml[:, h:h + 1], ALU.mult, [on, f"oml_fm{l}"], [on])
                        self.dma("sp", self.scr[sname][row0:row0 + 128, s0:s0 + sl], o[:, 0:sl], [on],
                                 [f"{sname}@{s0 // 128 + k}" for k in range(sl // 128)])
        tm = [(OFF["hg_ff"], 512, "hgf", ("GHF", "KHFt")), (OFF["hg_fb"], 512, "hgf", ("GHB", "KHBt")),
              (OFF["hg_i"], 512, "vbf", ("VH",)), (OFF["gl_k"], 256, "copy", ("KGt",)), (OFF["gl_v"], 512, "vbf", ("VG",))]
        for c0, ncols, kind, snames in tm:
            wb, wn = load_w(c0, ncols)
            for ti in range(NT):
                P, pn = next_p()
                for kc in range(8):
                    self.mm(P[:, 0:ncols], hT[:, kc, ti * 128:(ti + 1) * 128], wb[:, kc, 0:ncols], [wn, "hT"], [pn],
                            start=(kc == 0), stop=(kc == 7))
                rows = slice(ti * 128, (ti + 1) * 128)
                if kind == "copy":
                    o, on = next_o()
                    self.cp("act", o[:, 0:ncols], P[:, 0:ncols], [pn], [on])
                    self.dma("sp", self.scr[snames[0]][rows, :], o[:, 0:ncols], [on], [f"{snames[0]}@{ti}"])
                elif kind == "vbf":
                    o, on = next_o(BF16)
                    self.cp("act", o[:, 0:ncols], P[:, 0:ncols], [pn], [on])
                    self.dma("sp", self.scr[snames[0]][rows, :], o[:, 0:ncols], [on], [f"{snames[0]}@{ti}"])
                else:
                    o, on = next_o()
                    o2, on2 = next_o()
                    self.act(o[:], P[:], AF.Sigmoid, [pn], [on])
                    self.tt("dve", o[:], o[:], omlt[:], ALU.mult, [on, f"oml_tm{l}"], [on])
                    self.tt("dve", o[:], o[:], lbt[:], ALU.add, [on, f"lb_tm{l}"], [on])
                    self.act(o[:], o[:], AF.Ln, [on], [on])
                    self.dma("sp", self.scr[snames[0]][rows, :], o[:], [on], [f"{snames[0]}@{ti}"])
                    self.act(o2[:], P[:], AF.Sigmoid, [pn], [on2], scale=-1.0)
                    self.tt("dve", o2[:], o2[:], omlt[:], ALU.mult, [on2, f"oml_tm{l}"], [on2])
                    self.dma("sp", self.scr[snames[1]][rows, :], o2[:], [on2], [f"{snames[1]}@{ti}"])
        upf = psb("pg_upf", [33, 512])
        upb = psb("pg_upb", [33, 512], BF16)
        self.memset("pool", upf[:], 0.0, ["pg_upf"])
        self.dma("sp", upf[0:16, 0:256], self.w["gla_gate_up"][l, 0], (), ["pg_upf"])
        self.dma("sp", upf[16:32, 256:512], self.w["gla_gate_up"][l, 1], (), ["pg_upf"])
        self.dma("sp", upf[32:33, :], self.w["gla_gate_b"][l:l + 1].rearrange("o d k -> o (d k)"), (), ["pg_upf"])
        self.cp("dve", upb[:], upf[:], ["pg_upf"], ["pg_upb"])
        wb, wn = load_w(OFF["gl_df"], 32)
        for (s0, sl) in tbs:
            P, pn = next_p()
            for kc in range(8):
                self.mm(P[0:32, 0:sl], wb[:, kc, 0:32], hT[:, kc, s0:s0 + sl], [wn, "hT"], [pn], start=(kc == 0), stop=(kc == 7))
            i = (s0 // 512) % 2
            pdt = psb(f"pg_pd{i}", [33, 512], BF16)
            if s0 < 1024:
                self.memset("pool", pdt[32:33, :], 1.0, [f"pg_pd{i}"])
            self.cp("act", pdt[0:32, 0:sl], P[0:32, 0:sl], [pn], [f"pg_pd{i}"])
            for k in range(sl // 128):
                ti = s0 // 128 + k
                P2, pn2 = next_p()
                self.mm(P2[:], pdt[0:33, k * 128:(k + 1) * 128], upb[0:33, :], [f"pg_pd{i}", "pg_upb"], [pn2])
                o, on = next_o()
                self.act(o[:], P2[:], AF.Sigmoid, [pn2], [on])
                self.act(o[:], o[:], AF.Ln, [on], [on])
                self.ts("dve", o[:], o[:], 1.0 / 16.0, ALU.mult, [on], [on])
                self.dma("sp", self.scr["GG"][ti * 128:(ti + 1) * 128, :], o[:], [on], [f"GG@{ti}"])

    def s5_setup(self, stack, l, d):
        S, t = self.S, self.t
        psb = lambda name, shape, dt=F32: (self.sb(stack, name, shape, dt) if name not in t else t[name])
        W = psb("s5_W", [128, 16, 2, 128])
        V = psb("s5_V", [128, 16, 2, 128])
        BT = psb("s5_BT", [128, 16, 256], BF16)
        CT = psb("s5_CT", [128, 16, 2, 128], BF16)
        with self.phase() as tmp:
            tsb = lambda name, shape, dt=F32: self.sb(tmp, name, shape, dt)
            lre = tsb("s5t_lre", [128, 16])
            lim = tsb("s5t_lim", [128, 16])
            lst = tsb("s5t_lst", [128, 16])
            self.dma("sp", lre[:], self.w["s5_a_re"][l, d].rearrange("g n -> (g n)").rearrange("(j p) -> p j", p=128), (), ["s5t_lre"], allow_slow_non_contiguous=True)
            self.dma("sp", lim[:], self.w["s5_a_im"][l, d].rearrange("g n -> (g n)").rearrange("(j p) -> p j", p=128), (), ["s5t_lim"], allow_slow_non_contiguous=True)
            ls = self.w["s5_log_step"]
            base = ls[l, d, 0:1]
            for g2 in (0, 1):
                src = bass.AP(ls.tensor, base.offset + g2, [[0, 64], [2, 16]])
                self.dma("sp", lst[g2 * 64:(g2 + 1) * 64, :], src, (), ["s5t_lst"], allow_slow_non_contiguous=True)
            self.act(lst[:], lst[:], AF.Exp, ["s5t_lst"], ["s5t_lst"])
            lr = tsb("s5t_lr", [128, 16])
            li = tsb("s5t_li", [128, 16])
            self.tt("dve", lr[:], lre[:], lst[:], ALU.mult, ["s5t_lre", "s5t_lst"], ["s5t_lr"])
            self.tt("dve", li[:], lim[:], lst[:], ALU.mult, ["s5t_lim", "s5t_lst"], ["s5t_li"])
            ti_ = tsb("s5t_ti", [128, 128], I32)
            tau = tsb("s5t_tau", [128, 128])
            if d == 0:
                S.op("pool", lambda e: e.iota(ti_[:], pattern=[[1, 128]], base=1, channel_multiplier=0), (), ["s5t_ti"])
            else:
                S.op("pool", lambda e: e.iota(ti_[:], pattern=[[-1, 128]], base=128, channel_multiplier=0), (), ["s5t_ti"])
            self.cp("dve", tau[:], ti_[:], ["s5t_ti"], ["s5t_tau"])
            mag = tsb("s5t_mag", [128, 16, 128])
            ang = tsb("s5t_ang", [128, 16, 128])
            a2 = tsb("s5t_a2", [128, 16, 128])
            bi = tsb("s5t_bi", [128, 16, 128], I32)
            sn = tsb("s5t_sin", [128, 16, 128])
            cs = tsb("s5t_cos", [128, 16, 128])
            for j in range(16):
                self.act(mag[:, j, :], tau[:], AF.Exp, ["s5t_tau", "s5t_lr"], ["s5t_mag"], scale=lr[:, j:j + 1])
                self.ts("dve", ang[:, j, :], tau[:], li[:, j:j + 1], ALU.mult, ["s5t_tau", "s5t_li"], ["s5t_ang"])
            for dst, phase in ((sn, 0.0), (cs, math.pi / 2)):
                dn = "s5t_sin" if phase == 0.0 else "s5t_cos"
                self.ts("dve", a2[:], ang[:], phase, ALU.add, ["s5t_ang"], ["s5t_a2"])
                self.ts("dve", dst[:], a2[:], 1.0 / TWO_PI, ALU.mult, ["s5t_a2"], [dn])
                self.cp("dve", bi[:], dst[:], [dn], ["s5t_bi"])
                self.cp("dve", dst[:], bi[:], ["s5t_bi"], [dn])
                self.stt(a2[:], dst[:], -TWO_PI, a2[:], ALU.mult, ALU.add, [dn, "s5t_a2"], ["s5t_a2"])
                self.ts("dve", a2[:], a2[:], PI_SAFE, ALU.min, ["s5t_a2"], ["s5t_a2"], s2=-PI_SAFE, op1=ALU.max)
                self.act(dst[:], a2[:], AF.Sin, ["s5t_a2"], [dn])
            self.tt("dve", V[:, :, 0, :], mag[:], cs[:], ALU.mult, ["s5t_mag", "s5t_cos"], ["s5_V"])
            self.tt("dve", V[:, :, 1, :], mag[:], sn[:], ALU.mult, ["s5t_mag", "s5t_sin"], ["s5_V"])
            S.op("dve", lambda e: e.reciprocal(out=mag[:], in_=mag[:]), ["s5t_mag"], ["s5t_mag"])
            wfm = tsb("s5t_wfm", [128, 16, 2, 128])
            self.tt("dve", wfm[:, :, 0, :], mag[:], cs[:], ALU.mult, ["s5t_mag", "s5t_cos"], ["s5t_wfm"])
            self.stt(wfm[:, :, 1, :], mag[:], -1.0, sn[:], ALU.mult, ALU.mult, ["s5t_mag", "s5t_sin"], ["s5t_wfm"])
            k = 0
            for j in range(16):
                for c in range(2):
                    pn = f"P{k % 4}"
                    k += 1
                    self.tr(t[pn][:, 0:128], wfm[:, j, c, :], ["s5t_wfm"], [pn])
                    self.cp("act" if k % 2 else "dve", W[:, j, c, :], t[pn][:, 0:128], [pn], ["s5_W"])
            c1 = 0 if d == 0 else 127
            are = V[:, :, 0, c1]
            aim = V[:, :, 1, c1]
            den = tsb("s5t_den", [128, 16])
            tmp1 = tsb("s5t_t1", [128, 16])
            tmp2 = tsb("s5t_t2", [128, 16])
            ar1 = tsb("s5t_ar1", [128, 16])
            zre = tsb("s5t_zre", [128, 16])
            zim = tsb("s5t_zim", [128, 16])
            self.tt("dve", den[:], lre[:], lre[:], ALU.mult, ["s5t_lre"], ["s5t_den"])
            self.tt("dve", tmp1[:], lim[:], lim[:], ALU.mult, ["s5t_lim"], ["s5t_t1"])
            self.tt("dve", den[:], den[:], tmp1[:], ALU.add, ["s5t_den", "s5t_t1"], ["s5t_den"])
            S.op("dve", lambda e: e.reciprocal(out=den[:], in_=den[:]), ["s5t_den"], ["s5t_den"])
            self.ts("dve", ar1[:], are, -1.0, ALU.add, ["s5_V"], ["s5t_ar1"])
            self.tt("dve", tmp1[:], ar1[:], lre[:], ALU.mult, ["s5t_ar1", "s5t_lre"], ["s5t_t1"])
            self.tt("dve", tmp2[:], aim, lim[:], ALU.mult, ["s5_V", "s5t_lim"], ["s5t_t2"])
            self.tt("dve", tmp1[:], tmp1[:], tmp2[:], ALU.add, ["s5t_t1", "s5t_t2"], ["s5t_t1"])
            self.tt("dve", zre[:], tmp1[:], den[:], ALU.mult, ["s5t_t1", "s5t_den"], ["s5t_zre"])
            self.tt("dve", tmp1[:], aim, lre[:], ALU.mult, ["s5_V", "s5t_lre"], ["s5t_t1"])
            self.tt("dve", tmp2[:], ar1[:], lim[:], ALU.mult, ["s5t_ar1", "s5t_lim"], ["s5t_t2"])
            self.tt("dve", tmp1[:], tmp1[:], tmp2[:], ALU.subtract, ["s5t_t1", "s5t_t2"], ["s5t_t1"])
            self.tt("dve", zim[:], tmp1[:], den[:], ALU.mult, ["s5t_t1", "s5t_den"], ["s5t_zim"])
            bre = tsb("s5t_bre", [128, 16, 16])
            bim = tsb("s5t_bim", [128, 16, 16])
            self.dma("sp", bre[:], self.w["s5_b_re"][l, d].rearrange("g n q -> (g n) q").rearrange("(j p) q -> p j q", p=128), (), ["s5t_bre"])
            self.dma("sp", bim[:], self.w["s5_b_im"][l, d].rearrange("g n q -> (g n) q").rearrange("(j p) q -> p j q", p=128), (), ["s5t_bim"])
            bbr = tsb("s5t_bbr", [128, 16, 16])
            bbi = tsb("s5t_bbi", [128, 16, 16])
            u1 = tsb("s5t_u1", [128, 16, 16])
            zr_b = zre[:].unsqueeze(2).to_broadcast([128, 16, 16])
            zi_b = zim[:].unsqueeze(2).to_broadcast([128, 16, 16])
            self.tt("dve", bbr[:], bre[:], zr_b, ALU.mult, ["s5t_bre", "s5t_zre"], ["s5t_bbr"])
            self.tt("dve", u1[:], bim[:], zi_b, ALU.mult, ["s5t_bim", "s5t_zim"], ["s5t_u1"])
            self.tt("dve", bbr[:], bbr[:], u1[:], ALU.subtract, ["s5t_bbr", "s5t_u1"], ["s5t_bbr"])
            self.tt("dve", bbi[:], bim[:], zr_b, ALU.mult, ["s5t_bim", "s5t_zre"], ["s5t_bbi"])
            self.tt("dve", u1[:], bre[:], zi_b, ALU.mult, ["s5t_bre", "s5t_zim"], ["s5t_u1"])
            self.tt("dve", bbi[:], bbi[:], u1[:], ALU.add, ["s5t_bbi", "s5t_u1"], ["s5t_bbi"])
            bm = tsb("s5t_bm", [128, 16, 2, 128])
            for j in range(16):
                mk = t[f"mask{j % 4}"]
                for c, src, sname in ((0, bbr, "s5t_bbr"), (1, bbi, "s5t_bbi")):
                    self.tt("dve", bm[:, j, c, :].rearrange("p (r q) -> p r q", r=8),
                            src[:, j, :].unsqueeze(1).to_broadcast([128, 8, 16]),
                            mk[:].rearrange("p (r q) -> p r q", r=8), ALU.mult, [sname, f"mask{j % 4}"], ["s5t_bm"])
            for j in range(16):
                for c in range(2):
                    pn = f"P{k % 4}"
                    k += 1
                    self.tr(t[pn][:, 0:128], bm[:, j, c, :], ["s5t_bm"], [pn])
                    self.cp("act" if k % 2 else "dve", BT[:, j, c * 128:(c + 1) * 128], t[pn][:, 0:128], [pn], ["s5_BT"])
            for c, cname in ((0, "s5_c_re"), (1, "s5_c_im")):
                ctm = tsb(f"s5t_ctm{c}", [128, 4, 2, 64])
                src = self.w[cname][l, d].rearrange("g p n -> (g p) n").rearrange("(b rp) n -> rp b n", rp=128)
                for dup in range(2):
                    self.dma("sp", ctm[:, :, dup, :], src, (), [f"s5t_ctm{c}"])
                for b in range(4):
                    pn = f"P{k % 4}"
                    k += 1
                    self.tr(t[pn][:, 0:128], ctm[:, b, :, :].rearrange("p a n -> p (a n)"), [f"s5t_ctm{c}"], [pn])
                    for kk in range(4):
                        j = 4 * b + kk
                        self.stt(CT[:, j, c, :], t[pn][:, 0:128], 1.0 if c == 0 else -1.0, t[f"mask{kk}"][:], ALU.mult, ALU.mult,
                                 [pn, f"mask{kk}"], ["s5_CT"])


FAMS = {
    "hg": dict(G=4, W=512, g=("GHF", "GHB"), kt=("KHFt", "KHBt"), v="VH", q="QH", kf=("KHF", "KHB"),
               heads=[(h, 0, 128) for h in range(4)], orow=0),
    "gl": dict(G=2, W=256, g=("GG", "GG"), kt=("KGt", "KGt"), v="VG", q="QG", kf=("KG", "KG"),
               heads=[(h // 2, (h % 2) * 64, 64) for h in range(4)], orow=512),
}


class KB3(KB2):
    def rec_pass(self, l, d):
        S, t = self.S, self.t
        T, NT, nct = self.T, self.NT, self.nct
        for nm, rows in ((f"O{d}", 1024), (f"Y5{d}", 512)):
            if nm not in self.scr:
                self.dscr(nm, [rows, T])
        order = list(range(NT)) if d == 0 else (list(range(nct - 1, -1, -1)) + list(range(NT - 1, nct - 1, -1)))
        last = 127 if d == 0 else 0
        with self.phase() as ph:
            psb = lambda name, shape, dt=F32: (self.sb(ph, name, shape, dt) if name not in t else t[name])
            self.s5_setup(ph, l, d)
            minc, mstr, mincb = t[f"minc{d}"], t[f"mstr{d}"], t[f"mincb{d}"]
            mincn, mstrn, mincbn = f"minc{d}", f"mstr{d}", f"mincb{d}"
            W5, V5, BT5, CT5 = t["s5_W"], t["s5_V"], t["s5_BT"], t["s5_CT"]
            for f in ("hg", "gl"):
                psb(f"{f}_Sf", [128, 4, 128])
                psb(f"{f}_Sb", [128, 4, 128], BF16)
                self.memset("pool", t[f"{f}_Sf"][:], 0.0, [f"{f}_Sf"])
                self.memset("pool", t[f"{f}_Sb"][:], 0.0, [f"{f}_Sb"])
            carry = psb("s5_carry", [128, 16, 2])
            self.memset("pool", carry[:], 0.0, ["s5_carry0", "s5_carry1"])
            dbuf = psb("r_D", [128, 2, 8, 128])

            def loads(ti, par):
                rows = slice(ti * 128, (ti + 1) * 128)
                cols = slice(ti * 128, (ti + 1) * 128)
                for f, F_ in FAMS.items():
                    G, W = F_["G"], F_["W"]
                    gt = psb(f"{f}_g{par}", [128, W])
                    gsrc = self.scr[F_["g"][d]]
                    gs = gsrc[rows, :] if f == "hg" else gsrc[rows, d * 256:(d + 1) * 256]
                    self.dma("sp", gt[:], gs, [f"{F_['g'][d]}@{ti}"], [f"{f}_g{par}"])
                    kt = psb(f"{f}_kt{par}", [128, W])
                    self.dma("sp", kt[:], self.scr[F_["kt"][d]][rows, :], [f"{F_['kt'][d]}@{ti}"], [f"{f}_kt{par}"])
                    vt = psb(f"{f}_v{par}", [128, 512], BF16)
                    self.dma("sp", vt[:], self.scr[F_["v"]][rows, :], [f"{F_['v']}@{ti}"], [f"{f}_v{par}"])
                    qf = psb(f"{f}_q{par}", [128, G, 128])
                    self.dma("sp", qf[:], self.scr[F_["q"]].rearrange("(g p) t -> p g t", p=128)[:, :, cols], [f"{F_['q']}@{ti}"], [f"{f}_q{par}"])
                    kf = psb(f"{f}_kf{par}", [128, G, 128])
                    self.dma("sp", kf[:], self.scr[F_["kf"][d]].rearrange("(g p) t -> p g t", p=128)[:, :, cols], [f"{F_['kf'][d]}@{ti}"], [f"{f}_kf{par}"])
                u = psb(f"s5_u{par}", [128, 4, 128])
                self.dma("sp", u[:], self.scr["U5"].rearrange("(g p) t -> p g t", p=128)[:, :, cols], [f"U5@{ti}"], [f"s5_u{par}"])

            def fam_tile(f, ti, par):
                F_ = FAMS[f]
                G, W, heads = F_["G"], F_["W"], F_["heads"]
                gt, kt, vt, qf, kf = (t[f"{f}_{x}{par}"] for x in ("g", "kt", "v", "q", "kf"))
                gn, ktn, vn, qn, kfn = (f"{f}_{x}{par}" for x in ("g", "kt", "v", "q", "kf"))
                Sf, Sb = t[f"{f}_Sf"], t[f"{f}_Sb"]
                PE_, PB, PS, PO = (t[f"P{i}"] for i in range(4))
                PU = t["P0"]
                ex = psb(f"{f}_ex", [128, W])
                if f == "hg":
                    khat = psb(f"{f}_khat", [128, 4, 128], BF16)
                else:
                    fresh = "gl_khat" not in t
                    khat = psb("gl_khat", [128, 4, 128], BF16)
                    if fresh:
                        self.memset("pool", khat[:], 0.0, ["gl_khat"])
                bt = psb(f"{f}_bt", [128, G, 128])
                eb = psb(f"{f}_eb", [128, G, 128])
                qbar = psb(f"{f}_qbar", [128, G, 128], BF16)
                bprev = psb(f"{f}_bprev", [128, G, 8])
                bq = psb(f"{f}_bq", [128, G, 128])
                qt = psb(f"{f}_qt", [128, G, 128], BF16)
                pt = psb(f"{f}_pt", [128, 4, 128], BF16)
                osb = psb(f"{f}_osb", [128, 4, 128])
                ktb = psb(f"{f}_Kt", [128, 4, 8, 128], BF16)
                self.mm(PE_[:, 0:W], mstr[:], gt[:], [mstrn, gn], ["P0"])
                self.act(ex[:], PE_[:, 0:W], AF.Exp, ["P0"], [f"{f}_ex"])
                if f == "hg":
                    self.tt("dve", khat[:].rearrange("p h c -> p (h c)"), ex[:], kt[:], ALU.mult, [f"{f}_ex", ktn], [f"{f}_khat"])
                else:
                    kb_ = khat[:]
                    kout = bass.AP(kb_.tensor, kb_.offset, [list(kb_.ap[0]), [256, 2], [192, 2], [1, 64]])
                    self.tt("dve", kout, ex[:].rearrange("p (g a c) -> p g a c", g=2, a=2), kt[:].rearrange("p (g a c) -> p g a c", g=2, a=2),
                            ALU.mult, [f"{f}_ex", ktn], [f"{f}_khat"])
                yield
                for g in range(G):
                    self.mm(PB[:, g * 128:(g + 1) * 128], gt[:, g * 128:(g + 1) * 128], minc[:], [gn, mincn], ["P1"])
                pb3 = PB[:, 0:G * 128].rearrange("p (g c) -> p g c", g=G)
                self.cp("act", bt[:], pb3, ["P1"], [f"{f}_bt"])
                self.act(eb[:], pb3, AF.Exp, ["P1"], [f"{f}_eb"])
                self.tt("dve", qbar[:], qf[:], eb[:], ALU.mult, [qn, f"{f}_eb"], [f"{f}_qbar"])
                yield
                if d == 0:
                    self.memset("pool", bprev[:, :, 0:1], 0.0, [f"{f}_bprev"])
                    self.cp("pool", bprev[:, :, 1:8], bt[:, :, 15:127:16], [f"{f}_bt"], [f"{f}_bprev"])
                else:
                    self.memset("pool", bprev[:, :, 7:8], 0.0, [f"{f}_bprev"])
                    self.cp("pool", bprev[:, :, 0:7], bt[:, :, 16:128:16], [f"{f}_bt"], [f"{f}_bprev"])
                self.tt("dve", bq[:].rearrange("p g (i j) -> p g i j", i=8), bt[:].rearrange("p g (i j) -> p g i j", i=8),
                        bprev[:].unsqueeze(3).to_broadcast([128, G, 8, 16]), ALU.subtract, [f"{f}_bt", f"{f}_bprev"], [f"{f}_bq"])
                self.act(bq[:], bq[:], AF.Exp, [f"{f}_bq"], [f"{f}_bq"])
                self.tt("dve", qt[:], bq[:], qf[:], ALU.mult, [f"{f}_bq", qn], [f"{f}_qt"])
                yield
                if f == "gl":
                    kfm = psb("gl_kfm", [128, 4, 128])
                    for h, (g, p0, ps_) in enumerate(heads):
                        self.ts("dve", kfm[:, h, :], kf[:, g, :], t[f"hm{h % 2}"][:, 0:1], ALU.mult, [kfn, f"hm{h % 2}"], ["gl_kfm"])
                for g0 in range(0, G, 2):
                    dv = dbuf[:, 0:2, :, :]
                    self.tt("dve", dv, bprev[:, g0:g0 + 2, :].unsqueeze(3).to_broadcast([128, 2, 8, 128]),
                            bt[:, g0:g0 + 2, :].unsqueeze(2).to_broadcast([128, 2, 8, 128]), ALU.subtract, [f"{f}_bprev", f"{f}_bt"], ["r_D"])
                    self.ts("pool", dv, dv, 60.0, ALU.min, ["r_D"], ["r_D"])
                    self.act(dv, dv, AF.Exp, ["r_D"], ["r_D"])
                    if f == "hg":
                        self.tt("dve", ktb[:, g0:g0 + 2, :, :], dv, kf[:, g0:g0 + 2, :].unsqueeze(2).to_broadcast([128, 2, 8, 128]), ALU.mult, ["r_D", kfn], [f"{f}_Kt"])
                    else:
                        for h, (g, p0, ps_) in enumerate(heads):
                            self.tt("dve", ktb[:, h, :, :], dbuf[:, g, :, :], kfm[:, h, :].unsqueeze(1).to_broadcast([128, 8, 128]), ALU.mult,
                                    ["r_D", "gl_kfm"], [f"{f}_Kt"])
                yield
                for h, (g, p0, ps_) in enumerate(heads):
                    for i in range(8):
                        self.mm(PS[:, h * 128 + 16 * i:h * 128 + 16 * i + 16], ktb[:, h, i, :],
                                qt[:, g, 16 * i:16 * i + 16], [f"{f}_Kt", f"{f}_qt"], ["P2"])
                self.tt("dve", pt[:], PS[:].rearrange("p (h c) -> p h c", h=4), minc[:].unsqueeze(1).to_broadcast([128, 4, 128]),
                        ALU.mult, ["P2", mincn], [f"{f}_pt"])
                yield
                for h, (g, p0, ps_) in enumerate(heads):
                    self.mm(PO[:, h * 128:(h + 1) * 128], vt[:, h * 128:(h + 1) * 128], pt[:, h, :], [vn, f"{f}_pt"], ["P3"], start=True, stop=False)
                    self.mm(PO[:, h * 128:(h + 1) * 128], Sb[:, h, :], qbar[:, g, :], [f"{f}_Sb", f"{f}_qbar"], ["P3"], start=False, stop=True)
                self.cp("act", osb[:], PO[:].rearrange("p (h c) -> p h c", h=4), ["P3"], [f"{f}_osb"])
                orow = F_["orow"]
                self.dma("act", self.scr[f"O{d}"][orow:orow + 512, :].rearrange("(h p) t -> p h t", p=128)[:, :, ti * 128:(ti + 1) * 128],
                         osb[:], [f"{f}_osb"], [f"O{d}{f}@{ti}"])
                yield
                for h, (g, p0, ps_) in enumerate(heads):
                    self.mm(PU[:, h * 128:(h + 1) * 128], khat[:, h, :], vt[:, h * 128:(h + 1) * 128], [f"{f}_khat", vn], ["P0"])
                for h, (g, p0, ps_) in enumerate(heads):
                    self.stt(Sf[:, h, :], Sf[:, h, :], eb[:, g, last:last + 1], PU[:, h * 128:(h + 1) * 128], ALU.mult, ALU.add,
                             [f"{f}_Sf", f"{f}_eb", "P0"], [f"{f}_Sf"])
                self.cp("act", Sb[:], Sf[:], [f"{f}_Sf"], [f"{f}_Sb"])
                yield

            def s5_prep(ti, par):
                u = t[f"s5_u{par}"]
                ub = psb("s5_ub", [128, 4, 128], BF16)
                self.cp("pool", ub[:], u[:], [f"s5_u{par}"], ["s5_ub"])
                psb("s5_sbf", [128, 16, 2, 128], BF16)

            def s5_half(ti, par, hp):
                ub = t["s5_ub"]
                sbf = t["s5_sbf"]
                PBU, pbun = (t["P5"], "P5") if hp == 0 else (t["P7"], "P7")
                PZ, pzn = (t["P6"], "P6") if hp == 0 else (t["P4"], "P4")
                sbfn, carn = f"s5_sbf{hp}", f"s5_carry{hp}"
                for r in range(hp, 8, 2):
                    for jj in range(2):
                        j = 2 * r + jj
                        self.mm(PBU[:, jj * 256:(jj + 1) * 256], ub[:, j // 4, :], BT5[:, j, :], ["s5_ub", "s5_BT"], [pbun])
                    yield
                    pbu = PBU[:].rearrange("p (a c n) -> p a c n", a=2, c=2)
                    Wv = W5[:, 2 * r:2 * r + 2, :, :]
                    t1 = psb(f"s5_t1_{hp}", [128, 2, 128])
                    t2 = psb(f"s5_t2_{hp}", [128, 2, 128])
                    x = psb(f"s5_x{hp}", [128, 2, 2, 128], BF16)
                    xn, t1n, t2n = f"s5_x{hp}", f"s5_t1_{hp}", f"s5_t2_{hp}"
                    self.tt("dve", t1[:], pbu[:, :, 0, :], Wv[:, :, 0, :], ALU.mult, [pbun, "s5_W"], [t1n])
                    self.tt("dve", t2[:], pbu[:, :, 1, :], Wv[:, :, 1, :], ALU.mult, [pbun, "s5_W"], [t2n])
                    self.tt("dve", x[:, :, 0, :], t1[:], t2[:], ALU.subtract, [t1n, t2n], [xn])
                    self.tt("dve", t1[:], pbu[:, :, 0, :], Wv[:, :, 1, :], ALU.mult, [pbun, "s5_W"], [t1n])
                    self.tt("dve", t2[:], pbu[:, :, 1, :], Wv[:, :, 0, :], ALU.mult, [pbun, "s5_W"], [t2n])
                    self.tt("dve", x[:, :, 1, :], t1[:], t2[:], ALU.add, [t1n, t2n], [xn])
                    yield
                    zs = psb(f"s5_zs_{hp}", [128, 2, 2, 128])
                    zsn = f"s5_zs_{hp}"
                    for jj in range(2):
                        for c in range(2):
                            col = (jj * 2 + c) * 128
                            self.mm(PZ[:, col:col + 128], x[:, jj, c, :], mincb[:], [xn, mincbn], [pzn])
                    for jj in range(2):
                        for c in range(2):
                            col = (jj * 2 + c) * 128
                            self.act(zs[:, jj, c, :], PZ[:, col:col + 128], AF.Identity, [pzn, carn], [zsn],
                                     bias=carry[:, 2 * r + jj, c:c + 1])
                    yield
                    Vv = V5[:, 2 * r:2 * r + 2, :, :]
                    sre = psb(f"s5_sre_{hp}", [128, 2, 128])
                    sim = psb(f"s5_sim_{hp}", [128, 2, 128])
                    m1 = psb(f"s5_m1_{hp}", [128, 2, 128])
                    m2 = psb(f"s5_m2_{hp}", [128, 2, 128])
                    sren, simn, m1n, m2n = f"s5_sre_{hp}", f"s5_sim_{hp}", f"s5_m1_{hp}", f"s5_m2_{hp}"
                    self.tt("dve", m1[:], zs[:, :, 0, :], Vv[:, :, 0, :], ALU.mult, [zsn, "s5_V"], [m1n])
                    self.tt("pool", m2[:], zs[:, :, 1, :], Vv[:, :, 1, :], ALU.mult, [zsn, "s5_V"], [m2n])
                    self.tt("dve", sre[:], m1[:], m2[:], ALU.subtract, [m1n, m2n], [sren])
                    self.tt("dve", m1[:], zs[:, :, 0, :], Vv[:, :, 1, :], ALU.mult, [zsn, "s5_V"], [m1n])
                    self.tt("pool", m2[:], zs[:, :, 1, :], Vv[:, :, 0, :], ALU.mult, [zsn, "s5_V"], [m2n])
                    self.tt("dve", sim[:], m1[:], m2[:], ALU.add, [m1n, m2n], [simn])
                    yield
                    self.cp("pool", sbf[:, 2 * r:2 * r + 2, 0, :], sre[:], [sren], [sbfn])
                    self.cp("pool", sbf[:, 2 * r:2 * r + 2, 1, :], sim[:], [simn], [sbfn])
                    self.cp("pool", carry[:, 2 * r:2 * r + 2, 0], sre[:, :, last], [sren], [carn])
                    self.cp("pool", carry[:, 2 * r:2 * r + 2, 1], sim[:, :, last], [simn], [carn])
                    yield

            def s5_fin(ti, par):
                sbf = t["s5_sbf"]
                PY = t["P5"]
                for b in range(4):
                    for kk in range(4):
                        j = 4 * b + kk
                        self.mm(PY[:, b * 128:(b + 1) * 128], CT5[:, j, 0, :], sbf[:, j, 0, :], ["s5_CT", "s5_sbf0", "s5_sbf1"], ["P5"], start=(kk == 0), stop=False)
                        self.mm(PY[:, b * 128:(b + 1) * 128], CT5[:, j, 1, :], sbf[:, j, 1, :], ["s5_CT", "s5_sbf0", "s5_sbf1"], ["P5"], start=False, stop=(kk == 3))
                ysb = psb("s5_ysb", [128, 4, 128])
                self.cp("act", ysb[:], PY[:].rearrange("p (b c) -> p b c", b=4), ["P5"], ["s5_ysb"])
                self.dma("act", self.scr[f"Y5{d}"].rearrange("(b p) t -> p b t", p=128)[:, :, ti * 128:(ti + 1) * 128], ysb[:],
                         ["s5_ysb"], [f"Y5{d}@{ti}"])

            import os as _os
            skip = ""
            if "all" in skip:
                return
            loads(order[0], 0)
            for n, ti in enumerate(order):
                par = n % 2
                if n + 1 < len(order):
                    loads(order[n + 1], 1 - par)
                gens = []
                if "hg" not in skip:
                    gens.append(fam_tile("hg", ti, par))
                if "gl" not in skip:
                    gens.append(fam_tile("gl", ti, par))
                if "s5" not in skip:
                    s5_prep(ti, par)
                    gens.append(s5_half(ti, par, 0))
                    gens.append(s5_half(ti, par, 1))
                while gens:
                    for g_ in list(gens):
                        try:
                            next(g_)
                        except StopIteration:
                            gens.remove(g_)
                if "s5" not in skip:
                    s5_fin(ti, par)


class KB4(KB3):
    def load_cast(self, stack, dst, dst_name, src_ap, nk, ncols, kstep):
        t = self.t
        for k0 in range(0, nk, kstep):
            kn = min(kstep, nk - k0)
            sn = f"lc_st{kstep}_{self._lc % 2}"
            self._lc += 1
            stg = self.sb(stack, sn, [128, kstep, 1024]) if sn not in t else t[sn]
            self.dma("sp", stg[:, 0:kn, 0:ncols], src_ap[:, k0:k0 + kn, :], (), [sn])
            self.cp("pool", dst[:, k0:k0 + kn, 0:ncols], stg[:, 0:kn, 0:ncols], [sn], [dst_name])

    def merge_phase(self, l, last):
        S, t = self.S, self.t
        T, NT = self.T, self.NT
        self._lc = 0
        with self.phase() as ph:
            psb = lambda name, shape, dt=F32: (self.sb(ph, name, shape, dt) if name not in t else t[name])
            bp = psb("mg_bp", [128, 12, D], BF16)
            wo = psb("mg_wo", [128, 8, D], BF16)
            glu = psb("mg_glu", [128, 4, 512], BF16)
            with self.phase() as tmpw:
                self.load_cast(tmpw, bp, "mg_bp", self.w["branch_proj"][l].rearrange("b (kc p) n -> p (b kc) n", p=128), 12, D, 4)
                self.load_cast(tmpw, wo, "mg_wo", self.w["w_out"][l].rearrange("(kc p) n -> p kc n", p=128), 8, D, 4)
                self.load_cast(tmpw, glu, "mg_glu", self.w["s5_glu_w"][l].rearrange("(kc p) n -> p kc n", p=128), 4, 512, 4)
            gains = {}
            for nm, src in (("hgn", "hg_norm_g"), ("gln", "gla_norm_g"), ("s5d", "s5_d")):
                g = psb("mg_" + nm, [128, 4])
                self.dma("sp", g[:], self.w[src][l].rearrange("(h p) -> p h", p=128), (), ["mg_" + nm], allow_slow_non_contiguous=True)
                gains[nm] = g
            self.load_mod(ph, l, 2, "mg_g1")
            epsT = psb("mg_eps", [128, 1])
            self.memset("pool", epsT[:], EPS, ["mg_eps"])
            tiles = [ti for ti in range(NT) if not (last and self.is_ctx(ti))]

            def ld(name, par, src_ap, shape, deps, dt=F32):
                nm = f"mg_{name}{par}"
                h = psb(nm, shape, dt)
                self.dma("sp", h[:], src_ap, deps, [nm])
                return h

            def loads(ti, par):
                cols = slice(ti * 128, (ti + 1) * 128)
                fmv = lambda ap: ap.rearrange("(h p) t -> p h t", p=128)[:, :, cols]
                for d in (0, 1):
                    ld(f"ohg{d}", par, fmv(self.scr[f"O{d}"][0:512, :]), [128, 4, 128], [f"O{d}hg@{ti}"])
                    ld(f"ogl{d}", par, fmv(self.scr[f"O{d}"][512:1024, :]), [128, 4, 128], [f"O{d}gl@{ti}"])
                    ld(f"y5{d}", par, fmv(self.scr[f"Y5{d}"]), [128, 4, 128], [f"Y5{d}@{ti}"])
                ld("u5", par, fmv(self.scr["U5"]), [128, 4, 128], [f"U5@{ti}"])
                ld("gh", par, fmv(self.scr["GH"]), [128, 4, 128], [f"GH@{ti}"])
                ld("rg", par, fmv(self.scr["RG"]), [128, 4, 128], [f"RG@{ti}"])
                for nm in ("GA", "GB", "GC"):
                    ld(nm, par, fmv(self.scr[nm]), [128, 8, 128], [f"{nm}@{ti}"])
                ld("x", par, self.scr["X"][ti * 128:(ti + 1) * 128, :], [128, D], [f"X@{ti}"])

            def compute(ti, par):
                v = 1 if self.is_ctx(ti) else 0
                g = lambda name: (t[f"mg_{name}{par}"], f"mg_{name}{par}")
                ybr = {}
                for bi, (fam, gate, gain) in enumerate((("hg", "gh", "hgn"), ("gl", "rg", "gln"))):
                    o0, o0n = g(f"o{fam}0")
                    o1, o1n = g(f"o{fam}1")
                    gt, gtn = g(gate)
                    o = psb(f"mg_o{bi}", [128, 4, 128])
                    osq = psb(f"mg_osq{bi}", [128, 4, 128])
                    rs = psb(f"mg_rs{bi}", [128, 4, 128])
                    yb = psb(f"mg_y{bi}", [128, 4, 128], BF16)
                    on, sqn, rsn, ybn = f"mg_o{bi}", f"mg_osq{bi}", f"mg_rs{bi}", f"mg_y{bi}"
                    self.tt("dve", o[:], o0[:], o1[:], ALU.add, [o0n, o1n], [on])
                    self.tt("pool", osq[:], o[:], o[:], ALU.mult, [on], [sqn])
                    self.mm(t["P0"][:], t["ones"][:], osq[:].rearrange("p h c -> p (h c)"), ["ones", sqn], ["P0"])
                    self.act(rs[:].rearrange("p h c -> p (h c)"), t["P0"][:], AF.Sqrt, ["P0", "mg_eps"], [rsn], bias=epsT[:], scale=1.0 / 128.0)
                    S.op("dve", lambda e, rs=rs: e.reciprocal(out=rs[:], in_=rs[:]), [rsn], [rsn])
                    self.tt("dve", o[:], o[:], rs[:], ALU.mult, [on, rsn], [on])
                    for h in range(4):
                        self.stt(yb[:, h, :], o[:, h, :], gains[gain][:, h:h + 1], gt[:, h, :], ALU.mult, ALU.mult,
                                 [on, "mg_" + gain, gtn], [ybn])
                    ybr[bi] = (yb, ybn)
                u5, u5n = g("u5")
                y50, y50n = g("y50")
                y51, y51n = g("y51")
                yy = psb("mg_yy", [128, 4, 128])
                tq = psb("mg_tq", [128, 4, 128])
                for k in range(4):
                    self.stt(yy[:, k, :], u5[:, k, :], gains["s5d"][:, k:k + 1], y50[:, k, :], ALU.mult, ALU.add, [u5n, "mg_s5d", y50n], ["mg_yy"])
                self.tt("dve", yy[:], yy[:], y51[:], ALU.add, ["mg_yy", y51n], ["mg_yy"])
                self.tt("pool", tq[:], yy[:], yy[:], ALU.mult, ["mg_yy"], ["mg_tq"])
                self.ts("pool", tq[:], tq[:], 0.044715, ALU.mult, ["mg_tq"], ["mg_tq"], s2=1.0, op1=ALU.add)
                self.tt("pool", tq[:], tq[:], yy[:], ALU.mult, ["mg_tq", "mg_yy"], ["mg_tq"])
                self.act(tq[:], tq[:], AF.Tanh, ["mg_tq"], ["mg_tq"], scale=math.sqrt(2.0 / math.pi))
                self.ts("dve", tq[:], tq[:], 1.0, ALU.add, ["mg_tq"], ["mg_tq"], s2=0.5, op1=ALU.mult)
                self.tt("dve", yy[:], yy[:], tq[:], ALU.mult, ["mg_yy", "mg_tq"], ["mg_yy"])
                ygb = psb("mg_ygb", [128, 4, 128], BF16)
                self.cp("pool", ygb[:], yy[:], ["mg_yy"], ["mg_ygb"])
                for oc in range(4):
                    for kc in range(4):
                        self.mm(t["P1"][:, oc * 128:(oc + 1) * 128], glu[:, kc, oc * 128:(oc + 1) * 128], ygb[:, kc, :],
                                ["mg_glu", "mg_ygb"], ["P1"], start=(kc == 0), stop=(kc == 3))
                self.act(tq[:].rearrange("p h c -> p (h c)"), t["P1"][:], AF.Sigmoid, ["P1"], ["mg_tq"])
                yc = psb("mg_y2", [128, 4, 128], BF16)
                self.tt("dve", yc[:], yy[:], tq[:], ALU.mult, ["mg_yy", "mg_tq"], ["mg_y2"])
                ybr[2] = (yc, "mg_y2")
                mg = psb("mg_m", [128, 8, 128])
                mt = psb("mg_mt", [128, 8, 128])
                for bi, gname in enumerate(("GA", "GB", "GC")):
                    yb, ybn = ybr[bi]
                    gt, gtn = g(gname)
                    for oc in range(8):
                        pn = f"P{2 + oc // 4}"
                        for kc in range(4):
                            self.mm(t[pn][:, (oc % 4) * 128:(oc % 4 + 1) * 128], bp[:, bi * 4 + kc, oc * 128:(oc + 1) * 128], yb[:, kc, :],
                                    ["mg_bp", ybn], [pn], start=(kc == 0), stop=(kc == 3))
                    for half in range(2):
                        pn = f"P{2 + half}"
                        pv = t[pn][:].rearrange("p (o c) -> p o c", o=4)
                        dst = mg if bi == 0 else mt
                        dn = "mg_m" if bi == 0 else "mg_mt"
                        self.tt("dve", dst[:, half * 4:(half + 1) * 4, :], pv, gt[:, half * 4:(half + 1) * 4, :], ALU.mult, [pn, gtn], [dn])
                    if bi > 0:
                        self.tt("pool", mg[:], mg[:], mt[:], ALU.add, ["mg_m", "mg_mt"], ["mg_m"])
                mgb = psb("mg_mb", [128, 8, 128], BF16)
                self.cp("pool", mgb[:], mg[:], ["mg_m"], ["mg_mb"])
                x, xn = g("x")
                xt_ = psb("mg_xt", [128, D])
                for half in range(2):
                    pn = f"P{4 + half}"
                    for kc in range(8):
                        self.mm(t[pn][:], mgb[:, kc, :], wo[:, kc, half * 512:(half + 1) * 512], ["mg_mb", "mg_wo"], [pn],
                                start=(kc == 0), stop=(kc == 7))
                    self.tt("dve", xt_[:, half * 512:(half + 1) * 512], t[pn][:], t[f"mg_g1{v}"][:, half * 512:(half + 1) * 512], ALU.mult,
                            [pn, f"mg_g1{v}"], ["mg_xt"])
                self.tt("pool", x[:], x[:], xt_[:], ALU.add, [xn, "mg_xt"], [xn])
                self.dma("act", self.scr["X"][ti * 128:(ti + 1) * 128, :], x[:], [xn], [f"X@{ti}"])

            loads(tiles[0], 0)
            for n, ti in enumerate(tiles):
                if n + 1 < len(tiles):
                    loads(tiles[n + 1], (n + 1) % 2)
                compute(ti, n % 2)

    def ffn_phase(self, l, moe):
        S, t = self.S, self.t
        T, NT = self.T, self.NT
        self._lc = 0
        GF = 4
        if moe:
            NL = self.NLOC
            tiles = list(range(NL))
            TT_ = NL * 128
            if "XL" not in self.scr:
                self.dscr("XL", [TT_, D])
            xs, xtag = self.scr["XL"], "XL"
        else:
            tiles = list(range(NT))
            TT_ = T
            xs, xtag = self.scr["X"], "X"
        tbs = [(s_, min(512, TT_ - s_)) for s_ in range(0, TT_, 512)]
        with self.phase() as ph:
            psb = lambda name, shape, dt=F32: (self.sb(ph, name, shape, dt) if name not in t else t[name])
            cmbt = psb("ff_cmbt", [128, self.NLOC, NE]) if moe else None
            hT = self.norm_phase(ph, l, True, tiles, router=moe, local=cmbt)
            self.load_mod(ph, l, 5, "ff_g2")
            ag = psb("ff_a", [128, GF, TT_], BF16)
            w2g = psb("ff_w2", [128, GF, D], BF16)
            experts = list(range(NE)) if moe else [None]
            cnt = 0
            for e in experts:
                if moe:
                    w1s, w3s, w2s = self.w["moe_w1"][0, e], self.w["moe_w3"][0, e], self.w["moe_w2"][0, e]
                else:
                    w1s, w3s, w2s = self.w["ffn_w1"][0], self.w["ffn_w3"][0], self.w["ffn_w2"][0]
                w1v = w1s.rearrange("(kc p) f -> p kc f", p=128)
                w3v = w3s.rearrange("(kc p) f -> p kc f", p=128)
                w2v = w2s.rearrange("(fc p) n -> p fc n", p=128)
                for grp in range(FF // 128 // GF):
                    for fi in range(GF):
                        fc = grp * GF + fi
                        i = cnt % 2
                        cnt += 1
                        wf = psb(f"ff_wf{i}", [128, 2, 8, 128])
                        wb = psb(f"ff_wb{i}", [128, 2, 8, 128], BF16)
                        self.dma("sp", wf[:, 0, :, :], w1v[:, :, fc * 128:(fc + 1) * 128], (), [f"ff_wf{i}"])
                        self.dma("sp", wf[:, 1, :, :], w3v[:, :, fc * 128:(fc + 1) * 128], (), [f"ff_wf{i}"])
                        self.cp("pool", wb[:], wf[:], [f"ff_wf{i}"], [f"ff_wb{i}"])
                        for bi_, (s0, sl) in enumerate(tbs):
                            pa, pb_ = f"P{(bi_ % 2) * 2}", f"P{(bi_ % 2) * 2 + 1}"
                            for kc in range(8):
                                self.mm(t[pa][:, 0:sl], wb[:, 0, kc, :], hT[:, kc, s0:s0 + sl], [f"ff_wb{i}", "hT"], [pa], start=(kc == 0), stop=(kc == 7))
                            for kc in range(8):
                                self.mm(t[pb_][:, 0:sl], wb[:, 1, kc, :], hT[:, kc, s0:s0 + sl], [f"ff_wb{i}", "hT"], [pb_], start=(kc == 0), stop=(kc == 7))
                            sg = psb(f"ff_s{bi_ % 2}", [128, 512])
                            sgn = f"ff_s{bi_ % 2}"
                            self.act(sg[:, 0:sl], t[pa][:, 0:sl], AF.Silu, [pa], [sgn])
                            self.tt("dve", ag[:, fi, s0:s0 + sl], sg[:, 0:sl], t[pb_][:, 0:sl], ALU.mult, [sgn, pb_], ["ff_a"])
                    self.load_cast(ph, w2g, "ff_w2", w2v[:, grp * GF:(grp + 1) * GF, :], GF, D, 1)
                    for n, ti in enumerate(tiles):
                        v = 1 if (not moe and self.is_ctx(ti)) else 0
                        xn = f"ff_x{n % 2}"
                        x = psb(xn, [128, D])
                        self.dma("sp", x[:], xs[ti * 128:(ti + 1) * 128, :], [f"{xtag}@{ti}"], [xn])
                        xt_ = psb("ff_xt", [128, D])
                        for half in range(2):
                            pn = f"P{4 + (n % 2) * 2 + half}"
                            for fi in range(GF):
                                self.mm(t[pn][:], ag[:, fi, ti * 128:(ti + 1) * 128], w2g[:, fi, half * 512:(half + 1) * 512], ["ff_a", "ff_w2"], [pn],
                                        start=(fi == 0), stop=(fi == GF - 1))
                            g2 = t[f"ff_g2{v}"][:, half * 512:(half + 1) * 512]
                            if moe:
                                self.stt(xt_[:, half * 512:(half + 1) * 512], t[pn][:], cmbt[:, ti, e:e + 1], g2, ALU.mult, ALU.mult,
                                         [pn, f"ff_g2{v}", "ff_cmbt"], ["ff_xt"])
                            else:
                                self.tt("dve", xt_[:, half * 512:(half + 1) * 512], t[pn][:], g2, ALU.mult, [pn, f"ff_g2{v}"], ["ff_xt"])
                        self.tt("dve", x[:], x[:], xt_[:], ALU.add, [xn, "ff_xt"], [xn])
                        self.dma("pool", xs[ti * 128:(ti + 1) * 128, :], x[:], [xn], [f"{xtag}@{ti}"])

    def final_phase(self):
        S, t = self.S, self.t
        with self.phase() as ph:
            psb = lambda name, shape, dt=F32: (self.sb(ph, name, shape, dt) if name not in t else t[name])
            g = psb("fn_g", [128, D])
            self.dma("sp", g[:], pbc(self.w["final_norm_g"].rearrange("(o d) -> o d", o=1)), (), ["fn_g"])
            if "XL" not in self.scr:
                self.dscr("XL", [self.NLOC * 128, D])
                idxt = psb("fn_idx", [128, self.NLOC], I32)
                self.dma("sp", idxt[:], self.idx_in, (), ["fn_idx"])
                for lt in range(self.NLOC):
                    g_ = psb(f"fn_g{lt % 2}", [128, D])
                    self.S.dma_gather(g_[:], self.scr["X"], idxt[:, lt:lt + 1], ["fn_idx"] + [f"X@{k}" for k in range(self.NT)], [f"fn_g{lt % 2}"])
                    self.dma("sp", self.scr["XL"][lt * 128:(lt + 1) * 128, :], g_[:], [f"fn_g{lt % 2}"], [f"XL@{lt}"])
            for n, ti in enumerate(range(self.NLOC)):
                xn, jn, sn = f"fn_x{n % 2}", f"fn_j{n % 2}", f"fn_s{n % 2}"
                x, junk, ss = psb(xn, [128, D]), psb(jn, [128, D]), psb(sn, [128, 1])
                self.dma("sp", x[:], self.scr["XL"][ti * 128:(ti + 1) * 128, :], [f"XL@{ti}"], [xn])
                self.act(junk[:], x[:], AF.Square, [xn], [jn, sn], accum=ss[:])
                self.ts("dve", ss[:], ss[:], 1.0 / D, ALU.mult, [sn], [sn], s2=EPS, op1=ALU.add)
                self.act(ss[:], ss[:], AF.Sqrt, [sn], [sn])
                S.op("dve", lambda e, ss=ss: e.reciprocal(out=ss[:], in_=ss[:]), [sn], [sn])
                self.stt(junk[:], x[:], ss[:], g[:], ALU.mult, ALU.mult, [xn, sn, "fn_g"], [jn])
                self.dma("pool", self.out[ti * 128:(ti + 1) * 128, :], junk[:], [jn], ["out"])
            self.S.finish(["out"])

    def mixer(self, l, sub=None):
        with self.phase() as ph:
            self.norm_phase(ph, l, False, list(range(self.NT)))
            if sub == "norm":
                return
            self.proj_phase(ph, l)
        if sub == "proj":
            return
        self.rec_pass(l, 0)
        if sub == "rec0":
            return
        self.rec_pass(l, 1)
        if sub == "rec1":
            return
        self.merge_phase(l, last=(l == 1))

    def build_all(self, upto="all"):
        stages = ["setup", "mod0", "mix0", "ffn0", "mod1", "mix1", "ffn1"]
        self.setup()
        if upto != "setup":
            self.mod_phase(0)
            if upto != "mod0":
                self.mixer(0, upto[4:] if upto.startswith("sub0") else None)
                if upto != "mix0" and not upto.startswith("sub0"):
                    self.ffn_phase(0, moe=False)
                    if upto != "ffn0":
                        self.mod_phase(1)
                        self.mixer(1)
                        if upto != "mix1":
                            self.ffn_phase(1, moe=True)
        self.final_phase()
        self.st.close()
        return self.nc


_NC_CACHE = {}


def _build_full():
    if "nc" not in _NC_CACHE:
        k = KB4(nct=2, nlt=32)
        _NC_CACHE["nc"] = k.build_all("all")
    return _NC_CACHE["nc"]


def kernel(**inputs):
    x = np.asarray(inputs["x"], dtype=np.float32)
    c = np.asarray(inputs["c"], dtype=np.float32)
    ctx = np.asarray(inputs["ctx"], dtype=np.float32)
    c_ctx = np.asarray(inputs["c_ctx"], dtype=np.float32)
    nb = x.shape[0]
    shared = {}
    for name, shape in WSPECS:
        shared[name] = np.ascontiguousarray(np.asarray(inputs[name], dtype=np.float32)).reshape(shape)
    cctxcol = np.ascontiguousarray(c_ctx.reshape(8, 128).T)
    in_maps = []
    nloc = 16
    for core in range(8):
        b, half = core % nb, core // nb
        m = dict(shared)
        m["xin"] = np.ascontiguousarray(np.concatenate([ctx[b], x[b]], axis=0))
        m["ccol"] = np.ascontiguousarray(c[b].reshape(8, 128).T)
        m["cctxcol"] = cctxcol
        m["idx"] = (256 + half * nloc * 128 + np.arange(nloc)[None, :] * 128 + np.arange(128)[:, None]).astype(np.int32)
        in_maps.append(m)
    nc = _build_full()
    res = run_bass_kernel_spmd(nc, in_maps, core_ids=list(range(8)))
    out = np.stack([np.concatenate([np.asarray(res.results[b + nb * h]["out"], dtype=np.float32) for h in range(2)], axis=0)
                    for b in range(nb)], axis=0)
    return out
```

```python
import math
import numpy as np
from contextlib import ExitStack, contextmanager
import concourse.bass as bass
import concourse.mybir as mybir
from concourse.bass_utils import run_bass_kernel_spmd


def pbc(ap):
    b = ap.partition_broadcast(128)
    if len(b.shape) == 3 and b.shape[1] == 1:
        return b[:, 0, :]
    return b


F32 = mybir.dt.float32
BF16 = mybir.dt.bfloat16
I32 = mybir.dt.int32
AF = mybir.ActivationFunctionType
ALU = mybir.AluOpType
AX = mybir.AxisListType

ENGS = ("pe", "act", "dve", "pool", "sp")
EPOCH = 50000
NDMASEM = 4
DEPOCH = 2000


class Buf:
    __slots__ = ("name", "w", "r")

    def __init__(self, name):
        self.name = name
        self.w = None
        self.r = []


class Sched:
    def __init__(self, nc, stack):
        self.nc = nc
        self.stack = stack
        self.eng = {"pe": nc.tensor, "act": nc.scalar, "dve": nc.vector, "pool": nc.gpsimd, "sp": nc.sync}
        self.q = {e: [] for e in ENGS}
        self.cnt = {e: 0 for e in ENGS}
        self.sems = {e: [] for e in ENGS}
        self.dcnt = {e: 0 for e in ENGS}
        self.dsems = {e: None for e in ENGS}
        self.seen = {e: {} for e in ENGS}
        self.bufs = {}

    def buf(self, name):
        b = self.bufs.get(name)
        if b is None:
            b = self.bufs[name] = Buf(name)
        return b

    def _sem(self, name):
        return self.stack.enter_context(self.nc.semaphore(name))

    def _esem(self, e, epoch):
        while len(self.sems[e]) <= epoch:
            self.sems[e].append(self._sem(f"s_{e}_{len(self.sems[e])}"))
        return self.sems[e][epoch]

    def _dsem(self, e, n):
        if self.dsems[e] is None:
            self.dsems[e] = []
        setsz = NDMASEM * DEPOCH
        si, r = divmod(n, setsz)
        while len(self.dsems[e]) <= si:
            k = len(self.dsems[e])
            self.dsems[e].append([self._sem(f"d_{e}_{k}_{j}") for j in range(NDMASEM)])
        return self.dsems[e][si][r % NDMASEM], 16 * (r // NDMASEM + 1)

    def _wait(self, e, ev):
        if ev is None:
            return
        kind, src, n = ev
        if kind == "c":
            if src == e and e == "pe":
                return
            key = ("c", src)
            if self.seen[e].get(key, 0) >= n:
                return
            self.seen[e][key] = n
            ep, loc = divmod(n - 1, EPOCH)
            sem = self._esem(src, ep)
            val = loc + 1
        else:
            key = ("d", src, n % NDMASEM)
            if self.seen[e].get(key, -1) >= n:
                return
            self.seen[e][key] = n
            sem, val = self._dsem(src, n)
        self.q[e].append(lambda eng, sem=sem, val=val: eng.wait_ge(sem, val))

    def _deps(self, e, reads, writes):
        for b in reads:
            if b.w is not None:
                self._wait(e, b.w)
        for b in writes:
            if b.w is not None:
                self._wait(e, b.w)
            for ev in b.r:
                self._wait(e, ev)

    def _commit(self, ev, reads, writes):
        for b in reads:
            b.r.append(ev)
            if len(b.r) > 64:
                b.r = b.r[-64:] if False else b.r
        for b in writes:
            b.w = ev
            b.r = []

    def op(self, e, fn, reads=(), writes=()):
        reads = [self.buf(b) if isinstance(b, str) else b for b in reads]
        writes = [self.buf(b) if isinstance(b, str) else b for b in writes]
        self._deps(e, reads, writes)
        self.cnt[e] += 1
        n = self.cnt[e]
        ep, loc = divmod(n - 1, EPOCH)
        sem = self._esem(e, ep)
        import sys as _sys
        fr = _sys._getframe(1)
        tag = []
        while fr is not None and len(tag) < 4:
            tag.append(f"{fr.f_code.co_name}:{fr.f_lineno}")
            fr = fr.f_back

        def _emit(eng, fn=fn, sem=sem, tag=tag):
            try:
                fn(eng).then_inc(sem, 1)
            except Exception:
                print("EMIT FAILED at", tag, flush=True)
                raise
        self.q[e].append(_emit)
        self._commit(("c", e, n), reads, writes)

    def dma(self, e, out, in_, reads=(), writes=(), **kw):
        reads = [self.buf(b) if isinstance(b, str) else b for b in reads]
        writes = [self.buf(b) if isinstance(b, str) else b for b in writes]
        self._deps(e, reads, writes)
        n = self.dcnt[e]
        self.dcnt[e] += 1
        if n >= NDMASEM:
            self._wait(e, ("d", e, n - NDMASEM))
        sem, _v = self._dsem(e, n)
        self.q[e].append(lambda eng, out=out, in_=in_, sem=sem, kw=kw: eng.dma_start(out=out, in_=in_, **kw).then_inc(sem, 16))
        self._commit(("d", e, n), reads, writes)

    def dma_gather(self, out, in_, idx_ap, reads=(), writes=()):
        e = "pool"
        reads = [self.buf(b) if isinstance(b, str) else b for b in reads]
        writes = [self.buf(b) if isinstance(b, str) else b for b in writes]
        self._deps(e, reads, writes)
        n = self.dcnt[e]
        self.dcnt[e] += 1
        if n >= NDMASEM:
            self._wait(e, ("d", e, n - NDMASEM))
        sem, _v = self._dsem(e, n)
        self.q[e].append(lambda eng, out=out, in_=in_, idx_ap=idx_ap, sem=sem: eng.indirect_dma_start(
            out=out, out_offset=None, in_=in_, in_offset=bass.IndirectOffsetOnAxis(ap=idx_ap, axis=0)).then_inc(sem, 16))
        self._commit(("d", e, n), reads, writes)

    def finish(self, final_bufs):
        for b in final_bufs:
            b = self.buf(b) if isinstance(b, str) else b
            self._wait("sp", b.w)
        for e in ENGS:
            for n in range(max(0, self.dcnt[e] - NDMASEM), self.dcnt[e]):
                self._wait("sp", ("d", e, n))
        for e in ENGS:
            if e != "sp" and self.cnt[e] > 0:
                self._wait("sp", ("c", e, self.cnt[e]))

    def barrier(self):
        for e in ENGS:
            for f in ENGS:
                if f != e and self.cnt[f] > 0:
                    self._wait(e, ("c", f, self.cnt[f]))
            for f in ENGS:
                for n in range(max(0, self.dcnt[f] - NDMASEM), self.dcnt[f]):
                    self._wait(e, ("d", f, n))

    def emit(self):
        nc = self.nc
        if not any(self.q[e] for e in ENGS):
            return
        with nc.Block() as block:
            @block.tensor
            def _(eng):
                for f in self.q["pe"]:
                    f(eng)

            @block.scalar
            def _(eng):
                for f in self.q["act"]:
                    f(eng)

            @block.vector
            def _(eng):
                for f in self.q["dve"]:
                    f(eng)

            @block.gpsimd
            def _(eng):
                for f in self.q["pool"]:
                    f(eng)

            @block.sync
            def _(eng):
                for f in self.q["sp"]:
                    f(eng)
        self.q = {e: [] for e in ENGS}


D = 1024
FF = 3584
NE = 8
NIN = 7712
EPS = 1e-6
OFF = dict(hg_q=0, hg_ff=512, hg_fb=1024, hg_i=1536, hg_g=2048, gl_q=2560, gl_k=2816, gl_v=3072,
           gl_df=3584, gl_db=3600, gl_r=3616, s5_u=4128, ga=4640, gb=5664, gc=6688)
TWO_PI = 6.283185307179586
PI_SAFE = 3.1415925

WSPECS = [("norm1_g", [2, D]), ("norm2_g", [2, D]), ("ada_w", [2, D, 6 * D]), ("ada_b", [2, 6 * D]),
          ("w_in", [2, D, NIN]), ("hg_lb_logits", [2, 512]), ("hg_norm_g", [2, 512]),
          ("gla_gate_up", [2, 2, 16, 256]), ("gla_gate_b", [2, 2, 256]), ("gla_norm_g", [2, 512]),
          ("s5_a_re", [2, 2, 32, 64]), ("s5_a_im", [2, 2, 32, 64]), ("s5_log_step", [2, 2, 32]),
          ("s5_b_re", [2, 2, 32, 64, 16]), ("s5_b_im", [2, 2, 32, 64, 16]),
          ("s5_c_re", [2, 2, 32, 16, 64]), ("s5_c_im", [2, 2, 32, 16, 64]),
          ("s5_d", [2, 512]), ("s5_glu_w", [2, 512, 512]), ("branch_proj", [2, 3, 512, D]),
          ("w_out", [2, D, D]), ("ffn_w1", [1, D, FF]), ("ffn_w3", [1, D, FF]), ("ffn_w2", [1, FF, D]),
          ("router_w", [1, D, NE]), ("moe_w1", [1, NE, D, FF]), ("moe_w3", [1, NE, D, FF]),
          ("moe_w2", [1, NE, FF, D]), ("final_norm_g", [D])]


class KB:
    def __init__(self, nct=2, nlt=32, dumps=(), stop=None):
        self.nct, self.nlt = nct, nlt
        self.NT = nct + nlt
        self.T = self.NT * 128
        self.dumps = set(dumps)
        self.stop = stop
        self.nc = nc = bass.Bass("TRN2", target_bir_lowering=False)
        self.st = ExitStack()
        self.S = Sched(nc, self.st)
        self.t = {}
        din = lambda name, shape: nc.dram_tensor(name, list(shape), F32, kind="ExternalInput").ap()
        self.xin = din("xin", [self.T, D])
        self.ccol = din("ccol", [128, 8])
        self.cctxcol = din("cctxcol", [128, 8])
        self.w = {name: din(name, shape) for name, shape in WSPECS}
        self.NLOC = nlt // 2
        self.idx_in = nc.dram_tensor("idx", [128, self.NLOC], I32, kind="ExternalInput").ap()
        self.out = nc.dram_tensor("out", [self.NLOC * 128, D], F32, kind="ExternalOutput").ap()
        self.scr = {}

    def dscr(self, name, shape, dt=F32):
        kind = "ExternalOutput" if name in self.dumps else "Internal"
        ap = self.nc.dram_tensor(name, list(shape), dt, kind=kind).ap()
        self.scr[name] = ap
        return ap

    def sb(self, stack, name, shape, dt=F32):
        self._uid = getattr(self, "_uid", 0) + 1
        h = stack.enter_context(self.nc.sbuf_tensor(f"{name}_u{self._uid}", list(shape), dt))
        self.t[name] = h
        if hasattr(stack, "_names"):
            stack._names.append(name)
        return h

    @contextmanager
    def phase(self):
        with ExitStack() as ph:
            ph._names = []
            yield ph
            self.phase_end()
            for n in ph._names:
                self.t.pop(n, None)

    def mm(self, out, lhsT, rhs, r, w, start=True, stop=True):
        self.S.op("pe", lambda e: e.matmul(out, lhsT=lhsT, rhs=rhs, start=start, stop=stop), r, w)

    def tr(self, out, in_, r, w):
        ident = self.t["ident"]
        self.S.op("pe", lambda e: e.transpose(out, in_, ident[:]), list(r) + ["ident"], w)

    def act(self, out, in_, func, r, w, bias=None, scale=None, accum=None):
        kw = {}
        if bias is not None:
            kw["bias"] = bias
        if scale is not None:
            kw["scale"] = scale
        if accum is not None:
            kw["accum_out"] = accum
        self.S.op("act", lambda e: e.activation(out=out, in_=in_, func=func, **kw), r, w)

    def tt(self, eng, out, in0, in1, op, r, w):
        self.S.op(eng, lambda e: e.tensor_tensor(out=out, in0=in0, in1=in1, op=op), r, w)

    def ts(self, eng, out, in0, s1, op0, r, w, s2=None, op1=None):
        if op1 is None:
            self.S.op(eng, lambda e: e.tensor_scalar(out=out, in0=in0, scalar1=s1, scalar2=None, op0=op0), r, w)
        else:
            self.S.op(eng, lambda e: e.tensor_scalar(out=out, in0=in0, scalar1=s1, scalar2=s2, op0=op0, op1=op1), r, w)

    def stt(self, out, in0, scalar, in1, op0, op1, r, w, accum=None):
        if accum is None:
            self.S.op("dve", lambda e: e.scalar_tensor_tensor(out=out, in0=in0, scalar=scalar, in1=in1, op0=op0, op1=op1), r, w)
        else:
            self.S.op("dve", lambda e: e.scalar_tensor_tensor(out=out, in0=in0, scalar=scalar, in1=in1, op0=op0, op1=op1, accum_out=accum), r, w)

    def cp(self, eng, out, in_, r, w):
        if eng == "act":
            self.S.op("act", lambda e: e.activation(out=out, in_=in_, func=AF.Identity), r, w)
        else:
            self.S.op(eng, lambda e: e.tensor_copy(out=out, in_=in_), r, w)

    def memset(self, eng, ap, val, w):
        self.S.op(eng, lambda e: e.memset(ap, val), (), w)

    def dma(self, q, out, in_, r, w, **kw):
        self.S.dma(q, out, in_, r, w, **kw)

    def phase_end(self):
        self.S.barrier()
        self.S.emit()

    def is_ctx(self, ti):
        return ti < self.nct

    def setup(self):
        S, t, st = self.S, self.t, self.st
        sb = lambda name, shape, dt=F32: self.sb(st, name, shape, dt)
        for i in range(8):
            h = st.enter_context(self.nc.psum_tensor(f"P{i}", [128, 512], F32))
            t[f"P{i}"] = h
        self.dscr("X", [self.T, D])
        di = sb("c_di", [128, 128], I32)
        df = sb("c_df", [128, 128])
        S.op("pool", lambda e: e.iota(di[:], pattern=[[1, 128]], base=0, channel_multiplier=-1), (), ["c_di"])
        self.cp("dve", df[:], di[:], ["c_di"], ["c_df"])
        for name, op in (("ident", ALU.is_equal), ("minc0", ALU.is_ge), ("minc1", ALU.is_le),
                         ("mstr0", ALU.is_lt), ("mstr1", ALU.is_gt)):
            h = sb(name, [128, 128])
            S.op("dve", lambda e, h=h, op=op: e.tensor_single_scalar(out=h[:], in_=df[:], scalar=0.0, op=op), ["c_df"], [name])
        for d in (0, 1):
            h = sb(f"mincb{d}", [128, 128], BF16)
            self.cp("dve", h[:], t[f"minc{d}"][:], [f"minc{d}"], [f"mincb{d}"])
        for hh in (0, 1):
            hm = sb(f"hm{hh}", [128, 1])
            self.memset("pool", hm[:], 0.0, [f"hm{hh}"])
            self.memset("pool", hm[hh * 64:(hh + 1) * 64, :], 1.0, [f"hm{hh}"])
        ones = sb("ones", [128, 128])
        self.memset("pool", ones[:], 1.0, ["ones"])
        for k in range(4):
            m = sb(f"mask{k}", [128, 128])
            self.memset("pool", m[:], 0.0, [f"mask{k}"])
            self.memset("pool", m[0:64, (2 * k) * 16:(2 * k + 1) * 16], 1.0, [f"mask{k}"])
            self.memset("pool", m[64:128, (2 * k + 1) * 16:(2 * k + 2) * 16], 1.0, [f"mask{k}"])
        lg = self.w["hg_lb_logits"]
        for nm, shape in (("fm", [128, 4]), ("tm", [128, 512])):
            for l in (0, 1):
                sb(f"lb_{nm}{l}", shape)
                sb(f"oml_{nm}{l}", shape)
        with self.phase() as ph0:
            lfm = self.sb(ph0, "lb_lfm", [128, 2, 4])
            self.dma("sp", lfm[:], lg.rearrange("l (h p) -> p l h", p=128), (), ["lb_lfm"], allow_slow_non_contiguous=True)
            ltm = self.sb(ph0, "lb_ltm", [128, 2, 512])
            self.dma("sp", ltm[:], lg.partition_broadcast(128), (), ["lb_ltm"])
            for nm, shape, src in (("fm", [128, 4], lfm), ("tm", [128, 512], ltm)):
                self.memset("pool", t[f"lb_{nm}0"][:], 0.0, [f"lb_{nm}0"])
                self.memset("pool", t[f"oml_{nm}0"][:], 1.0, [f"oml_{nm}0"])
                dtile = self.sb(ph0, f"lb_d{nm}", shape)
                self.tt("dve", dtile[:], src[:, 1, :], src[:, 0, :], ALU.subtract, [f"lb_l{nm}"], [f"lb_d{nm}"])
                self.act(t[f"lb_{nm}1"][:], dtile[:], AF.Sigmoid, [f"lb_d{nm}"], [f"lb_{nm}1"])
                self.act(t[f"oml_{nm}1"][:], dtile[:], AF.Sigmoid, [f"lb_d{nm}"], [f"oml_{nm}1"], scale=-1.0)
        with self.phase() as ph:
            psb = lambda name, shape, dt=F32: self.sb(ph, name, shape, dt)
            ji = psb("pe_ji", [128, 256], I32)
            om2 = psb("pe_om2", [128, 512])
            ph2 = psb("pe_ph", [128, 512])
            S.op("pool", lambda e: e.iota(ji[:], pattern=[[1, 256]], base=0, channel_multiplier=0), (), ["pe_ji"])
            self.cp("dve", om2[:, 0:256], ji[:], ["pe_ji"], ["pe_om2"])
            self.act(om2[:, 0:256], om2[:, 0:256], AF.Exp, ["pe_om2"], ["pe_om2"], scale=-math.log(10000.0) / 256.0)
            self.cp("dve", om2[:, 256:512], om2[:, 0:256], ["pe_om2"], ["pe_om2"])
            self.memset("pool", ph2[:, 0:256], 0.0, ["pe_ph"])
            self.memset("pool", ph2[:, 256:512], math.pi / 2, ["pe_ph"])
            pi_ = psb("pe_pi", [128, 1], I32)
            pf = psb("pe_pf", [128, 1])
            hi = psb("pe_hi", [128, 1])
            cpos = psb("pe_c", [128, 1])
            S.op("pool", lambda e: e.iota(pi_[:], pattern=[[0, 1]], base=0, channel_multiplier=1), (), ["pe_pi"])
            self.cp("dve", pf[:], pi_[:], ["pe_pi"], ["pe_pf"])
            self.ts("dve", hi[:], pf[:], 64.0, ALU.is_ge, ["pe_pf"], ["pe_hi"])
            self.stt(cpos[:], hi[:], -64.0, pf[:], ALU.mult, ALU.add, ["pe_hi", "pe_pf"], ["pe_c"])

            def sincos(dst, scal, tag):
                a = t["pe_a"]
                b = t["pe_b"]
                bi = t["pe_bi"]
                self.stt(a[:], om2[:], scal, ph2[:], ALU.mult, ALU.add, ["pe_om2", "pe_ph", tag], ["pe_a"])
                self.ts("dve", b[:], a[:], 1.0 / TWO_PI, ALU.mult, ["pe_a"], ["pe_b"])
                self.cp("dve", bi[:], b[:], ["pe_b"], ["pe_bi"])
                self.cp("dve", b[:], bi[:], ["pe_bi"], ["pe_b"])
                self.stt(a[:], b[:], -TWO_PI, a[:], ALU.mult, ALU.add, ["pe_b", "pe_a"], ["pe_a"])
                self.ts("dve", a[:], a[:], PI_SAFE, ALU.min, ["pe_a"], ["pe_a"], s2=-PI_SAFE, op1=ALU.max)
                self.act(dst, a[:], AF.Sin, ["pe_a"], [tag + "_o"])

            psb("pe_a", [128, 512])
            psb("pe_b", [128, 512])
            psb("pe_bi", [128, 512], I32)
            colp = psb("pe_col", [128, 512])
            S.buf("pe_c_o")
            sincos(colp[:], cpos[:], "pe_c")
            rsc = psb("pe_r", [128, 1])
            for ti in range(self.NT):
                xt = psb(f"pe_x{ti % 2}", [128, D]) if ti < 2 else t[f"pe_x{ti % 2}"]
                nm = f"pe_x{ti % 2}"
                self.dma("sp", xt[:], self.xin[ti * 128:(ti + 1) * 128, :], (), [nm])
                if not self.is_ctx(ti):
                    li = ti - self.nct
                    rowp = psb(f"pe_row{ti % 2}", [128, 512]) if f"pe_row{ti % 2}" not in t else t[f"pe_row{ti % 2}"]
                    self.ts("dve", rsc[:], hi[:], float(2 * li), ALU.add, ["pe_hi"], ["pe_r"])
                    S.buf("pe_r_o")
                    sincos(rowp[:], rsc[:], "pe_r")
                    self.tt("dve", xt[:, 0:512], xt[:, 0:512], rowp[:], ALU.add, [nm, "pe_r_o"], [nm])
                    self.tt("pool", xt[:, 512:1024], xt[:, 512:1024], colp[:], ALU.add, [nm, "pe_c_o"], [nm])
                self.dma("pool", self.scr["X"][ti * 128:(ti + 1) * 128, :], xt[:], [nm], [f"X@{ti}"])

    def mod_phase(self, l):
        S, t = self.S, self.t
        if "MODS" not in self.scr:
            self.dscr("MODS", [2, 2, 6 * D])
        with self.phase() as ph:
            psb = lambda name, shape, dt=F32: self.sb(ph, name, shape, dt)
            cb = {}
            for v, src in ((0, self.ccol), (1, self.cctxcol)):
                c = psb(f"md_c{v}", [128, 8])
                self.dma("sp", c[:], src, (), [f"md_c{v}"])
                self.act(c[:], c[:], AF.Silu, [f"md_c{v}"], [f"md_c{v}"])
                cbt = psb(f"md_cb{v}", [128, 8, 128])
                self.cp("dve", cbt[:], c[:].unsqueeze(2).to_broadcast([128, 8, 128]), [f"md_c{v}"], [f"md_cb{v}"])
                cb[v] = cbt
            bb = psb("md_b", [128, 6 * D])
            self.dma("sp", bb[:], pbc(self.w["ada_b"][l:l + 1, :]), (), ["md_b"])
            wsrc = self.w["ada_w"][l].rearrange("(kc p) n -> p kc n", p=128)
            for cbk in range(12):
                wn = f"md_w{cbk % 2}"
                wt = psb(wn, [128, 8, 512]) if wn not in t else t[wn]
                self.dma("sp", wt[:], wsrc[:, :, cbk * 512:(cbk + 1) * 512], (), [wn])
                for v in (0, 1):
                    pn = f"P{v}"
                    for kc in range(8):
                        self.mm(t[pn][:], cb[v][:, kc, :], wt[:, kc, :], [f"md_cb{v}", wn], [pn], start=(kc == 0), stop=(kc == 7))
                    on = f"md_o{v}"
                    ot = psb(on, [128, 512]) if on not in t else t[on]
                    self.tt("dve", ot[:], t[pn][:], bb[:, cbk * 512:(cbk + 1) * 512], ALU.add, [pn, "md_b"], [on])
                    self.dma("pool", self.scr["MODS"][l, v:v + 1, cbk * 512:(cbk + 1) * 512], ot[0:1, :], [on], [f"MODS@{l}"])

    def load_mod(self, stack, l, which, tag):
        for v in (0, 1):
            h = self.sb(stack, f"{tag}{v}", [128, D])
            self.dma("sp", h[:], pbc(self.scr["MODS"][l, v:v + 1, which * D:(which + 1) * D]),
                     [f"MODS@{l}"], [f"{tag}{v}"])

    def norm_phase(self, stack, l, second, tiles, router=False, local=None):
        hT = self.sb(stack, "hT", [128, 8, self.T if local is None else self.NLOC * 128], BF16)
        with self.phase() as inner:
            self._norm_phase(inner, l, second, tiles, router, hT, local)
        return hT

    def _norm_phase(self, stack, l, second, tiles, router, hT, local=None):
        S, t = self.S, self.t
        psb = lambda name, shape, dt=F32: self.sb(stack, name, shape, dt)
        pre = "n2" if second else "n1"
        self.load_mod(stack, l, 4 if second else 1, pre + "sc")
        self.load_mod(stack, l, 3 if second else 0, pre + "sh")
        gn = psb(pre + "g", [128, D])
        gsrc = self.w["norm2_g" if second else "norm1_g"]
        self.dma("sp", gn[:], pbc(gsrc[l:l + 1, :]), (), [pre + "g"])
        for v in (0, 1):
            self.stt(t[f"{pre}sc{v}"][:], t[f"{pre}sc{v}"][:], 1.0, gn[:], ALU.add, ALU.mult, [f"{pre}sc{v}", pre + "g"], [f"{pre}sc{v}"])
        _sk = ""
        if router:
            wr = psb("rt_w", [128, 8, 128])
            self.memset("pool", wr[:], 0.0, ["rt_w"])
            if "noW" not in _sk:
                self.dma("sp", wr[:, :, 0:NE], self.w["router_w"][0].rearrange("(kc p) e -> p kc e", p=128), (), ["rt_w"])
            pass
        if local is not None:
            idxt = psb("n_idx", [128, self.NLOC], I32)
            self.dma("sp", idxt[:], self.idx_in, (), ["n_idx"])
        for ti in tiles:
            v = 1 if (local is None and self.is_ctx(ti)) else 0
            xn = f"nx{ti % 2}"
            xt = psb(xn, [128, D]) if xn not in t else t[xn]
            if local is None:
                self.dma("sp", xt[:], self.scr["X"][ti * 128:(ti + 1) * 128, :], [f"X@{ti}"], [xn])
            else:
                self.S.dma_gather(xt[:], self.scr["X"], idxt[:, ti:ti + 1], ["n_idx"] + [f"X@{k}" for k in range(self.NT)], [xn])
                self.dma("sp", self.scr["XL"][ti * 128:(ti + 1) * 128, :], xt[:], [xn], [f"XL@{ti}"])
            jn = f"njunk{ti % 2}"
            junk = psb(jn, [128, D]) if jn not in t else t[jn]
            sn = f"nss{ti % 2}"
            ss = psb(sn, [128, 1]) if sn not in t else t[sn]
            self.act(junk[:], xt[:], AF.Square, [xn], [jn, sn], accum=ss[:])
            self.ts("dve", ss[:], ss[:], 1.0 / D, ALU.mult, [sn], [sn], s2=EPS, op1=ALU.add)
            self.act(ss[:], ss[:], AF.Sqrt, [sn], [sn])
            S.op("dve", lambda e, ss=ss: e.reciprocal(out=ss[:], in_=ss[:]), [sn], [sn])
            self.stt(junk[:], xt[:], ss[:], t[f"{pre}sc{v}"][:], ALU.mult, ALU.mult, [xn, sn, f"{pre}sc{v}"], [jn])
            self.tt("pool", junk[:], junk[:], t[f"{pre}sh{v}"][:], ALU.add, [jn, f"{pre}sh{v}"], [jn])
            for half in range(2):
                pn = f"P{half}"
                for k4 in range(4):
                    kc = half * 4 + k4
                    self.tr(t[pn][:, k4 * 128:(k4 + 1) * 128], junk[:, kc * 128:(kc + 1) * 128], [jn], [pn])
                eng = "act" if half == 0 else "dve"
                if router:
                    hn = f"rt_h{half}"
                    hf = psb(hn, [128, 4, 128]) if hn not in t else t[hn]
                    self.cp(eng, hf[:], t[pn][:].rearrange("p (k c) -> p k c", k=4), [pn], [hn])
                    self.cp("pool", hT[:, half * 4:(half + 1) * 4, ti * 128:(ti + 1) * 128], hf[:], [hn], ["hT"])
                else:
                    self.cp(eng, hT[:, half * 4:(half + 1) * 4, ti * 128:(ti + 1) * 128],
                            t[pn][:].rearrange("p (k c) -> p k c", k=4), [pn], ["hT"])
            if router and "rtA" in "":
                continue
            if router:
                for kc in range(8):
                    self.mm(t["P2"][:, 0:128], t[f"rt_h{kc // 4}"][:, kc % 4, :], t["rt_w"][:, kc, :],
                            [f"rt_h{kc // 4}", "rt_w"], ["P2"], start=(kc == 0), stop=(kc == 7))
                self.router_post(stack, ti, local)
        return hT

    def router_post(self, stack, ti, cmbt):
        S, t = self.S, self.t
        import os as _os
        _sk = ""
        psb = lambda name, shape, dt=F32: (self.sb(stack, name, shape, dt) if name not in t else t[name])
        lgt = psb("rt_lg", [128, NE])
        m1 = psb("rt_m1", [128, 1])
        m2 = psb("rt_m2", [128, 1])
        eq = psb("rt_eq", [128, NE])
        ex = psb("rt_ex", [128, NE])
        den = psb("rt_den", [128, 1])
        self.cp("dve", lgt[:], t["P2"][:, 0:NE], ["P2"], ["rt_lg"])
        if "rt1" in _sk:
            return
        S.op("dve", lambda e: e.reduce_max(out=m1[:], in_=lgt[:], axis=AX.X), ["rt_lg"], ["rt_m1"])
        self.ts("dve", eq[:], lgt[:], m1[:], ALU.is_equal, ["rt_lg", "rt_m1"], ["rt_eq"])
        self.stt(eq[:], eq[:], -1e30, lgt[:], ALU.mult, ALU.add, ["rt_eq", "rt_lg"], ["rt_eq"])
        S.op("dve", lambda e: e.reduce_max(out=m2[:], in_=eq[:], axis=AX.X), ["rt_eq"], ["rt_m2"])
        self.ts("dve", eq[:], lgt[:], m2[:], ALU.is_ge, ["rt_lg", "rt_m2"], ["rt_eq"])
        self.ts("dve", m1[:], m1[:], -1.0, ALU.mult, ["rt_m1"], ["rt_m1"])
        self.act(ex[:], lgt[:], AF.Exp, ["rt_lg", "rt_m1"], ["rt_ex"], bias=m1[:])
        self.tt("dve", ex[:], ex[:], eq[:], ALU.mult, ["rt_ex", "rt_eq"], ["rt_ex"])
        S.op("dve", lambda e: e.reduce_sum(out=den[:], in_=ex[:], axis=AX.X), ["rt_ex"], ["rt_den"])
        S.op("dve", lambda e: e.reciprocal(out=den[:], in_=den[:]), ["rt_den"], ["rt_den"])
        self.ts("dve", cmbt[:, ti, :], ex[:], den[:], ALU.mult, ["rt_ex", "rt_den"], ["ff_cmbt"])


class KB2(KB):
    def proj_phase(self, stack, l):
        S, t = self.S, self.t
        T, NT = self.T, self.NT
        psb = lambda name, shape, dt=F32: (self.sb(stack, name, shape, dt) if name not in t else t[name])
        hT = t["hT"]
        for name, rows in (("QH", 512), ("KHF", 512), ("KHB", 512), ("GH", 512), ("QG", 256), ("KG", 256),
                           ("RG", 512), ("U5", 512), ("GA", D), ("GB", D), ("GC", D)):
            if name not in self.scr:
                self.dscr(name, [rows, T])
        for name, cols, dt in (("GHF", 512, F32), ("GHB", 512, F32), ("KHFt", 512, F32), ("KHBt", 512, F32),
                               ("VH", 512, BF16), ("KGt", 256, F32), ("VG", 512, BF16), ("GG", 512, F32)):
            if name not in self.scr:
                self.dscr(name, [T, cols], dt)
        wsrc = self.w["w_in"][l].rearrange("(kc p) n -> p kc n", p=128)
        tbs = [(s, min(512, T - s)) for s in range(0, T, 512)]
        self._wcnt = getattr(self, "_wcnt", 0)

        def load_w(c0, ncol):
            i = self._wcnt % 2
            self._wcnt += 1
            wf = psb(f"pw_f{i}", [128, 8, 512])
            wb = psb(f"pw_b{i}", [128, 8, 512], BF16)
            self.dma("sp", wf[:, :, 0:ncol], wsrc[:, :, c0:c0 + ncol], (), [f"pw_f{i}"])
            self.cp("pool", wb[:, :, 0:ncol], wf[:, :, 0:ncol], [f"pw_f{i}"], [f"pw_b{i}"])
            return wb, f"pw_b{i}"

        self._pcnt = getattr(self, "_pcnt", 0)

        def next_p():
            i = self._pcnt % 4
            self._pcnt += 1
            return t[f"P{i}"], f"P{i}"

        self._ocnt = getattr(self, "_ocnt", 0)

        def next_o(dt=F32):
            i = self._ocnt % 3
            self._ocnt += 1
            nm = f"po_{'b' if dt == BF16 else 'f'}{i}"
            return psb(nm, [128, 512], dt), nm

        lb, oml = t[f"lb_fm{l}"], t[f"oml_fm{l}"]
        lbt, omlt = t[f"lb_tm{l}"], t[f"oml_tm{l}"]

        fm = [(OFF["hg_q"], 512, "QH", "copy"), (OFF["hg_ff"], 512, "KHF", "hk"), (OFF["hg_fb"], 512, "KHB", "hk"),
              (OFF["hg_g"], 512, "GH", "silu"), (OFF["gl_q"], 256, "QG", "q8"), (OFF["gl_k"], 256, "KG", "copy"),
              (OFF["gl_r"], 512, "RG", "silu"), (OFF["s5_u"], 512, "U5", "copy"),
              (OFF["ga"], D, "GA", "sig"), (OFF["gb"], D, "GB", "sig"), (OFF["gc"], D, "GC", "sig")]
        for c0, ncols, sname, kind in fm:
            for cb0 in range(0, ncols, 512):
                nb = min(512, ncols - cb0)
                wb, wn = load_w(c0 + cb0, nb)
                for sub in range(nb // 128):
                    row0 = cb0 + sub * 128
                    for (s0, sl) in tbs:
                        P, pn = next_p()
                        for kc in range(8):
                            self.mm(P[:, 0:sl], wb[:, kc, sub * 128:(sub + 1) * 128], hT[:, kc, s0:s0 + sl],
                                    [wn, "hT"], [pn], start=(kc == 0), stop=(kc == 7))
                        o, on = next_o()
                        if kind == "copy":
                            self.cp("act", o[:, 0:sl], P[:, 0:sl], [pn], [on])
                        elif kind == "q8":
                            self.act(o[:, 0:sl], P[:, 0:sl], AF.Identity, [pn], [on], scale=0.125)
                        elif kind == "silu":
                            self.act(o[:, 0:sl], P[:, 0:sl], AF.Silu, [pn], [on])
                        elif kind == "sig":
                            self.act(o[:, 0:sl], P[:, 0:sl], AF.Sigmoid, [pn], [on])
                        elif kind == "hk":
                            h = row0 // 128
                            self.act(o[:, 0:sl], P[:, 0:sl], AF.Sigmoid, [pn], [on], scale=-1.0)
                            self.ts("dve", o[:, 0:sl], o[:, 0:sl], oml[:, h:h + 1], ALU.mult, [on, f"oml_fm{l}"], [on])
                        self.dma("sp", self.scr[sname][row0:row0 + 128, s0:s0 + sl], o[:, 0:sl], [on],
                                 [f"{sname}@{s0 // 128 + k}" for k in range(sl // 128)])
        tm = [(OFF["hg_ff"], 512, "hgf", ("GHF", "KHFt")), (OFF["hg_fb"], 512, "hgf", ("GHB", "KHBt")),
              (OFF["hg_i"], 512, "vbf", ("VH",)), (OFF["gl_k"], 256, "copy", ("KGt",)), (OFF["gl_v"], 512, "vbf", ("VG",))]
        for c0, ncols, kind, snames in tm:
            wb, wn = load_w(c0, ncols)
            for ti in range(NT):
                P, pn = next_p()
                for kc in range(8):
                    self.mm(P[:, 0:ncols], hT[:, kc, ti * 128:(ti + 1) * 128], wb[:, kc, 0:ncols], [wn, "hT"], [pn],
                            start=(kc == 0), stop=(kc == 7))
                rows = slice(ti * 128, (ti + 1) * 128)
                if kind == "copy":
                    o, on = next_o()
                    self.cp("act", o[:, 0:ncols], P[:, 0:ncols], [pn], [on])
                    self.dma("sp", self.scr[snames[0]][rows, :], o[:, 0:ncols], [on], [f"{snames[0]}@{ti}"])
                elif kind == "vbf":
                    o, on = next_o(BF16)
                    self.cp("act", o[:, 0:ncols], P[:, 0:ncols], [pn], [on])
                    self.dma("sp", self.scr[snames[0]][rows, :], o[:, 0:ncols], [on], [f"{snames[0]}@{ti}"])
                else:
                    o, on = next_o()
                    o2, on2 = next_o()
                    self.act(o[:], P[:], AF.Sigmoid, [pn], [on])
                    self.tt("dve", o[:], o[:], omlt[:], ALU.mult, [on, f"oml_tm{l}"], [on])
                    self.tt("dve", o[:], o[:], lbt[:], ALU.add, [on, f"lb_tm{l}"], [on])
                    self.act(o[:], o[:], AF.Ln, [on], [on])
                    self.dma("sp", self.scr[snames[0]][rows, :], o[:], [on], [f"{snames[0]}@{ti}"])
                    self.act(o2[:], P[:], AF.Sigmoid, [pn], [on2], scale=-1.0)
                    self.tt("dve", o2[:], o2[:], omlt[:], ALU.mult, [on2, f"oml_tm{l}"], [on2])
                    self.dma("sp", self.scr[snames[1]][rows, :], o2[:], [on2], [f"{snames[1]}@{ti}"])
        upf = psb("pg_upf", [33, 512])
        upb = psb("pg_upb", [33, 512], BF16)
        self.memset("pool", upf[:], 0.0, ["pg_upf"])
        self.dma("sp", upf[0:16, 0:256], self.w["gla_gate_up"][l, 0], (), ["pg_upf"])
        self.dma("sp", upf[16:32, 256:512], self.w["gla_gate_up"][l, 1], (), ["pg_upf"])
        self.dma("sp", upf[32:33, :], self.w["gla_gate_b"][l:l + 1].rearrange("o d k -> o (d k)"), (), ["pg_upf"])
        self.cp("dve", upb[:], upf[:], ["pg_upf"], ["pg_upb"])
        wb, wn = load_w(OFF["gl_df"], 32)
        for (s0, sl) in tbs:
            P, pn = next_p()
            for kc in range(8):
                self.mm(P[0:32, 0:sl], wb[:, kc, 0:32], hT[:, kc, s0:s0 + sl], [wn, "hT"], [pn], start=(kc == 0), stop=(kc == 7))
            i = (s0 // 512) % 2
            pdt = psb(f"pg_pd{i}", [33, 512], BF16)
            if s0 < 1024:
                self.memset("pool", pdt[32:33, :], 1.0, [f"pg_pd{i}"])
            self.cp("act", pdt[0:32, 0:sl], P[0:32, 0:sl], [pn], [f"pg_pd{i}"])
            for k in range(sl // 128):
                ti = s0 // 128 + k
                P2, pn2 = next_p()
                self.mm(P2[:], pdt[0:33, k * 128:(k + 1) * 128], upb[0:33, :], [f"pg_pd{i}", "pg_upb"], [pn2])
                o, on = next_o()
                self.act(o[:], P2[:], AF.Sigmoid, [pn2], [on])
                self.act(o[:], o[:], AF.Ln, [on], [on])
                self.ts("dve", o[:], o[:], 1.0 / 16.0, ALU.mult, [on], [on])
                self.dma("sp", self.scr["GG"][ti * 128:(ti + 1) * 128, :], o[:], [on], [f"GG@{ti}"])

    def s5_setup(self, stack, l, d):
        S, t = self.S, self.t
        psb = lambda name, shape, dt=F32: (self.sb(stack, name, shape, dt) if name not in t else t[name])
        W = psb("s5_W", [128, 16, 2, 128])
        V = psb("s5_V", [128, 16, 2, 128])
        BT = psb("s5_BT", [128, 16, 256], BF16)
        CT = psb("s5_CT", [128, 16, 2, 128], BF16)
        with self.phase() as tmp:
            tsb = lambda name, shape, dt=F32: self.sb(tmp, name, shape, dt)
            lre = tsb("s5t_lre", [128, 16])
            lim = tsb("s5t_lim", [128, 16])
            lst = tsb("s5t_lst", [128, 16])
            self.dma("sp", lre[:], self.w["s5_a_re"][l, d].rearrange("g n -> (g n)").rearrange("(j p) -> p j", p=128), (), ["s5t_lre"], allow_slow_non_contiguous=True)
            self.dma("sp", lim[:], self.w["s5_a_im"][l, d].rearrange("g n -> (g n)").rearrange("(j p) -> p j", p=128), (), ["s5t_lim"], allow_slow_non_contiguous=True)
            ls = self.w["s5_log_step"]
            base = ls[l, d, 0:1]
            for g2 in (0, 1):
                src = bass.AP(ls.tensor, base.offset + g2, [[0, 64], [2, 16]])
                self.dma("sp", lst[g2 * 64:(g2 + 1) * 64, :], src, (), ["s5t_lst"], allow_slow_non_contiguous=True)
            self.act(lst[:], lst[:], AF.Exp, ["s5t_lst"], ["s5t_lst"])
            lr = tsb("s5t_lr", [128, 16])
            li = tsb("s5t_li", [128, 16])
            self.tt("dve", lr[:], lre[:], lst[:], ALU.mult, ["s5t_lre", "s5t_lst"], ["s5t_lr"])
            self.tt("dve", li[:], lim[:], lst[:], ALU.mult, ["s5t_lim", "s5t_lst"], ["s5t_li"])
            ti_ = tsb("s5t_ti", [128, 128], I32)
            tau = tsb("s5t_tau", [128, 128])
            if d == 0:
                S.op("pool", lambda e: e.iota(ti_[:], pattern=[[1, 128]], base=1, channel_multiplier=0), (), ["s5t_ti"])
            else:
                S.op("pool", lambda e: e.iota(ti_[:], pattern=[[-1, 128]], base=128, channel_multiplier=0), (), ["s5t_ti"])
            self.cp("dve", tau[:], ti_[:], ["s5t_ti"], ["s5t_tau"])
            mag = tsb("s5t_mag", [128, 16, 128])
            ang = tsb("s5t_ang", [128, 16, 128])
            a2 = tsb("s5t_a2", [128, 16, 128])
            bi = tsb("s5t_bi", [128, 16, 128], I32)
            sn = tsb("s5t_sin", [128, 16, 128])
            cs = tsb("s5t_cos", [128, 16, 128])
            for j in range(16):
                self.act(mag[:, j, :], tau[:], AF.Exp, ["s5t_tau", "s5t_lr"], ["s5t_mag"], scale=lr[:, j:j + 1])
                self.ts("dve", ang[:, j, :], tau[:], li[:, j:j + 1], ALU.mult, ["s5t_tau", "s5t_li"], ["s5t_ang"])
            for dst, phase in ((sn, 0.0), (cs, math.pi / 2)):
                dn = "s5t_sin" if phase == 0.0 else "s5t_cos"
                self.ts("dve", a2[:], ang[:], phase, ALU.add, ["s5t_ang"], ["s5t_a2"])
                self.ts("dve", dst[:], a2[:], 1.0 / TWO_PI, ALU.mult, ["s5t_a2"], [dn])
                self.cp("dve", bi[:], dst[:], [dn], ["s5t_bi"])
                self.cp("dve", dst[:], bi[:], ["s5t_bi"], [dn])
                self.stt(a2[:], dst[:], -TWO_PI, a2[:], ALU.mult, ALU.add, [dn, "s5t_a2"], ["s5t_a2"])
                self.ts("dve", a2[:], a2[:], PI_SAFE, ALU.min, ["s5t_a2"], ["s5t_a2"], s2=-PI_SAFE, op1=ALU.max)
                self.act(dst[:], a2[:], AF.Sin, ["s5t_a2"], [dn])
            self.tt("dve", V[:, :, 0, :], mag[:], cs[:], ALU.mult, ["s5t_mag", "s5t_cos"], ["s5_V"])
            self.tt("dve", V[:, :, 1, :], mag[:], sn[:], ALU.mult, ["s5t_mag", "s5t_sin"], ["s5_V"])
            S.op("dve", lambda e: e.reciprocal(out=mag[:], in_=mag[:]), ["s5t_mag"], ["s5t_mag"])
            wfm = tsb("s5t_wfm", [128, 16, 2, 128])
            self.tt("dve", wfm[:, :, 0, :], mag[:], cs[:], ALU.mult, ["s5t_mag", "s5t_cos"], ["s5t_wfm"])
            self.stt(wfm[:, :, 1, :], mag[:], -1.0, sn[:], ALU.mult, ALU.mult, ["s5t_mag", "s5t_sin"], ["s5t_wfm"])
            k = 0
            for j in range(16):
                for c in range(2):
                    pn = f"P{k % 4}"
                    k += 1
                    self.tr(t[pn][:, 0:128], wfm[:, j, c, :], ["s5t_wfm"], [pn])
                    self.cp("act" if k % 2 else "dve", W[:, j, c, :], t[pn][:, 0:128], [pn], ["s5_W"])
            c1 = 0 if d == 0 else 127
            are = V[:, :, 0, c1]
            aim = V[:, :, 1, c1]
            den = tsb("s5t_den", [128, 16])
            tmp1 = tsb("s5t_t1", [128, 16])
            tmp2 = tsb("s5t_t2", [128, 16])
            ar1 = tsb("s5t_ar1", [128, 16])
            zre = tsb("s5t_zre", [128, 16])
            zim = tsb("s5t_zim", [128, 16])
            self.tt("dve", den[:], lre[:], lre[:], ALU.mult, ["s5t_lre"], ["s5t_den"])
            self.tt("dve", tmp1[:], lim[:], lim[:], ALU.mult, ["s5t_lim"], ["s5t_t1"])
            self.tt("dve", den[:], den[:], tmp1[:], ALU.add, ["s5t_den", "s5t_t1"], ["s5t_den"])
            S.op("dve", lambda e: e.reciprocal(out=den[:], in_=den[:]), ["s5t_den"], ["s5t_den"])
            self.ts("dve", ar1[:], are, -1.0, ALU.add, ["s5_V"], ["s5t_ar1"])
            self.tt("dve", tmp1[:], ar1[:], lre[:], ALU.mult, ["s5t_ar1", "s5t_lre"], ["s5t_t1"])
            self.tt("dve", tmp2[:], aim, lim[:], ALU.mult, ["s5_V", "s5t_lim"], ["s5t_t2"])
            self.tt("dve", tmp1[:], tmp1[:], tmp2[:], ALU.add, ["s5t_t1", "s5t_t2"], ["s5t_t1"])
            self.tt("dve", zre[:], tmp1[:], den[:], ALU.mult, ["s5t_t1", "s5t_den"], ["s5t_zre"])
            self.tt("dve", tmp1[:], aim, lre[:], ALU.mult, ["s5_V", "s5t_lre"], ["s5t_t1"])
            self.tt("dve", tmp2[:], ar1[:], lim[:], ALU.mult, ["s5t_ar1", "s5t_lim"], ["s5t_t2"])
            self.tt("dve", tmp1[:], tmp1[:], tmp2[:], ALU.subtract, ["s5t_t1", "s5t_t2"], ["s5t_t1"])
            self.tt("dve", zim[:], tmp1[:], den[:], ALU.mult, ["s5t_t1", "s5t_den"], ["s5t_zim"])
            bre = tsb("s5t_bre", [128, 16, 16])
            bim = tsb("s5t_bim", [128, 16, 16])
            self.dma("sp", bre[:], self.w["s5_b_re"][l, d].rearrange("g n q -> (g n) q").rearrange("(j p) q -> p j q", p=128), (), ["s5t_bre"])
            self.dma("sp", bim[:], self.w["s5_b_im"][l, d].rearrange("g n q -> (g n) q").rearrange("(j p) q -> p j q", p=128), (), ["s5t_bim"])
            bbr = tsb("s5t_bbr", [128, 16, 16])
            bbi = tsb("s5t_bbi", [128, 16, 16])
            u1 = tsb("s5t_u1", [128, 16, 16])
            zr_b = zre[:].unsqueeze(2).to_broadcast([128, 16, 16])
            zi_b = zim[:].unsqueeze(2).to_broadcast([128, 16, 16])
            self.tt("dve", bbr[:], bre[:], zr_b, ALU.mult, ["s5t_bre", "s5t_zre"], ["s5t_bbr"])
            self.tt("dve", u1[:], bim[:], zi_b, ALU.mult, ["s5t_bim", "s5t_zim"], ["s5t_u1"])
            self.tt("dve", bbr[:], bbr[:], u1[:], ALU.subtract, ["s5t_bbr", "s5t_u1"], ["s5t_bbr"])
            self.tt("dve", bbi[:], bim[:], zr_b, ALU.mult, ["s5t_bim", "s5t_zre"], ["s5t_bbi"])
            self.tt("dve", u1[:], bre[:], zi_b, ALU.mult, ["s5t_bre", "s5t_zim"], ["s5t_u1"])
            self.tt("dve", bbi[:], bbi[:], u1[:], ALU.add, ["s5t_bbi", "s5t_u1"], ["s5t_bbi"])
            bm = tsb("s5t_bm", [128, 16, 2, 128])
            for j in range(16):
                mk = t[f"mask{j % 4}"]
                for c, src, sname in ((0, bbr, "s5t_bbr"), (1, bbi, "s5t_bbi")):
                    self.tt("dve", bm[:, j, c, :].rearrange("p (r q) -> p r q", r=8),
                            src[:, j, :].unsqueeze(1).to_broadcast([128, 8, 16]),
                            mk[:].rearrange("p (r q) -> p r q", r=8), ALU.mult, [sname, f"mask{j % 4}"], ["s5t_bm"])
            for j in range(16):
                for c in range(2):
                    pn = f"P{k % 4}"
                    k += 1
                    self.tr(t[pn][:, 0:128], bm[:, j, c, :], ["s5t_bm"], [pn])
                    self.cp("act" if k % 2 else "dve", BT[:, j, c * 128:(c + 1) * 128], t[pn][:, 0:128], [pn], ["s5_BT"])
            for c, cname in ((0, "s5_c_re"), (1, "s5_c_im")):
                ctm = tsb(f"s5t_ctm{c}", [128, 4, 2, 64])
                src = self.w[cname][l, d].rearrange("g p n -> (g p) n").rearrange("(b rp) n -> rp b n", rp=128)
                for dup in range(2):
                    self.dma("sp", ctm[:, :, dup, :], src, (), [f"s5t_ctm{c}"])
                for b in range(4):
                    pn = f"P{k % 4}"
                    k += 1
                    self.tr(t[pn][:, 0:128], ctm[:, b, :, :].rearrange("p a n -> p (a n)"), [f"s5t_ctm{c}"], [pn])
                    for kk in range(4):
                        j = 4 * b + kk
                        self.stt(CT[:, j, c, :], t[pn][:, 0:128], 1.0 if c == 0 else -1.0, t[f"mask{kk}"][:], ALU.mult, ALU.mult,
                                 [pn, f"mask{kk}"], ["s5_CT"])


FAMS = {
    "hg": dict(G=4, W=512, g=("GHF", "GHB"), kt=("KHFt", "KHBt"), v="VH", q="QH", kf=("KHF", "KHB"),
               heads=[(h, 0, 128) for h in range(4)], orow=0),
    "gl": dict(G=2, W=256, g=("GG", "GG"), kt=("KGt", "KGt"), v="VG", q="QG", kf=("KG", "KG"),
               heads=[(h // 2, (h % 2) * 64, 64) for h in range(4)], orow=512),
}


class KB3(KB2):
    def rec_pass(self, l, d):
        S, t = self.S, self.t
        T, NT, nct = self.T, self.NT, self.nct
        for nm, rows in ((f"O{d}", 1024), (f"Y5{d}", 512)):
            if nm not in self.scr:
                self.dscr(nm, [rows, T])
        order = list(range(NT)) if d == 0 else (list(range(nct - 1, -1, -1)) + list(range(NT - 1, nct - 1, -1)))
        last = 127 if d == 0 else 0
        with self.phase() as ph:
            psb = lambda name, shape, dt=F32: (self.sb(ph, name, shape, dt) if name not in t else t[name])
            self.s5_setup(ph, l, d)
            minc, mstr, mincb = t[f"minc{d}"], t[f"mstr{d}"], t[f"mincb{d}"]
            mincn, mstrn, mincbn = f"minc{d}", f"mstr{d}", f"mincb{d}"
            W5, V5, BT5, CT5 = t["s5_W"], t["s5_V"], t["s5_BT"], t["s5_CT"]
            for f in ("hg", "gl"):
                psb(f"{f}_Sf", [128, 4, 128])
                psb(f"{f}_Sb", [128, 4, 128], BF16)
                self.memset("pool", t[f"{f}_Sf"][:], 0.0, [f"{f}_Sf"])
                self.memset("pool", t[f"{f}_Sb"][:], 0.0, [f"{f}_Sb"])
            carry = psb("s5_carry", [128, 16, 2])
            self.memset("pool", carry[:], 0.0, ["s5_carry0", "s5_carry1"])
            dbuf = psb("r_D", [128, 2, 8, 128])

            def loads(ti, par):
                rows = slice(ti * 128, (ti + 1) * 128)
                cols = slice(ti * 128, (ti + 1) * 128)
                for f, F_ in FAMS.items():
                    G, W = F_["G"], F_["W"]
                    gt = psb(f"{f}_g{par}", [128, W])
                    gsrc = self.scr[F_["g"][d]]
                    gs = gsrc[rows, :] if f == "hg" else gsrc[rows, d * 256:(d + 1) * 256]
                    self.dma("sp", gt[:], gs, [f"{F_['g'][d]}@{ti}"], [f"{f}_g{par}"])
                    kt = psb(f"{f}_kt{par}", [128, W])
                    self.dma("sp", kt[:], self.scr[F_["kt"][d]][rows, :], [f"{F_['kt'][d]}@{ti}"], [f"{f}_kt{par}"])
                    vt = psb(f"{f}_v{par}", [128, 512], BF16)
                    self.dma("sp", vt[:], self.scr[F_["v"]][rows, :], [f"{F_['v']}@{ti}"], [f"{f}_v{par}"])
                    qf = psb(f"{f}_q{par}", [128, G, 128])
                    self.dma("sp", qf[:], self.scr[F_["q"]].rearrange("(g p) t -> p g t", p=128)[:, :, cols], [f"{F_['q']}@{ti}"], [f"{f}_q{par}"])
                    kf = psb(f"{f}_kf{par}", [128, G, 128])
                    self.dma("sp", kf[:], self.scr[F_["kf"][d]].rearrange("(g p) t -> p g t", p=128)[:, :, cols], [f"{F_['kf'][d]}@{ti}"], [f"{f}_kf{par}"])
                u = psb(f"s5_u{par}", [128, 4, 128])
                self.dma("sp", u[:], self.scr["U5"].rearrange("(g p) t -> p g t", p=128)[:, :, cols], [f"U5@{ti}"], [f"s5_u{par}"])

            def fam_tile(f, ti, par):
                F_ = FAMS[f]
                G, W, heads = F_["G"], F_["W"], F_["heads"]
                gt, kt, vt, qf, kf = (t[f"{f}_{x}{par}"] for x in ("g", "kt", "v", "q", "kf"))
                gn, ktn, vn, qn, kfn = (f"{f}_{x}{par}" for x in ("g", "kt", "v", "q", "kf"))
                Sf, Sb = t[f"{f}_Sf"], t[f"{f}_Sb"]
                PE_, PB, PS, PO = (t[f"P{i}"] for i in range(4))
                PU = t["P0"]
                ex = psb(f"{f}_ex", [128, W])
                if f == "hg":
                    khat = psb(f"{f}_khat", [128, 4, 128], BF16)
                else:
                    fresh = "gl_khat" not in t
                    khat = psb("gl_khat", [128, 4, 128], BF16)
                    if fresh:
                        self.memset("pool", khat[:], 0.0, ["gl_khat"])
                bt = psb(f"{f}_bt", [128, G, 128])
                eb = psb(f"{f}_eb", [128, G, 128])
                qbar = psb(f"{f}_qbar", [128, G, 128], BF16)
                bprev = psb(f"{f}_bprev", [128, G, 8])
                bq = psb(f"{f}_bq", [128, G, 128])
                qt = psb(f"{f}_qt", [128, G, 128], BF16)
                pt = psb(f"{f}_pt", [128, 4, 128], BF16)
                osb = psb(f"{f}_osb", [128, 4, 128])
                ktb = psb(f"{f}_Kt", [128, 4, 8, 128], BF16)
                self.mm(PE_[:, 0:W], mstr[:], gt[:], [mstrn, gn], ["P0"])
                self.act(ex[:], PE_[:, 0:W], AF.Exp, ["P0"], [f"{f}_ex"])
                if f == "hg":
                    self.tt("dve", khat[:].rearrange("p h c -> p (h c)"), ex[:], kt[:], ALU.mult, [f"{f}_ex", ktn], [f"{f}_khat"])
                else:
                    kb_ = khat[:]
                    kout = bass.AP(kb_.tensor, kb_.offset, [list(kb_.ap[0]), [256, 2], [192, 2], [1, 64]])
                    self.tt("dve", kout, ex[:].rearrange("p (g a c) -> p g a c", g=2, a=2), kt[:].rearrange("p (g a c) -> p g a c", g=2, a=2),
                            ALU.mult, [f"{f}_ex", ktn], [f"{f}_khat"])
                yield
                for g in range(G):
                    self.mm(PB[:, g * 128:(g + 1) * 128], gt[:, g * 128:(g + 1) * 128], minc[:], [gn, mincn], ["P1"])
                pb3 = PB[:, 0:G * 128].rearrange("p (g c) -> p g c", g=G)
                self.cp("act", bt[:], pb3, ["P1"], [f"{f}_bt"])
                self.act(eb[:], pb3, AF.Exp, ["P1"], [f"{f}_eb"])
                self.tt("dve", qbar[:], qf[:], eb[:], ALU.mult, [qn, f"{f}_eb"], [f"{f}_qbar"])
                yield
                if d == 0:
                    self.memset("dve", bprev[:, :, 0:1], 0.0, [f"{f}_bprev"])
                    self.cp("dve", bprev[:, :, 1:8], bt[:, :, 15:127:16], [f"{f}_bt"], [f"{f}_bprev"])
                else:
                    self.memset("dve", bprev[:, :, 7:8], 0.0, [f"{f}_bprev"])
                    self.cp("dve", bprev[:, :, 0:7], bt[:, :, 16:128:16], [f"{f}_bt"], [f"{f}_bprev"])
                self.tt("dve", bq[:].rearrange("p g (i j) -> p g i j", i=8), bt[:].rearrange("p g (i j) -> p g i j", i=8),
                        bprev[:].unsqueeze(3).to_broadcast([128, G, 8, 16]), ALU.subtract, [f"{f}_bt", f"{f}_bprev"], [f"{f}_bq"])
                self.act(bq[:], bq[:], AF.Exp, [f"{f}_bq"], [f"{f}_bq"])
                self.tt("dve", qt[:], bq[:], qf[:], ALU.mult, [f"{f}_bq", qn], [f"{f}_qt"])
                yield
                if f == "gl":
                    kfm = psb("gl_kfm", [128, 4, 128])
                    for h, (g, p0, ps_) in enumerate(heads):
                        self.ts("dve", kfm[:, h, :], kf[:, g, :], t[f"hm{h % 2}"][:, 0:1], ALU.mult, [kfn, f"hm{h % 2}"], ["gl_kfm"])
                for g0 in range(0, G, 2):
                    dv = dbuf[:, 0:2, :, :]
                    self.tt("dve", dv, bprev[:, g0:g0 + 2, :].unsqueeze(3).to_broadcast([128, 2, 8, 128]),
                            bt[:, g0:g0 + 2, :].unsqueeze(2).to_broadcast([128, 2, 8, 128]), ALU.subtract, [f"{f}_bprev", f"{f}_bt"], ["r_D"])
                    self.ts("dve", dv, dv, 60.0, ALU.min, ["r_D"], ["r_D"])
                    self.act(dv, dv, AF.Exp, ["r_D"], ["r_D"])
                    if f == "hg":
                        self.tt("dve", ktb[:, g0:g0 + 2, :, :], dv, kf[:, g0:g0 + 2, :].unsqueeze(2).to_broadcast([128, 2, 8, 128]), ALU.mult, ["r_D", kfn], [f"{f}_Kt"])
                    else:
                        for h, (g, p0, ps_) in enumerate(heads):
                            self.tt("dve", ktb[:, h, :, :], dbuf[:, g, :, :], kfm[:, h, :].unsqueeze(1).to_broadcast([128, 8, 128]), ALU.mult,
                                    ["r_D", "gl_kfm"], [f"{f}_Kt"])
                yield
                for h, (g, p0, ps_) in enumerate(heads):
                    for i in range(8):
                        self.mm(PS[:, h * 128 + 16 * i:h * 128 + 16 * i + 16], ktb[:, h, i, :],
                                qt[:, g, 16 * i:16 * i + 16], [f"{f}_Kt", f"{f}_qt"], ["P2"])
                self.tt("dve", pt[:], PS[:].rearrange("p (h c) -> p h c", h=4), minc[:].unsqueeze(1).to_broadcast([128, 4, 128]),
                        ALU.mult, ["P2", mincn], [f"{f}_pt"])
                yield
                for h, (g, p0, ps_) in enumerate(heads):
                    self.mm(PO[:, h * 128:(h + 1) * 128], vt[:, h * 128:(h + 1) * 128], pt[:, h, :], [vn, f"{f}_pt"], ["P3"], start=True, stop=False)
                    self.mm(PO[:, h * 128:(h + 1) * 128], Sb[:, h, :], qbar[:, g, :], [f"{f}_Sb", f"{f}_qbar"], ["P3"], start=False, stop=True)
                self.cp("act", osb[:], PO[:].rearrange("p (h c) -> p h c", h=4), ["P3"], [f"{f}_osb"])
                orow = F_["orow"]
                self.dma("act", self.scr[f"O{d}"][orow:orow + 512, :].rearrange("(h p) t -> p h t", p=128)[:, :, ti * 128:(ti + 1) * 128],
                         osb[:], [f"{f}_osb"], [f"O{d}{f}@{ti}"])
                yield
                for h, (g, p0, ps_) in enumerate(heads):
                    self.mm(PU[:, h * 128:(h + 1) * 128], khat[:, h, :], vt[:, h * 128:(h + 1) * 128], [f"{f}_khat", vn], ["P0"])
                for h, (g, p0, ps_) in enumerate(heads):
                    self.stt(Sf[:, h, :], Sf[:, h, :], eb[:, g, last:last + 1], PU[:, h * 128:(h + 1) * 128], ALU.mult, ALU.add,
                             [f"{f}_Sf", f"{f}_eb", "P0"], [f"{f}_Sf"])
                self.cp("act", Sb[:], Sf[:], [f"{f}_Sf"], [f"{f}_Sb"])
                yield

            def s5_prep(ti, par):
                u = t[f"s5_u{par}"]
                ub = psb("s5_ub", [128, 4, 128], BF16)
                self.cp("pool", ub[:], u[:], [f"s5_u{par}"], ["s5_ub"])
                psb("s5_sbf", [128, 16, 2, 128], BF16)

            def s5_half(ti, par, hp):
                ub = t["s5_ub"]
                sbf = t["s5_sbf"]
                PBU, pbun = (t["P5"], "P5") if hp == 0 else (t["P7"], "P7")
                PZ, pzn = (t["P6"], "P6") if hp == 0 else (t["P4"], "P4")
                sbfn, carn = f"s5_sbf{hp}", f"s5_carry{hp}"
                for r in range(hp, 8, 2):
                    for jj in range(2):
                        j = 2 * r + jj
                        self.mm(PBU[:, jj * 256:(jj + 1) * 256], ub[:, j // 4, :], BT5[:, j, :], ["s5_ub", "s5_BT"], [pbun])
                    yield
                    pbu = PBU[:].rearrange("p (a c n) -> p a c n", a=2, c=2)
                    Wv = W5[:, 2 * r:2 * r + 2, :, :]
                    t1 = psb(f"s5_t1_{hp}", [128, 2, 128])
                    t2 = psb(f"s5_t2_{hp}", [128, 2, 128])
                    x = psb(f"s5_x{hp}", [128, 2, 2, 128], BF16)
                    xn, t1n, t2n = f"s5_x{hp}", f"s5_t1_{hp}", f"s5_t2_{hp}"
                    self.tt("dve", t1[:], pbu[:, :, 0, :], Wv[:, :, 0, :], ALU.mult, [pbun, "s5_W"], [t1n])
                    self.tt("dve", t2[:], pbu[:, :, 1, :], Wv[:, :, 1, :], ALU.mult, [pbun, "s5_W"], [t2n])
                    self.tt("dve", x[:, :, 0, :], t1[:], t2[:], ALU.subtract, [t1n, t2n], [xn])
                    self.tt("dve", t1[:], pbu[:, :, 0, :], Wv[:, :, 1, :], ALU.mult, [pbun, "s5_W"], [t1n])
                    self.tt("dve", t2[:], pbu[:, :, 1, :], Wv[:, :, 0, :], ALU.mult, [pbun, "s5_W"], [t2n])
                    self.tt("dve", x[:, :, 1, :], t1[:], t2[:], ALU.add, [t1n, t2n], [xn])
                    yield
                    zs = psb(f"s5_zs_{hp}", [128, 2, 2, 128])
                    zsn = f"s5_zs_{hp}"
                    for jj in range(2):
                        for c in range(2):
                            col = (jj * 2 + c) * 128
                            self.mm(PZ[:, col:col + 128], x[:, jj, c, :], mincb[:], [xn, mincbn], [pzn])
                    for jj in range(2):
                        for c in range(2):
                            col = (jj * 2 + c) * 128
                            self.act(zs[:, jj, c, :], PZ[:, col:col + 128], AF.Identity, [pzn, carn], [zsn],
                                     bias=carry[:, 2 * r + jj, c:c + 1])
                    yield
                    Vv = V5[:, 2 * r:2 * r + 2, :, :]
                    sre = psb(f"s5_sre_{hp}", [128, 2, 128])
                    sim = psb(f"s5_sim_{hp}", [128, 2, 128])
                    m1 = psb(f"s5_m1_{hp}", [128, 2, 128])
                    m2 = psb(f"s5_m2_{hp}", [128, 2, 128])
                    sren, simn, m1n, m2n = f"s5_sre_{hp}", f"s5_sim_{hp}", f"s5_m1_{hp}", f"s5_m2_{hp}"
                    self.tt("dve", m1[:], zs[:, :, 0, :], Vv[:, :, 0, :], ALU.mult, [zsn, "s5_V"], [m1n])
                    self.tt("dve", m2[:], zs[:, :, 1, :], Vv[:, :, 1, :], ALU.mult, [zsn, "s5_V"], [m2n])
                    self.tt("dve", sre[:], m1[:], m2[:], ALU.subtract, [m1n, m2n], [sren])
                    self.tt("dve", m1[:], zs[:, :, 0, :], Vv[:, :, 1, :], ALU.mult, [zsn, "s5_V"], [m1n])
                    self.tt("dve", m2[:], zs[:, :, 1, :], Vv[:, :, 0, :], ALU.mult, [zsn, "s5_V"], [m2n])
                    self.tt("dve", sim[:], m1[:], m2[:], ALU.add, [m1n, m2n], [simn])
                    yield
                    self.cp("pool", sbf[:, 2 * r:2 * r + 2, 0, :], sre[:], [sren], [sbfn])
                    self.cp("pool", sbf[:, 2 * r:2 * r + 2, 1, :], sim[:], [simn], [sbfn])
                    self.cp("pool", carry[:, 2 * r:2 * r + 2, 0], sre[:, :, last], [sren], [carn])
                    self.cp("pool", carry[:, 2 * r:2 * r + 2, 1], sim[:, :, last], [simn], [carn])
                    yield

            def s5_fin(ti, par):
                sbf = t["s5_sbf"]
                PY = t["P5"]
                for b in range(4):
                    for kk in range(4):
                        j = 4 * b + kk
                        self.mm(PY[:, b * 128:(b + 1) * 128], CT5[:, j, 0, :], sbf[:, j, 0, :], ["s5_CT", "s5_sbf0", "s5_sbf1"], ["P5"], start=(kk == 0), stop=False)
                        self.mm(PY[:, b * 128:(b + 1) * 128], CT5[:, j, 1, :], sbf[:, j, 1, :], ["s5_CT", "s5_sbf0", "s5_sbf1"], ["P5"], start=False, stop=(kk == 3))
                ysb = psb("s5_ysb", [128, 4, 128])
                self.cp("act", ysb[:], PY[:].rearrange("p (b c) -> p b c", b=4), ["P5"], ["s5_ysb"])
                self.dma("act", self.scr[f"Y5{d}"].rearrange("(b p) t -> p b t", p=128)[:, :, ti * 128:(ti + 1) * 128], ysb[:],
                         ["s5_ysb"], [f"Y5{d}@{ti}"])

            import os as _os
            skip = ""
            if "all" in skip:
                return
            loads(order[0], 0)
            for n, ti in enumerate(order):
                par = n % 2
                if n + 1 < len(order):
                    loads(order[n + 1], 1 - par)
                gens = []
                if "hg" not in skip:
                    gens.append(fam_tile("hg", ti, par))
                if "gl" not in skip:
                    gens.append(fam_tile("gl", ti, par))
                if "s5" not in skip:
                    s5_prep(ti, par)
                    gens.append(s5_half(ti, par, 0))
                    gens.append(s5_half(ti, par, 1))
                while gens:
                    for g_ in list(gens):
                        try:
                            next(g_)
                        except StopIteration:
                            gens.remove(g_)
                if "s5" not in skip:
                    s5_fin(ti, par)


class KB4(KB3):
    def load_cast(self, stack, dst, dst_name, src_ap, nk, ncols, kstep):
        t = self.t
        for k0 in range(0, nk, kstep):
            kn = min(kstep, nk - k0)
            sn = f"lc_st{kstep}_{self._lc % 2}"
            self._lc += 1
            stg = self.sb(stack, sn, [128, kstep, 1024]) if sn not in t else t[sn]
            self.dma("sp", stg[:, 0:kn, 0:ncols], src_ap[:, k0:k0 + kn, :], (), [sn])
            self.cp("pool", dst[:, k0:k0 + kn, 0:ncols], stg[:, 0:kn, 0:ncols], [sn], [dst_name])

    def merge_phase(self, l, last):
        S, t = self.S, self.t
        T, NT = self.T, self.NT
        self._lc = 0
        with self.phase() as ph:
            psb = lambda name, shape, dt=F32: (self.sb(ph, name, shape, dt) if name not in t else t[name])
            bp = psb("mg_bp", [128, 12, D], BF16)
            wo = psb("mg_wo", [128, 8, D], BF16)
            glu = psb("mg_glu", [128, 4, 512], BF16)
            with self.phase() as tmpw:
                self.load_cast(tmpw, bp, "mg_bp", self.w["branch_proj"][l].rearrange("b (kc p) n -> p (b kc) n", p=128), 12, D, 4)
                self.load_cast(tmpw, wo, "mg_wo", self.w["w_out"][l].rearrange("(kc p) n -> p kc n", p=128), 8, D, 4)
                self.load_cast(tmpw, glu, "mg_glu", self.w["s5_glu_w"][l].rearrange("(kc p) n -> p kc n", p=128), 4, 512, 4)
            gains = {}
            for nm, src in (("hgn", "hg_norm_g"), ("gln", "gla_norm_g"), ("s5d", "s5_d")):
                g = psb("mg_" + nm, [128, 4])
                self.dma("sp", g[:], self.w[src][l].rearrange("(h p) -> p h", p=128), (), ["mg_" + nm], allow_slow_non_contiguous=True)
                gains[nm] = g
            self.load_mod(ph, l, 2, "mg_g1")
            epsT = psb("mg_eps", [128, 1])
            self.memset("pool", epsT[:], EPS, ["mg_eps"])
            tiles = [ti for ti in range(NT) if not (last and self.is_ctx(ti))]

            def ld(name, par, src_ap, shape, deps, dt=F32):
                nm = f"mg_{name}{par}"
                h = psb(nm, shape, dt)
                self.dma("sp", h[:], src_ap, deps, [nm])
                return h

            def loads(ti, par):
                cols = slice(ti * 128, (ti + 1) * 128)
                fmv = lambda ap: ap.rearrange("(h p) t -> p h t", p=128)[:, :, cols]
                for d in (0, 1):
                    ld(f"ohg{d}", par, fmv(self.scr[f"O{d}"][0:512, :]), [128, 4, 128], [f"O{d}hg@{ti}"])
                    ld(f"ogl{d}", par, fmv(self.scr[f"O{d}"][512:1024, :]), [128, 4, 128], [f"O{d}gl@{ti}"])
                    ld(f"y5{d}", par, fmv(self.scr[f"Y5{d}"]), [128, 4, 128], [f"Y5{d}@{ti}"])
                ld("u5", par, fmv(self.scr["U5"]), [128, 4, 128], [f"U5@{ti}"])
                ld("gh", par, fmv(self.scr["GH"]), [128, 4, 128], [f"GH@{ti}"])
                ld("rg", par, fmv(self.scr["RG"]), [128, 4, 128], [f"RG@{ti}"])
                for nm in ("GA", "GB", "GC"):
                    ld(nm, par, fmv(self.scr[nm]), [128, 8, 128], [f"{nm}@{ti}"])
                ld("x", par, self.scr["X"][ti * 128:(ti + 1) * 128, :], [128, D], [f"X@{ti}"])

            def compute(ti, par):
                v = 1 if self.is_ctx(ti) else 0
                g = lambda name: (t[f"mg_{name}{par}"], f"mg_{name}{par}")
                ybr = {}
                for bi, (fam, gate, gain) in enumerate((("hg", "gh", "hgn"), ("gl", "rg", "gln"))):
                    o0, o0n = g(f"o{fam}0")
                    o1, o1n = g(f"o{fam}1")
                    gt, gtn = g(gate)
                    o = psb(f"mg_o{bi}", [128, 4, 128])
                    osq = psb(f"mg_osq{bi}", [128, 4, 128])
                    rs = psb(f"mg_rs{bi}", [128, 4, 128])
                    yb = psb(f"mg_y{bi}", [128, 4, 128], BF16)
                    on, sqn, rsn, ybn = f"mg_o{bi}", f"mg_osq{bi}", f"mg_rs{bi}", f"mg_y{bi}"
                    self.tt("dve", o[:], o0[:], o1[:], ALU.add, [o0n, o1n], [on])
                    self.tt("pool", osq[:], o[:], o[:], ALU.mult, [on], [sqn])
                    self.mm(t["P0"][:], t["ones"][:], osq[:].rearrange("p h c -> p (h c)"), ["ones", sqn], ["P0"])
                    self.act(rs[:].rearrange("p h c -> p (h c)"), t["P0"][:], AF.Sqrt, ["P0", "mg_eps"], [rsn], bias=epsT[:], scale=1.0 / 128.0)
                    S.op("dve", lambda e, rs=rs: e.reciprocal(out=rs[:], in_=rs[:]), [rsn], [rsn])
                    self.tt("dve", o[:], o[:], rs[:], ALU.mult, [on, rsn], [on])
                    for h in range(4):
                        self.stt(yb[:, h, :], o[:, h, :], gains[gain][:, h:h + 1], gt[:, h, :], ALU.mult, ALU.mult,
                                 [on, "mg_" + gain, gtn], [ybn])
                    ybr[bi] = (yb, ybn)
                u5, u5n = g("u5")
                y50, y50n = g("y50")
                y51, y51n = g("y51")
                yy = psb("mg_yy", [128, 4, 128])
                tq = psb("mg_tq", [128, 4, 128])
                for k in range(4):
                    self.stt(yy[:, k, :], u5[:, k, :], gains["s5d"][:, k:k + 1], y50[:, k, :], ALU.mult, ALU.add, [u5n, "mg_s5d", y50n], ["mg_yy"])
                self.tt("dve", yy[:], yy[:], y51[:], ALU.add, ["mg_yy", y51n], ["mg_yy"])
                self.tt("pool", tq[:], yy[:], yy[:], ALU.mult, ["mg_yy"], ["mg_tq"])
                self.ts("pool", tq[:], tq[:], 0.044715, ALU.mult, ["mg_tq"], ["mg_tq"], s2=1.0, op1=ALU.add)
                self.tt("pool", tq[:], tq[:], yy[:], ALU.mult, ["mg_tq", "mg_yy"], ["mg_tq"])
                self.act(tq[:], tq[:], AF.Tanh, ["mg_tq"], ["mg_tq"], scale=math.sqrt(2.0 / math.pi))
                self.ts("dve", tq[:], tq[:], 1.0, ALU.add, ["mg_tq"], ["mg_tq"], s2=0.5, op1=ALU.mult)
                self.tt("dve", yy[:], yy[:], tq[:], ALU.mult, ["mg_yy", "mg_tq"], ["mg_yy"])
                ygb = psb("mg_ygb", [128, 4, 128], BF16)
                self.cp("pool", ygb[:], yy[:], ["mg_yy"], ["mg_ygb"])
                for oc in range(4):
                    for kc in range(4):
                        self.mm(t["P1"][:, oc * 128:(oc + 1) * 128], glu[:, kc, oc * 128:(oc + 1) * 128], ygb[:, kc, :],
                                ["mg_glu", "mg_ygb"], ["P1"], start=(kc == 0), stop=(kc == 3))
                self.act(tq[:].rearrange("p h c -> p (h c)"), t["P1"][:], AF.Sigmoid, ["P1"], ["mg_tq"])
                yc = psb("mg_y2", [128, 4, 128], BF16)
                self.tt("dve", yc[:], yy[:], tq[:], ALU.mult, ["mg_yy", "mg_tq"], ["mg_y2"])
                ybr[2] = (yc, "mg_y2")
                mg = psb("mg_m", [128, 8, 128])
                mt = psb("mg_mt", [128, 8, 128])
                for bi, gname in enumerate(("GA", "GB", "GC")):
                    yb, ybn = ybr[bi]
                    gt, gtn = g(gname)
                    for oc in range(8):
                        pn = f"P{2 + oc // 4}"
                        for kc in range(4):
                            self.mm(t[pn][:, (oc % 4) * 128:(oc % 4 + 1) * 128], bp[:, bi * 4 + kc, oc * 128:(oc + 1) * 128], yb[:, kc, :],
                                    ["mg_bp", ybn], [pn], start=(kc == 0), stop=(kc == 3))
                    for half in range(2):
                        pn = f"P{2 + half}"
                        pv = t[pn][:].rearrange("p (o c) -> p o c", o=4)
                        dst = mg if bi == 0 else mt
                        dn = "mg_m" if bi == 0 else "mg_mt"
                        self.tt("dve", dst[:, half * 4:(half + 1) * 4, :], pv, gt[:, half * 4:(half + 1) * 4, :], ALU.mult, [pn, gtn], [dn])
                    if bi > 0:
                        self.tt("pool", mg[:], mg[:], mt[:], ALU.add, ["mg_m", "mg_mt"], ["mg_m"])
                mgb = psb("mg_mb", [128, 8, 128], BF16)
                self.cp("pool", mgb[:], mg[:], ["mg_m"], ["mg_mb"])
                x, xn = g("x")
                xt_ = psb("mg_xt", [128, D])
                for half in range(2):
                    pn = f"P{4 + half}"
                    for kc in range(8):
                        self.mm(t[pn][:], mgb[:, kc, :], wo[:, kc, half * 512:(half + 1) * 512], ["mg_mb", "mg_wo"], [pn],
                                start=(kc == 0), stop=(kc == 7))
                    self.tt("dve", xt_[:, half * 512:(half + 1) * 512], t[pn][:], t[f"mg_g1{v}"][:, half * 512:(half + 1) * 512], ALU.mult,
                            [pn, f"mg_g1{v}"], ["mg_xt"])
                self.tt("pool", x[:], x[:], xt_[:], ALU.add, [xn, "mg_xt"], [xn])
                self.dma("act", self.scr["X"][ti * 128:(ti + 1) * 128, :], x[:], [xn], [f"X@{ti}"])

            loads(tiles[0], 0)
            for n, ti in enumerate(tiles):
                if n + 1 < len(tiles):
                    loads(tiles[n + 1], (n + 1) % 2)
                compute(ti, n % 2)

    def ffn_phase(self, l, moe):
        S, t = self.S, self.t
        T, NT = self.T, self.NT
        self._lc = 0
        GF = 4
        if moe:
            NL = self.NLOC
            tiles = list(range(NL))
            TT_ = NL * 128
            if "XL" not in self.scr:
                self.dscr("XL", [TT_, D])
            xs, xtag = self.scr["XL"], "XL"
        else:
            tiles = list(range(NT))
            TT_ = T
            xs, xtag = self.scr["X"], "X"
        tbs = [(s_, min(512, TT_ - s_)) for s_ in range(0, TT_, 512)]
        with self.phase() as ph:
            psb = lambda name, shape, dt=F32: (self.sb(ph, name, shape, dt) if name not in t else t[name])
            cmbt = psb("ff_cmbt", [128, self.NLOC, NE]) if moe else None
            hT = self.norm_phase(ph, l, True, tiles, router=moe, local=cmbt)
            self.load_mod(ph, l, 5, "ff_g2")
            ag = psb("ff_a", [128, GF, TT_], BF16)
            w2g = psb("ff_w2", [128, GF, D], BF16)
            experts = list(range(NE)) if moe else [None]
            cnt = 0
            for e in experts:
                if moe:
                    w1s, w3s, w2s = self.w["moe_w1"][0, e], self.w["moe_w3"][0, e], self.w["moe_w2"][0, e]
                else:
                    w1s, w3s, w2s = self.w["ffn_w1"][0], self.w["ffn_w3"][0], self.w["ffn_w2"][0]
                w1v = w1s.rearrange("(kc p) f -> p kc f", p=128)
                w3v = w3s.rearrange("(kc p) f -> p kc f", p=128)
                w2v = w2s.rearrange("(fc p) n -> p fc n", p=128)
                for grp in range(FF // 128 // GF):
                    for fi in range(GF):
                        fc = grp * GF + fi
                        i = cnt % 2
                        cnt += 1
                        wf = psb(f"ff_wf{i}", [128, 2, 8, 128])
                        wb = psb(f"ff_wb{i}", [128, 2, 8, 128], BF16)
                        self.dma("sp", wf[:, 0, :, :], w1v[:, :, fc * 128:(fc + 1) * 128], (), [f"ff_wf{i}"])
                        self.dma("sp", wf[:, 1, :, :], w3v[:, :, fc * 128:(fc + 1) * 128], (), [f"ff_wf{i}"])
                        self.cp("pool", wb[:], wf[:], [f"ff_wf{i}"], [f"ff_wb{i}"])
                        for bi_, (s0, sl) in enumerate(tbs):
                            pa, pb_ = f"P{(bi_ % 2) * 2}", f"P{(bi_ % 2) * 2 + 1}"
                            for kc in range(8):
                                self.mm(t[pa][:, 0:sl], wb[:, 0, kc, :], hT[:, kc, s0:s0 + sl], [f"ff_wb{i}", "hT"], [pa], start=(kc == 0), stop=(kc == 7))
                            for kc in range(8):
                                self.mm(t[pb_][:, 0:sl], wb[:, 1, kc, :], hT[:, kc, s0:s0 + sl], [f"ff_wb{i}", "hT"], [pb_], start=(kc == 0), stop=(kc == 7))
                            sg = psb(f"ff_s{bi_ % 2}", [128, 512])
                            sgn = f"ff_s{bi_ % 2}"
                            self.act(sg[:, 0:sl], t[pa][:, 0:sl], AF.Silu, [pa], [sgn])
                            self.tt("dve", ag[:, fi, s0:s0 + sl], sg[:, 0:sl], t[pb_][:, 0:sl], ALU.mult, [sgn, pb_], ["ff_a"])
                    self.load_cast(ph, w2g, "ff_w2", w2v[:, grp * GF:(grp + 1) * GF, :], GF, D, 1)
                    for n, ti in enumerate(tiles):
                        v = 1 if (not moe and self.is_ctx(ti)) else 0
                        xn = f"ff_x{n % 2}"
                        x = psb(xn, [128, D])
                        self.dma("sp", x[:], xs[ti * 128:(ti + 1) * 128, :], [f"{xtag}@{ti}"], [xn])
                        xt_ = psb("ff_xt", [128, D])
                        for half in range(2):
                            pn = f"P{4 + (n % 2) * 2 + half}"
                            for fi in range(GF):
                                self.mm(t[pn][:], ag[:, fi, ti * 128:(ti + 1) * 128], w2g[:, fi, half * 512:(half + 1) * 512], ["ff_a", "ff_w2"], [pn],
                                        start=(fi == 0), stop=(fi == GF - 1))
                            g2 = t[f"ff_g2{v}"][:, half * 512:(half + 1) * 512]
                            if moe:
                                self.stt(xt_[:, half * 512:(half + 1) * 512], t[pn][:], cmbt[:, ti, e:e + 1], g2, ALU.mult, ALU.mult,
                                         [pn, f"ff_g2{v}", "ff_cmbt"], ["ff_xt"])
                            else:
                                self.tt("dve", xt_[:, half * 512:(half + 1) * 512], t[pn][:], g2, ALU.mult, [pn, f"ff_g2{v}"], ["ff_xt"])
                        self.tt("dve", x[:], x[:], xt_[:], ALU.add, [xn, "ff_xt"], [xn])
                        self.dma("pool", xs[ti * 128:(ti + 1) * 128, :], x[:], [xn], [f"{xtag}@{ti}"])

    def final_phase(self):
        S, t = self.S, self.t
        with self.phase() as ph:
            psb = lambda name, shape, dt=F32: (self.sb(ph, name, shape, dt) if name not in t else t[name])
            g = psb("fn_g", [128, D])
            self.dma("sp", g[:], pbc(self.w["final_norm_g"].rearrange("(o d) -> o d", o=1)), (), ["fn_g"])
            if "XL" not in self.scr:
                self.dscr("XL", [self.NLOC * 128, D])
                idxt = psb("fn_idx", [128, self.NLOC], I32)
                self.dma("sp", idxt[:], self.idx_in, (), ["fn_idx"])
                for lt in range(self.NLOC):
                    g_ = psb(f"fn_g{lt % 2}", [128, D])
                    self.S.dma_gather(g_[:], self.scr["X"], idxt[:, lt:lt + 1], ["fn_idx"] + [f"X@{k}" for k in range(self.NT)], [f"fn_g{lt % 2}"])
                    self.dma("sp", self.scr["XL"][lt * 128:(lt + 1) * 128, :], g_[:], [f"fn_g{lt % 2}"], [f"XL@{lt}"])
            for n, ti in enumerate(range(self.NLOC)):
                xn, jn, sn = f"fn_x{n % 2}", f"fn_j{n % 2}", f"fn_s{n % 2}"
                x, junk, ss = psb(xn, [128, D]), psb(jn, [128, D]), psb(sn, [128, 1])
                self.dma("sp", x[:], self.scr["XL"][ti * 128:(ti + 1) * 128, :], [f"XL@{ti}"], [xn])
                self.act(junk[:], x[:], AF.Square, [xn], [jn, sn], accum=ss[:])
                self.ts("dve", ss[:], ss[:], 1.0 / D, ALU.mult, [sn], [sn], s2=EPS, op1=ALU.add)
                self.act(ss[:], ss[:], AF.Sqrt, [sn], [sn])
                S.op("dve", lambda e, ss=ss: e.reciprocal(out=ss[:], in_=ss[:]), [sn], [sn])
                self.stt(junk[:], x[:], ss[:], g[:], ALU.mult, ALU.mult, [xn, sn, "fn_g"], [jn])
                self.dma("pool", self.out[ti * 128:(ti + 1) * 128, :], junk[:], [jn], ["out"])
            self.S.finish(["out"])

    def mixer(self, l, sub=None):
        with self.phase() as ph:
            self.norm_phase(ph, l, False, list(range(self.NT)))
            if sub == "norm":
                return
            self.proj_phase(ph, l)
        if sub == "proj":
            return
        self.rec_pass(l, 0)
        if sub == "rec0":
            return
        self.rec_pass(l, 1)
        if sub == "rec1":
            return
        self.merge_phase(l, last=(l == 1))

    def build_all(self, upto="all"):
        stages = ["setup", "mod0", "mix0", "ffn0", "mod1", "mix1", "ffn1"]
        self.setup()
        if upto != "setup":
            self.mod_phase(0)
            if upto != "mod0":
                self.mixer(0, upto[4:] if upto.startswith("sub0") else None)
                if upto != "mix0" and not upto.startswith("sub0"):
                    self.ffn_phase(0, moe=False)
                    if upto != "ffn0":
                        self.mod_phase(1)
                        self.mixer(1)
                        if upto != "mix1":
                            self.ffn_phase(1, moe=True)
        self.final_phase()
        self.st.close()
        return self.nc


_NC_CACHE = {}


def _build_full():
    if "nc" not in _NC_CACHE:
        k = KB4(nct=2, nlt=32)
        _NC_CACHE["nc"] = k.build_all("all")
    return _NC_CACHE["nc"]


def kernel(**inputs):
    x = np.asarray(inputs["x"], dtype=np.float32)
    c = np.asarray(inputs["c"], dtype=np.float32)
    ctx = np.asarray(inputs["ctx"], dtype=np.float32)
    c_ctx = np.asarray(inputs["c_ctx"], dtype=np.float32)
    nb = x.shape[0]
    shared = {}
    for name, shape in WSPECS:
        shared[name] = np.ascontiguousarray(np.asarray(inputs[name], dtype=np.float32)).reshape(shape)
    cctxcol = np.ascontiguousarray(c_ctx.reshape(8, 128).T)
    in_maps = []
    nloc = 16
    for core in range(8):
        b, half = core % nb, core // nb
        m = dict(shared)
        m["xin"] = np.ascontiguousarray(np.concatenate([ctx[b], x[b]], axis=0))
        m["ccol"] = np.ascontiguousarray(c[b].reshape(8, 128).T)
        m["cctxcol"] = cctxcol
        m["idx"] = (256 + half * nloc * 128 + np.arange(nloc)[None, :] * 128 + np.arange(128)[:, None]).astype(np.int32)
        in_maps.append(m)
    nc = _build_full()
    res = run_bass_kernel_spmd(nc, in_maps, core_ids=list(range(8)))
    out = np.stack([np.concatenate([np.asarray(res.results[b + nb * h]["out"], dtype=np.float32) for h in range(2)], axis=0)
                    for b in range(nb)], axis=0)
    return out
```

```python
import math
import numpy as np
from contextlib import ExitStack, contextmanager
import concourse.bass as bass
import concourse.mybir as mybir
from concourse.bass_utils import run_bass_kernel_spmd


def pbc(ap):
    b = ap.partition_broadcast(128)
    if len(b.shape) == 3 and b.shape[1] == 1:
        return b[:, 0, :]
    return b


F32 = mybir.dt.float32
BF16 = mybir.dt.bfloat16
I32 = mybir.dt.int32
AF = mybir.ActivationFunctionType
ALU = mybir.AluOpType
AX = mybir.AxisListType

ENGS = ("pe", "act", "dve", "pool", "sp")
EPOCH = 50000
NDMASEM = 4
DEPOCH = 2000


class Buf:
    __slots__ = ("name", "w", "r")

    def __init__(self, name):
        self.name = name
        self.w = None
        self.r = []


class Sched:
    def __init__(self, nc, stack):
        self.nc = nc
        self.stack = stack
        self.eng = {"pe": nc.tensor, "act": nc.scalar, "dve": nc.vector, "pool": nc.gpsimd, "sp": nc.sync}
        self.q = {e: [] for e in ENGS}
        self.cnt = {e: 0 for e in ENGS}
        self.sems = {e: [] for e in ENGS}
        self.dcnt = {e: 0 for e in ENGS}
        self.dsems = {e: None for e in ENGS}
        self.seen = {e: {} for e in ENGS}
        self.bufs = {}

    def buf(self, name):
        b = self.bufs.get(name)
        if b is None:
            b = self.bufs[name] = Buf(name)
        return b

    def _sem(self, name):
        return self.stack.enter_context(self.nc.semaphore(name))

    def _esem(self, e, epoch):
        while len(self.sems[e]) <= epoch:
            self.sems[e].append(self._sem(f"s_{e}_{len(self.sems[e])}"))
        return self.sems[e][epoch]

    def _dsem(self, e, n):
        if self.dsems[e] is None:
            self.dsems[e] = []
        setsz = NDMASEM * DEPOCH
        si, r = divmod(n, setsz)
        while len(self.dsems[e]) <= si:
            k = len(self.dsems[e])
            self.dsems[e].append([self._sem(f"d_{e}_{k}_{j}") for j in range(NDMASEM)])
        return self.dsems[e][si][r % NDMASEM], 16 * (r // NDMASEM + 1)

    def _wait(self, e, ev):
        if ev is None:
            return
        kind, src, n = ev
        if kind == "c":
            if src == e and e == "pe":
                return
            key = ("c", src)
            if self.seen[e].get(key, 0) >= n:
                return
            self.seen[e][key] = n
            ep, loc = divmod(n - 1, EPOCH)
            sem = self._esem(src, ep)
            val = loc + 1
        else:
            key = ("d", src, n % NDMASEM)
            if self.seen[e].get(key, -1) >= n:
                return
            self.seen[e][key] = n
            sem, val = self._dsem(src, n)
        self.q[e].append(lambda eng, sem=sem, val=val: eng.wait_ge(sem, val))

    def _deps(self, e, reads, writes):
        for b in reads:
            if b.w is not None:
                self._wait(e, b.w)
        for b in writes:
            if b.w is not None:
                self._wait(e, b.w)
            for ev in b.r:
                self._wait(e, ev)

    def _commit(self, ev, reads, writes):
        for b in reads:
            b.r.append(ev)
            if len(b.r) > 64:
                b.r = b.r[-64:] if False else b.r
        for b in writes:
            b.w = ev
            b.r = []

    def op(self, e, fn, reads=(), writes=()):
        reads = [self.buf(b) if isinstance(b, str) else b for b in reads]
        writes = [self.buf(b) if isinstance(b, str) else b for b in writes]
        self._deps(e, reads, writes)
        self.cnt[e] += 1
        n = self.cnt[e]
        ep, loc = divmod(n - 1, EPOCH)
        sem = self._esem(e, ep)
        import sys as _sys
        fr = _sys._getframe(1)
        tag = []
        while fr is not None and len(tag) < 4:
            tag.append(f"{fr.f_code.co_name}:{fr.f_lineno}")
            fr = fr.f_back

        def _emit(eng, fn=fn, sem=sem, tag=tag):
            try:
                fn(eng).then_inc(sem, 1)
            except Exception:
                print("EMIT FAILED at", tag, flush=True)
                raise
        self.q[e].append(_emit)
        self._commit(("c", e, n), reads, writes)

    def dma(self, e, out, in_, reads=(), writes=(), **kw):
        reads = [self.buf(b) if isinstance(b, str) else b for b in reads]
        writes = [self.buf(b) if isinstance(b, str) else b for b in writes]
        self._deps(e, reads, writes)
        n = self.dcnt[e]
        self.dcnt[e] += 1
        if n >= NDMASEM:
            self._wait(e, ("d", e, n - NDMASEM))
        sem, _v = self._dsem(e, n)
        self.q[e].append(lambda eng, out=out, in_=in_, sem=sem, kw=kw: eng.dma_start(out=out, in_=in_, **kw).then_inc(sem, 16))
        self._commit(("d", e, n), reads, writes)

    def dma_gather(self, out, in_, idx_ap, reads=(), writes=()):
        e = "pool"
        reads = [self.buf(b) if isinstance(b, str) else b for b in reads]
        writes = [self.buf(b) if isinstance(b, str) else b for b in writes]
        self._deps(e, reads, writes)
        n = self.dcnt[e]
        self.dcnt[e] += 1
        if n >= NDMASEM:
            self._wait(e, ("d", e, n - NDMASEM))
        sem, _v = self._dsem(e, n)
        self.q[e].append(lambda eng, out=out, in_=in_, idx_ap=idx_ap, sem=sem: eng.indirect_dma_start(
            out=out, out_offset=None, in_=in_, in_offset=bass.IndirectOffsetOnAxis(ap=idx_ap, axis=0)).then_inc(sem, 16))
        self._commit(("d", e, n), reads, writes)

    def finish(self, final_bufs):
        for b in final_bufs:
            b = self.buf(b) if isinstance(b, str) else b
            self._wait("sp", b.w)
        for e in ENGS:
            for n in range(max(0, self.dcnt[e] - NDMASEM), self.dcnt[e]):
                self._wait("sp", ("d", e, n))
        for e in ENGS:
            if e != "sp" and self.cnt[e] > 0:
                self._wait("sp", ("c", e, self.cnt[e]))

    def barrier(self):
        for e in ENGS:
            for f in ENGS:
                if f != e and self.cnt[f] > 0:
                    self._wait(e, ("c", f, self.cnt[f]))
            for f in ENGS:
                for n in range(max(0, self.dcnt[f] - NDMASEM), self.dcnt[f]):
                    self._wait(e, ("d", f, n))

    def emit(self):
        nc = self.nc
        if not any(self.q[e] for e in ENGS):
            return
        with nc.Block() as block:
            @block.tensor
            def _(eng):
                for f in self.q["pe"]:
                    f(eng)

            @block.scalar
            def _(eng):
                for f in self.q["act"]:
                    f(eng)

            @block.vector
            def _(eng):
                for f in self.q["dve"]:
                    f(eng)

            @block.gpsimd
            def _(eng):
                for f in self.q["pool"]:
                    f(eng)

            @block.sync
            def _(eng):
                for f in self.q["sp"]:
                    f(eng)
        self.q = {e: [] for e in ENGS}


D = 1024
FF = 3584
NE = 8
NIN = 7712
EPS = 1e-6
OFF = dict(hg_q=0, hg_ff=512, hg_fb=1024, hg_i=1536, hg_g=2048, gl_q=2560, gl_k=2816, gl_v=3072,
           gl_df=3584, gl_db=3600, gl_r=3616, s5_u=4128, ga=4640, gb=5664, gc=6688)
TWO_PI = 6.283185307179586
PI_SAFE = 3.1415925

WSPECS = [("norm1_g", [2, D]), ("norm2_g", [2, D]), ("ada_w", [2, D, 6 * D]), ("ada_b", [2, 6 * D]),
          ("w_in", [2, D, NIN]), ("hg_lb_logits", [2, 512]), ("hg_norm_g", [2, 512]),
          ("gla_gate_up", [2, 2, 16, 256]), ("gla_gate_b", [2, 2, 256]), ("gla_norm_g", [2, 512]),
          ("s5_a_re", [2, 2, 32, 64]), ("s5_a_im", [2, 2, 32, 64]), ("s5_log_step", [2, 2, 32]),
          ("s5_b_re", [2, 2, 32, 64, 16]), ("s5_b_im", [2, 2, 32, 64, 16]),
          ("s5_c_re", [2, 2, 32, 16, 64]), ("s5_c_im", [2, 2, 32, 16, 64]),
          ("s5_d", [2, 512]), ("s5_glu_w", [2, 512, 512]), ("branch_proj", [2, 3, 512, D]),
          ("w_out", [2, D, D]), ("ffn_w1", [1, D, FF]), ("ffn_w3", [1, D, FF]), ("ffn_w2", [1, FF, D]),
          ("router_w", [1, D, NE]), ("moe_w1", [1, NE, D, FF]), ("moe_w3", [1, NE, D, FF]),
          ("moe_w2", [1, NE, FF, D]), ("final_norm_g", [D])]


class KB:
    def __init__(self, nct=2, nlt=32, dumps=(), stop=None):
        self.nct, self.nlt = nct, nlt
        self.NT = nct + nlt
        self.T = self.NT * 128
        self.dumps = set(dumps)
        self.stop = stop
        self.nc = nc = bass.Bass("TRN2", target_bir_lowering=False)
        self.st = ExitStack()
        self.S = Sched(nc, self.st)
        self.t = {}
        din = lambda name, shape: nc.dram_tensor(name, list(shape), F32, kind="ExternalInput").ap()
        self.xin = din("xin", [self.T, D])
        self.ccol = din("ccol", [128, 8])
        self.cctxcol = din("cctxcol", [128, 8])
        self.w = {name: din(name, shape) for name, shape in WSPECS}
        self.NLOC = nlt // 2
        self.idx_in = nc.dram_tensor("idx", [128, self.NLOC], I32, kind="ExternalInput").ap()
        self.out = nc.dram_tensor("out", [self.NLOC * 128, D], F32, kind="ExternalOutput").ap()
        self.scr = {}

    def dscr(self, name, shape, dt=F32):
        kind = "ExternalOutput" if name in self.dumps else "Internal"
        ap = self.nc.dram_tensor(name, list(shape), dt, kind=kind).ap()
        self.scr[name] = ap
        return ap

    def sb(self, stack, name, shape, dt=F32):
        self._uid = getattr(self, "_uid", 0) + 1
        h = stack.enter_context(self.nc.sbuf_tensor(f"{name}_u{self._uid}", list(shape), dt))
        self.t[name] = h
        if hasattr(stack, "_names"):
            stack._names.append(name)
        return h

    @contextmanager
    def phase(self):
        with ExitStack() as ph:
            ph._names = []
            yield ph
            self.phase_end()
            for n in ph._names:
                self.t.pop(n, None)

    def mm(self, out, lhsT, rhs, r, w, start=True, stop=True):
        self.S.op("pe", lambda e: e.matmul(out, lhsT=lhsT, rhs=rhs, start=start, stop=stop), r, w)

    def tr(self, out, in_, r, w):
        ident = self.t["ident"]
        self.S.op("pe", lambda e: e.transpose(out, in_, ident[:]), list(r) + ["ident"], w)

    def act(self, out, in_, func, r, w, bias=None, scale=None, accum=None):
        kw = {}
        if bias is not None:
            kw["bias"] = bias
        if scale is not None:
            kw["scale"] = scale
        if accum is not None:
            kw["accum_out"] = accum
        self.S.op("act", lambda e: e.activation(out=out, in_=in_, func=func, **kw), r, w)

    def tt(self, eng, out, in0, in1, op, r, w):
        self.S.op(eng, lambda e: e.tensor_tensor(out=out, in0=in0, in1=in1, op=op), r, w)

    def ts(self, eng, out, in0, s1, op0, r, w, s2=None, op1=None):
        if op1 is None:
            self.S.op(eng, lambda e: e.tensor_scalar(out=out, in0=in0, scalar1=s1, scalar2=None, op0=op0), r, w)
        else:
            self.S.op(eng, lambda e: e.tensor_scalar(out=out, in0=in0, scalar1=s1, scalar2=s2, op0=op0, op1=op1), r, w)

    def stt(self, out, in0, scalar, in1, op0, op1, r, w, accum=None):
        if accum is None:
            self.S.op("dve", lambda e: e.scalar_tensor_tensor(out=out, in0=in0, scalar=scalar, in1=in1, op0=op0, op1=op1), r, w)
        else:
            self.S.op("dve", lambda e: e.scalar_tensor_tensor(out=out, in0=in0, scalar=scalar, in1=in1, op0=op0, op1=op1, accum_out=accum), r, w)

    def cp(self, eng, out, in_, r, w):
        if eng == "act":
            self.S.op("act", lambda e: e.activation(out=out, in_=in_, func=AF.Identity), r, w)
        else:
            self.S.op(eng, lambda e: e.tensor_copy(out=out, in_=in_), r, w)

    def memset(self, eng, ap, val, w):
        self.S.op(eng, lambda e: e.memset(ap, val), (), w)

    def dma(self, q, out, in_, r, w, **kw):
        self.S.dma(q, out, in_, r, w, **kw)

    def phase_end(self):
        self.S.barrier()
        self.S.emit()

    def is_ctx(self, ti):
        return ti < self.nct

    def setup(self):
        S, t, st = self.S, self.t, self.st
        sb = lambda name, shape, dt=F32: self.sb(st, name, shape, dt)
        for i in range(8):
            h = st.enter_context(self.nc.psum_tensor(f"P{i}", [128, 512], F32))
            t[f"P{i}"] = h
        self.dscr("X", [self.T, D])
        di = sb("c_di", [128, 128], I32)
        df = sb("c_df", [128, 128])
        S.op("pool", lambda e: e.iota(di[:], pattern=[[1, 128]], base=0, channel_multiplier=-1), (), ["c_di"])
        self.cp("dve", df[:], di[:], ["c_di"], ["c_df"])
        for name, op in (("ident", ALU.is_equal), ("minc0", ALU.is_ge), ("minc1", ALU.is_le),
                         ("mstr0", ALU.is_lt), ("mstr1", ALU.is_gt)):
            h = sb(name, [128, 128])
            S.op("dve", lambda e, h=h, op=op: e.tensor_single_scalar(out=h[:], in_=df[:], scalar=0.0, op=op), ["c_df"], [name])
        for d in (0, 1):
            h = sb(f"mincb{d}", [128, 128], BF16)
            self.cp("dve", h[:], t[f"minc{d}"][:], [f"minc{d}"], [f"mincb{d}"])
        for hh in (0, 1):
            hm = sb(f"hm{hh}", [128, 1])
            self.memset("pool", hm[:], 0.0, [f"hm{hh}"])
            self.memset("pool", hm[hh * 64:(hh + 1) * 64, :], 1.0, [f"hm{hh}"])
        ones = sb("ones", [128, 128])
        self.memset("pool", ones[:], 1.0, ["ones"])
        for k in range(4):
            m = sb(f"mask{k}", [128, 128])
            self.memset("pool", m[:], 0.0, [f"mask{k}"])
            self.memset("pool", m[0:64, (2 * k) * 16:(2 * k + 1) * 16], 1.0, [f"mask{k}"])
            self.memset("pool", m[64:128, (2 * k + 1) * 16:(2 * k + 2) * 16], 1.0, [f"mask{k}"])
        lg = self.w["hg_lb_logits"]
        for nm, shape in (("fm", [128, 4]), ("tm", [128, 512])):
            for l in (0, 1):
                sb(f"lb_{nm}{l}", shape)
                sb(f"oml_{nm}{l}", shape)
        with self.phase() as ph0:
            lfm = self.sb(ph0, "lb_lfm", [128, 2, 4])
            self.dma("sp", lfm[:], lg.rearrange("l (h p) -> p l h", p=128), (), ["lb_lfm"], allow_slow_non_contiguous=True)
            ltm = self.sb(ph0, "lb_ltm", [128, 2, 512])
            self.dma("sp", ltm[:], lg.partition_broadcast(128), (), ["lb_ltm"])
            for nm, shape, src in (("fm", [128, 4], lfm), ("tm", [128, 512], ltm)):
                self.memset("pool", t[f"lb_{nm}0"][:], 0.0, [f"lb_{nm}0"])
                self.memset("pool", t[f"oml_{nm}0"][:], 1.0, [f"oml_{nm}0"])
                dtile = self.sb(ph0, f"lb_d{nm}", shape)
                self.tt("dve", dtile[:], src[:, 1, :], src[:, 0, :], ALU.subtract, [f"lb_l{nm}"], [f"lb_d{nm}"])
                self.act(t[f"lb_{nm}1"][:], dtile[:], AF.Sigmoid, [f"lb_d{nm}"], [f"lb_{nm}1"])
                self.act(t[f"oml_{nm}1"][:], dtile[:], AF.Sigmoid, [f"lb_d{nm}"], [f"oml_{nm}1"], scale=-1.0)
        with self.phase() as ph:
            psb = lambda name, shape, dt=F32: self.sb(ph, name, shape, dt)
            ji = psb("pe_ji", [128, 256], I32)
            om2 = psb("pe_om2", [128, 512])
            ph2 = psb("pe_ph", [128, 512])
            S.op("pool", lambda e: e.iota(ji[:], pattern=[[1, 256]], base=0, channel_multiplier=0), (), ["pe_ji"])
            self.cp("dve", om2[:, 0:256], ji[:], ["pe_ji"], ["pe_om2"])
            self.act(om2[:, 0:256], om2[:, 0:256], AF.Exp, ["pe_om2"], ["pe_om2"], scale=-math.log(10000.0) / 256.0)
            self.cp("dve", om2[:, 256:512], om2[:, 0:256], ["pe_om2"], ["pe_om2"])
            self.memset("pool", ph2[:, 0:256], 0.0, ["pe_ph"])
            self.memset("pool", ph2[:, 256:512], math.pi / 2, ["pe_ph"])
            pi_ = psb("pe_pi", [128, 1], I32)
            pf = psb("pe_pf", [128, 1])
            hi = psb("pe_hi", [128, 1])
            cpos = psb("pe_c", [128, 1])
            S.op("pool", lambda e: e.iota(pi_[:], pattern=[[0, 1]], base=0, channel_multiplier=1), (), ["pe_pi"])
            self.cp("dve", pf[:], pi_[:], ["pe_pi"], ["pe_pf"])
            self.ts("dve", hi[:], pf[:], 64.0, ALU.is_ge, ["pe_pf"], ["pe_hi"])
            self.stt(cpos[:], hi[:], -64.0, pf[:], ALU.mult, ALU.add, ["pe_hi", "pe_pf"], ["pe_c"])

            def sincos(dst, scal, tag):
                a = t["pe_a"]
                b = t["pe_b"]
                bi = t["pe_bi"]
                self.stt(a[:], om2[:], scal, ph2[:], ALU.mult, ALU.add, ["pe_om2", "pe_ph", tag], ["pe_a"])
                self.ts("dve", b[:], a[:], 1.0 / TWO_PI, ALU.mult, ["pe_a"], ["pe_b"])
                self.cp("dve", bi[:], b[:], ["pe_b"], ["pe_bi"])
                self.cp("dve", b[:], bi[:], ["pe_bi"], ["pe_b"])
                self.stt(a[:], b[:], -TWO_PI, a[:], ALU.mult, ALU.add, ["pe_b", "pe_a"], ["pe_a"])
                self.ts("dve", a[:], a[:], PI_SAFE, ALU.min, ["pe_a"], ["pe_a"], s2=-PI_SAFE, op1=ALU.max)
                self.act(dst, a[:], AF.Sin, ["pe_a"], [tag + "_o"])

            psb("pe_a", [128, 512])
            psb("pe_b", [128, 512])
            psb("pe_bi", [128, 512], I32)
            colp = psb("pe_col", [128, 512])
            S.buf("pe_c_o")
            sincos(colp[:], cpos[:], "pe_c")
            rsc = psb("pe_r", [128, 1])
            for ti in range(self.NT):
                xt = psb(f"pe_x{ti % 2}", [128, D]) if ti < 2 else t[f"pe_x{ti % 2}"]
                nm = f"pe_x{ti % 2}"
                self.dma("sp", xt[:], self.xin[ti * 128:(ti + 1) * 128, :], (), [nm])
                if not self.is_ctx(ti):
                    li = ti - self.nct
                    rowp = psb(f"pe_row{ti % 2}", [128, 512]) if f"pe_row{ti % 2}" not in t else t[f"pe_row{ti % 2}"]
                    self.ts("dve", rsc[:], hi[:], float(2 * li), ALU.add, ["pe_hi"], ["pe_r"])
                    S.buf("pe_r_o")
                    sincos(rowp[:], rsc[:], "pe_r")
                    self.tt("dve", xt[:, 0:512], xt[:, 0:512], rowp[:], ALU.add, [nm, "pe_r_o"], [nm])
                    self.tt("pool", xt[:, 512:1024], xt[:, 512:1024], colp[:], ALU.add, [nm, "pe_c_o"], [nm])
                self.dma("pool", self.scr["X"][ti * 128:(ti + 1) * 128, :], xt[:], [nm], [f"X@{ti}"])

    def mod_phase(self, l):
        S, t = self.S, self.t
        if "MODS" not in self.scr:
            self.dscr("MODS", [2, 2, 6 * D])
        with self.phase() as ph:
            psb = lambda name, shape, dt=F32: self.sb(ph, name, shape, dt)
            cb = {}
            for v, src in ((0, self.ccol), (1, self.cctxcol)):
                c = psb(f"md_c{v}", [128, 8])
                self.dma("sp", c[:], src, (), [f"md_c{v}"])
                self.act(c[:], c[:], AF.Silu, [f"md_c{v}"], [f"md_c{v}"])
                cbt = psb(f"md_cb{v}", [128, 8, 128])
                self.cp("dve", cbt[:], c[:].unsqueeze(2).to_broadcast([128, 8, 128]), [f"md_c{v}"], [f"md_cb{v}"])
                cb[v] = cbt
            bb = psb("md_b", [128, 6 * D])
            self.dma("sp", bb[:], pbc(self.w["ada_b"][l:l + 1, :]), (), ["md_b"])
            wsrc = self.w["ada_w"][l].rearrange("(kc p) n -> p kc n", p=128)
            for cbk in range(12):
                wn = f"md_w{cbk % 2}"
                wt = psb(wn, [128, 8, 512]) if wn not in t else t[wn]
                self.dma("sp", wt[:], wsrc[:, :, cbk * 512:(cbk + 1) * 512], (), [wn])
                for v in (0, 1):
                    pn = f"P{v}"
                    for kc in range(8):
                        self.mm(t[pn][:], cb[v][:, kc, :], wt[:, kc, :], [f"md_cb{v}", wn], [pn], start=(kc == 0), stop=(kc == 7))
                    on = f"md_o{v}"
                    ot = psb(on, [128, 512]) if on not in t else t[on]
                    self.tt("dve", ot[:], t[pn][:], bb[:, cbk * 512:(cbk + 1) * 512], ALU.add, [pn, "md_b"], [on])
                    self.dma("pool", self.scr["MODS"][l, v:v + 1, cbk * 512:(cbk + 1) * 512], ot[0:1, :], [on], [f"MODS@{l}"])

    def load_mod(self, stack, l, which, tag):
        for v in (0, 1):
            h = self.sb(stack, f"{tag}{v}", [128, D])
            self.dma("sp", h[:], pbc(self.scr["MODS"][l, v:v + 1, which * D:(which + 1) * D]),
                     [f"MODS@{l}"], [f"{tag}{v}"])

    def norm_phase(self, stack, l, second, tiles, router=False, local=None):
        hT = self.sb(stack, "hT", [128, 8, self.T if local is None else self.NLOC * 128], BF16)
        with self.phase() as inner:
            self._norm_phase(inner, l, second, tiles, router, hT, local)
        return hT

    def _norm_phase(self, stack, l, second, tiles, router, hT, local=None):
        S, t = self.S, self.t
        psb = lambda name, shape, dt=F32: self.sb(stack, name, shape, dt)
        pre = "n2" if second else "n1"
        self.load_mod(stack, l, 4 if second else 1, pre + "sc")
        self.load_mod(stack, l, 3 if second else 0, pre + "sh")
        gn = psb(pre + "g", [128, D])
        gsrc = self.w["norm2_g" if second else "norm1_g"]
        self.dma("sp", gn[:], pbc(gsrc[l:l + 1, :]), (), [pre + "g"])
        for v in (0, 1):
            self.stt(t[f"{pre}sc{v}"][:], t[f"{pre}sc{v}"][:], 1.0, gn[:], ALU.add, ALU.mult, [f"{pre}sc{v}", pre + "g"], [f"{pre}sc{v}"])
        _sk = ""
        if router:
            wr = psb("rt_w", [128, 8, 128])
            self.memset("pool", wr[:], 0.0, ["rt_w"])
            if "noW" not in _sk:
                self.dma("sp", wr[:, :, 0:NE], self.w["router_w"][0].rearrange("(kc p) e -> p kc e", p=128), (), ["rt_w"])
            pass
        if local is not None:
            idxt = psb("n_idx", [128, self.NLOC], I32)
            self.dma("sp", idxt[:], self.idx_in, (), ["n_idx"])
        for ti in tiles:
            v = 1 if (local is None and self.is_ctx(ti)) else 0
            xn = f"nx{ti % 2}"
            xt = psb(xn, [128, D]) if xn not in t else t[xn]
            if local is None:
                self.dma("sp", xt[:], self.scr["X"][ti * 128:(ti + 1) * 128, :], [f"X@{ti}"], [xn])
            else:
                self.S.dma_gather(xt[:], self.scr["X"], idxt[:, ti:ti + 1], ["n_idx"] + [f"X@{k}" for k in range(self.NT)], [xn])
                self.dma("sp", self.scr["XL"][ti * 128:(ti + 1) * 128, :], xt[:], [xn], [f"XL@{ti}"])
            jn = f"njunk{ti % 2}"
            junk = psb(jn, [128, D]) if jn not in t else t[jn]
            sn = f"nss{ti % 2}"
            ss = psb(sn, [128, 1]) if sn not in t else t[sn]
            self.act(junk[:], xt[:], AF.Square, [xn], [jn, sn], accum=ss[:])
            self.ts("dve", ss[:], ss[:], 1.0 / D, ALU.mult, [sn], [sn], s2=EPS, op1=ALU.add)
            self.act(ss[:], ss[:], AF.Sqrt, [sn], [sn])
            S.op("dve", lambda e, ss=ss: e.reciprocal(out=ss[:], in_=ss[:]), [sn], [sn])
            self.stt(junk[:], xt[:], ss[:], t[f"{pre}sc{v}"][:], ALU.mult, ALU.mult, [xn, sn, f"{pre}sc{v}"], [jn])
            self.tt("dve", junk[:], junk[:], t[f"{pre}sh{v}"][:], ALU.add, [jn, f"{pre}sh{v}"], [jn])
            for half in range(2):
                pn = f"P{half}"
                for k4 in range(4):
                    kc = half * 4 + k4
                    self.tr(t[pn][:, k4 * 128:(k4 + 1) * 128], junk[:, kc * 128:(kc + 1) * 128], [jn], [pn])
                eng = "act" if half == 0 else "dve"
                if router:
                    hn = f"rt_h{half}"
                    hf = psb(hn, [128, 4, 128]) if hn not in t else t[hn]
                    self.cp(eng, hf[:], t[pn][:].rearrange("p (k c) -> p k c", k=4), [pn], [hn])
                    self.cp("act", hT[:, half * 4:(half + 1) * 4, ti * 128:(ti + 1) * 128], hf[:], [hn], ["hT"])
                else:
                    self.cp(eng, hT[:, half * 4:(half + 1) * 4, ti * 128:(ti + 1) * 128],
                            t[pn][:].rearrange("p (k c) -> p k c", k=4), [pn], ["hT"])
            if router and "rtA" in "":
                continue
            if router:
                for kc in range(8):
                    self.mm(t["P2"][:, 0:128], t[f"rt_h{kc // 4}"][:, kc % 4, :], t["rt_w"][:, kc, :],
                            [f"rt_h{kc // 4}", "rt_w"], ["P2"], start=(kc == 0), stop=(kc == 7))
                self.router_post(stack, ti, local)
        return hT

    def router_post(self, stack, ti, cmbt):
        S, t = self.S, self.t
        import os as _os
        _sk = ""
        psb = lambda name, shape, dt=F32: (self.sb(stack, name, shape, dt) if name not in t else t[name])
        lgt = psb("rt_lg", [128, NE])
        m1 = psb("rt_m1", [128, 1])
        m2 = psb("rt_m2", [128, 1])
        eq = psb("rt_eq", [128, NE])
        ex = psb("rt_ex", [128, NE])
        den = psb("rt_den", [128, 1])
        self.cp("dve", lgt[:], t["P2"][:, 0:NE], ["P2"], ["rt_lg"])
        if "rt1" in _sk:
            return
        S.op("dve", lambda e: e.reduce_max(out=m1[:], in_=lgt[:], axis=AX.X), ["rt_lg"], ["rt_m1"])
        self.ts("dve", eq[:], lgt[:], m1[:], ALU.is_equal, ["rt_lg", "rt_m1"], ["rt_eq"])
        self.stt(eq[:], eq[:], -1e30, lgt[:], ALU.mult, ALU.add, ["rt_eq", "rt_lg"], ["rt_eq"])
        S.op("dve", lambda e: e.reduce_max(out=m2[:], in_=eq[:], axis=AX.X), ["rt_eq"], ["rt_m2"])
        self.ts("dve", eq[:], lgt[:], m2[:], ALU.is_ge, ["rt_lg", "rt_m2"], ["rt_eq"])
        self.ts("dve", m1[:], m1[:], -1.0, ALU.mult, ["rt_m1"], ["rt_m1"])
        self.act(ex[:], lgt[:], AF.Exp, ["rt_lg", "rt_m1"], ["rt_ex"], bias=m1[:])
        self.tt("dve", ex[:], ex[:], eq[:], ALU.mult, ["rt_ex", "rt_eq"], ["rt_ex"])
        S.op("dve", lambda e: e.reduce_sum(out=den[:], in_=ex[:], axis=AX.X), ["rt_ex"], ["rt_den"])
        S.op("dve", lambda e: e.reciprocal(out=den[:], in_=den[:]), ["rt_den"], ["rt_den"])
        self.ts("dve", cmbt[:, ti, :], ex[:], den[:], ALU.mult, ["rt_ex", "rt_den"], ["ff_cmbt"])


class KB2(KB):
    def proj_phase(self, stack, l):
        S, t = self.S, self.t
        T, NT = self.T, self.NT
        psb = lambda name, shape, dt=F32: (self.sb(stack, name, shape, dt) if name not in t else t[name])
        hT = t["hT"]
        for name, rows in (("QH", 512), ("KHF", 512), ("KHB", 512), ("GH", 512), ("QG", 256), ("KG", 256),
                           ("RG", 512), ("U5", 512), ("GA", D), ("GB", D), ("GC", D)):
            if name not in self.scr:
                self.dscr(name, [rows, T])
        for name, cols, dt in (("GHF", 512, F32), ("GHB", 512, F32), ("KHFt", 512, F32), ("KHBt", 512, F32),
                               ("VH", 512, BF16), ("KGt", 256, F32), ("VG", 512, BF16), ("GG", 512, F32)):
            if name not in self.scr:
                self.dscr(name, [T, cols], dt)
        wsrc = self.w["w_in"][l].rearrange("(kc p) n -> p kc n", p=128)
        tbs = [(s, min(512, T - s)) for s in range(0, T, 512)]
        self._wcnt = getattr(self, "_wcnt", 0)

        def load_w(c0, ncol):
            i = self._wcnt % 2
            self._wcnt += 1
            wf = psb(f"pw_f{i}", [128, 8, 512])
            wb = psb(f"pw_b{i}", [128, 8, 512], BF16)
            self.dma("sp", wf[:, :, 0:ncol], wsrc[:, :, c0:c0 + ncol], (), [f"pw_f{i}"])
            self.cp("pool", wb[:, :, 0:ncol], wf[:, :, 0:ncol], [f"pw_f{i}"], [f"pw_b{i}"])
            return wb, f"pw_b{i}"

        self._pcnt = getattr(self, "_pcnt", 0)

        def next_p():
            i = self._pcnt % 4
            self._pcnt += 1
            return t[f"P{i}"], f"P{i}"

        self._ocnt = getattr(self, "_ocnt", 0)

        def next_o(dt=F32):
            i = self._ocnt % 3
            self._ocnt += 1
            nm = f"po_{'b' if dt == BF16 else 'f'}{i}"
            return psb(nm, [128, 512], dt), nm

        lb, oml = t[f"lb_fm{l}"], t[f"oml_fm{l}"]
        lbt, omlt = t[f"lb_tm{l}"], t[f"oml_tm{l}"]

        fm = [(OFF["hg_q"], 512, "QH", "copy"), (OFF["hg_ff"], 512, "KHF", "hk"), (OFF["hg_fb"], 512, "KHB", "hk"),
              (OFF["hg_g"], 512, "GH", "silu"), (OFF["gl_q"], 256, "QG", "q8"), (OFF["gl_k"], 256, "KG", "copy"),
              (OFF["gl_r"], 512, "RG", "silu"), (OFF["s5_u"], 512, "U5", "copy"),
              (OFF["ga"], D, "GA", "sig"), (OFF["gb"], D, "GB", "sig"), (OFF["gc"], D, "GC", "sig")]
        for c0, ncols, sname, kind in fm:
            for cb0 in range(0, ncols, 512):
                nb = min(512, ncols - cb0)
                wb, wn = load_w(c0 + cb0, nb)
                for sub in range(nb // 128):
                    row0 = cb0 + sub * 128
                    for (s0, sl) in tbs:
                        P, pn = next_p()
                        for kc in range(8):
                            self.mm(P[:, 0:sl], wb[:, kc, sub * 128:(sub + 1) * 128], hT[:, kc, s0:s0 + sl],
                                    [wn, "hT"], [pn], start=(kc == 0), stop=(kc == 7))
                        o, on = next_o()
                        if kind == "copy":
                            self.cp("act", o[:, 0:sl], P[:, 0:sl], [pn], [on])
                        elif kind == "q8":
                            self.act(o[:, 0:sl], P[:, 0:sl], AF.Identity, [pn], [on], scale=0.125)
                        elif kind == "silu":
                            self.act(o[:, 0:sl], P[:, 0:sl], AF.Silu, [pn], [on])
                        elif kind == "sig":
                            self.act(o[:, 0:sl], P[:, 0:sl], AF.Sigmoid, [pn], [on])
                        elif kind == "hk":
                            h = row0 // 128
                            self.act(o[:, 0:sl], P[:, 0:sl], AF.Sigmoid, [pn], [on], scale=-1.0)
                            self.ts("dve", o[:, 0:sl], o[:, 0:sl], oml[:, h:h + 1], ALU.mult, [on, f"oml_fm{l}"], [on])
                        self.dma("sp", self.scr[sname][row0:row0 + 128, s0:s0 + sl], o[:, 0:sl], [on],
                                 [f"{sname}@{s0 // 128 + k}" for k in range(sl // 128)])
        tm = [(OFF["hg_ff"], 512, "hgf", ("GHF", "KHFt")), (OFF["hg_fb"], 512, "hgf", ("GHB", "KHBt")),
              (OFF["hg_i"], 512, "vbf", ("VH",)), (OFF["gl_k"], 256, "copy", ("KGt",)), (OFF["gl_v"], 512, "vbf", ("VG",))]
        for c0, ncols, kind, snames in tm:
            wb, wn = load_w(c0, ncols)
            for ti in range(NT):
                P, pn = next_p()
                for kc in range(8):
                    self.mm(P[:, 0:ncols], hT[:, kc, ti * 128:(ti + 1) * 128], wb[:, kc, 0:ncols], [wn, "hT"], [pn],
                            start=(kc == 0), stop=(kc == 7))
                rows = slice(ti * 128, (ti + 1) * 128)
                if kind == "copy":
                    o, on = next_o()
                    self.cp("act", o[:, 0:ncols], P[:, 0:ncols], [pn], [on])
                    self.dma("sp", self.scr[snames[0]][rows, :], o[:, 0:ncols], [on], [f"{snames[0]}@{ti}"])
                elif kind == "vbf":
                    o, on = next_o(BF16)
                    self.cp("act", o[:, 0:ncols], P[:, 0:ncols], [pn], [on])
                    self.dma("sp", self.scr[snames[0]][rows, :], o[:, 0:ncols], [on], [f"{snames[0]}@{ti}"])
                else:
                    o, on = next_o()
                    o2, on2 = next_o()
                    self.act(o[:], P[:], AF.Sigmoid, [pn], [on])
                    self.tt("dve", o[:], o[:], omlt[:], ALU.mult, [on, f"oml_tm{l}"], [on])
                    self.tt("dve", o[:], o[:], lbt[:], ALU.add, [on, f"lb_tm{l}"], [on])
                    self.act(o[:], o[:], AF.Ln, [on], [on])
                    self.dma("sp", self.scr[snames[0]][rows, :], o[:], [on], [f"{snames[0]}@{ti}"])
                    self.act(o2[:], P[:], AF.Sigmoid, [pn], [on2], scale=-1.0)
                    self.tt("dve", o2[:], o2[:], omlt[:], ALU.mult, [on2, f"oml_tm{l}"], [on2])
                    self.dma("sp", self.scr[snames[1]][rows, :], o2[:], [on2], [f"{snames[1]}@{ti}"])
        upf = psb("pg_upf", [33, 512])
        upb = psb("pg_upb", [33, 512], BF16)
        self.memset("pool", upf[:], 0.0, ["pg_upf"])
        self.dma("sp", upf[0:16, 0:256], self.w["gla_gate_up"][l, 0], (), ["pg_upf"])
        self.dma("sp", upf[16:32, 256:512], self.w["gla_gate_up"][l, 1], (), ["pg_upf"])
        self.dma("sp", upf[32:33, :], self.w["gla_gate_b"][l:l + 1].rearrange("o d k -> o (d k)"), (), ["pg_upf"])
        self.cp("dve", upb[:], upf[:], ["pg_upf"], ["pg_upb"])
        wb, wn = load_w(OFF["gl_df"], 32)
        for (s0, sl) in tbs:
            P, pn = next_p()
            for kc in range(8):
                self.mm(P[0:32, 0:sl], wb[:, kc, 0:32], hT[:, kc, s0:s0 + sl], [wn, "hT"], [pn], start=(kc == 0), stop=(kc == 7))
            i = (s0 // 512) % 2
            pdt = psb(f"pg_pd{i}", [33, 512], BF16)
            if s0 < 1024:
                self.memset("pool", pdt[32:33, :], 1.0, [f"pg_pd{i}"])
            self.cp("act", pdt[0:32, 0:sl], P[0:32, 0:sl], [pn], [f"pg_pd{i}"])
            for k in range(sl // 128):
                ti = s0 // 128 + k
                P2, pn2 = next_p()
                self.mm(P2[:], pdt[0:33, k * 128:(k + 1) * 128], upb[0:33, :], [f"pg_pd{i}", "pg_upb"], [pn2])
                o, on = next_o()
                self.act(o[:], P2[:], AF.Sigmoid, [pn2], [on])
                self.act(o[:], o[:], AF.Ln, [on], [on])
                self.ts("dve", o[:], o[:], 1.0 / 16.0, ALU.mult, [on], [on])
                self.dma("sp", self.scr["GG"][ti * 128:(ti + 1) * 128, :], o[:], [on], [f"GG@{ti}"])

    def s5_setup(self, stack, l, d):
        S, t = self.S, self.t
        psb = lambda name, shape, dt=F32: (self.sb(stack, name, shape, dt) if name not in t else t[name])
        W = psb("s5_W", [128, 16, 2, 128])
        V = psb("s5_V", [128, 16, 2, 128])
        BT = psb("s5_BT", [128, 16, 256], BF16)
        CT = psb("s5_CT", [128, 16, 2, 128], BF16)
        with self.phase() as tmp:
            tsb = lambda name, shape, dt=F32: self.sb(tmp, name, shape, dt)
            lre = tsb("s5t_lre", [128, 16])
            lim = tsb("s5t_lim", [128, 16])
            lst = tsb("s5t_lst", [128, 16])
            self.dma("sp", lre[:], self.w["s5_a_re"][l, d].rearrange("g n -> (g n)").rearrange("(j p) -> p j", p=128), (), ["s5t_lre"], allow_slow_non_contiguous=True)
            self.dma("sp", lim[:], self.w["s5_a_im"][l, d].rearrange("g n -> (g n)").rearrange("(j p) -> p j", p=128), (), ["s5t_lim"], allow_slow_non_contiguous=True)
            ls = self.w["s5_log_step"]
            base = ls[l, d, 0:1]
            for g2 in (0, 1):
                src = bass.AP(ls.tensor, base.offset + g2, [[0, 64], [2, 16]])
                self.dma("sp", lst[g2 * 64:(g2 + 1) * 64, :], src, (), ["s5t_lst"], allow_slow_non_contiguous=True)
            self.act(lst[:], lst[:], AF.Exp, ["s5t_lst"], ["s5t_lst"])
            lr = tsb("s5t_lr", [128, 16])
            li = tsb("s5t_li", [128, 16])
            self.tt("dve", lr[:], lre[:], lst[:], ALU.mult, ["s5t_lre", "s5t_lst"], ["s5t_lr"])
            self.tt("dve", li[:], lim[:], lst[:], ALU.mult, ["s5t_lim", "s5t_lst"], ["s5t_li"])
            ti_ = tsb("s5t_ti", [128, 128], I32)
            tau = tsb("s5t_tau", [128, 128])
            if d == 0:
                S.op("pool", lambda e: e.iota(ti_[:], pattern=[[1, 128]], base=1, channel_multiplier=0), (), ["s5t_ti"])
            else:
                S.op("pool", lambda e: e.iota(ti_[:], pattern=[[-1, 128]], base=128, channel_multiplier=0), (), ["s5t_ti"])
            self.cp("dve", tau[:], ti_[:], ["s5t_ti"], ["s5t_tau"])
            mag = tsb("s5t_mag", [128, 16, 128])
            ang = tsb("s5t_ang", [128, 16, 128])
            a2 = tsb("s5t_a2", [128, 16, 128])
            bi = tsb("s5t_bi", [128, 16, 128], I32)
            sn = tsb("s5t_sin", [128, 16, 128])
            cs = tsb("s5t_cos", [128, 16, 128])
            for j in range(16):
                self.act(mag[:, j, :], tau[:], AF.Exp, ["s5t_tau", "s5t_lr"], ["s5t_mag"], scale=lr[:, j:j + 1])
                self.ts("dve", ang[:, j, :], tau[:], li[:, j:j + 1], ALU.mult, ["s5t_tau", "s5t_li"], ["s5t_ang"])
            for dst, phase in ((sn, 0.0), (cs, math.pi / 2)):
                dn = "s5t_sin" if phase == 0.0 else "s5t_cos"
                self.ts("dve", a2[:], ang[:], phase, ALU.add, ["s5t_ang"], ["s5t_a2"])
                self.ts("dve", dst[:], a2[:], 1.0 / TWO_PI, ALU.mult, ["s5t_a2"], [dn])
                self.cp("dve", bi[:], dst[:], [dn], ["s5t_bi"])
                self.cp("dve", dst[:], bi[:], ["s5t_bi"], [dn])
                self.stt(a2[:], dst[:], -TWO_PI, a2[:], ALU.mult, ALU.add, [dn, "s5t_a2"], ["s5t_a2"])
                self.ts("dve", a2[:], a2[:], PI_SAFE, ALU.min, ["s5t_a2"], ["s5t_a2"], s2=-PI_SAFE, op1=ALU.max)
                self.act(dst[:], a2[:], AF.Sin, ["s5t_a2"], [dn])
            self.tt("dve", V[:, :, 0, :], mag[:], cs[:], ALU.mult, ["s5t_mag", "s5t_cos"], ["s5_V"])
            self.tt("dve", V[:, :, 1, :], mag[:], sn[:], ALU.mult, ["s5t_mag", "s5t_sin"], ["s5_V"])
            S.op("dve", lambda e: e.reciprocal(out=mag[:], in_=mag[:]), ["s5t_mag"], ["s5t_mag"])
            wfm = tsb("s5t_wfm", [128, 16, 2, 128])
            self.tt("dve", wfm[:, :, 0, :], mag[:], cs[:], ALU.mult, ["s5t_mag", "s5t_cos"], ["s5t_wfm"])
            self.stt(wfm[:, :, 1, :], mag[:], -1.0, sn[:], ALU.mult, ALU.mult, ["s5t_mag", "s5t_sin"], ["s5t_wfm"])
            k = 0
            for j in range(16):
                for c in range(2):
                    pn = f"P{k % 4}"
                    k += 1
                    self.tr(t[pn][:, 0:128], wfm[:, j, c, :], ["s5t_wfm"], [pn])
                    self.cp("act" if k % 2 else "dve", W[:, j, c, :], t[pn][:, 0:128], [pn], ["s5_W"])
            c1 = 0 if d == 0 else 127
            are = V[:, :, 0, c1]
            aim = V[:, :, 1, c1]
            den = tsb("s5t_den", [128, 16])
            tmp1 = tsb("s5t_t1", [128, 16])
            tmp2 = tsb("s5t_t2", [128, 16])
            ar1 = tsb("s5t_ar1", [128, 16])
            zre = tsb("s5t_zre", [128, 16])
            zim = tsb("s5t_zim", [128, 16])
            self.tt("dve", den[:], lre[:], lre[:], ALU.mult, ["s5t_lre"], ["s5t_den"])
            self.tt("dve", tmp1[:], lim[:], lim[:], ALU.mult, ["s5t_lim"], ["s5t_t1"])
            self.tt("dve", den[:], den[:], tmp1[:], ALU.add, ["s5t_den", "s5t_t1"], ["s5t_den"])
            S.op("dve", lambda e: e.reciprocal(out=den[:], in_=den[:]), ["s5t_den"], ["s5t_den"])
            self.ts("dve", ar1[:], are, -1.0, ALU.add, ["s5_V"], ["s5t_ar1"])
            self.tt("dve", tmp1[:], ar1[:], lre[:], ALU.mult, ["s5t_ar1", "s5t_lre"], ["s5t_t1"])
            self.tt("dve", tmp2[:], aim, lim[:], ALU.mult, ["s5_V", "s5t_lim"], ["s5t_t2"])
            self.tt("dve", tmp1[:], tmp1[:], tmp2[:], ALU.add, ["s5t_t1", "s5t_t2"], ["s5t_t1"])
            self.tt("dve", zre[:], tmp1[:], den[:], ALU.mult, ["s5t_t1", "s5t_den"], ["s5t_zre"])
            self.tt("dve", tmp1[:], aim, lre[:], ALU.mult, ["s5_V", "s5t_lre"], ["s5t_t1"])
            self.tt("dve", tmp2[:], ar1[:], lim[:], ALU.mult, ["s5t_ar1", "s5t_lim"], ["s5t_t2"])
            self.tt("dve", tmp1[:], tmp1[:], tmp2[:], ALU.subtract, ["s5t_t1", "s5t_t2"], ["s5t_t1"])
            self.tt("dve", zim[:], tmp1[:], den[:], ALU.mult, ["s5t_t1", "s5t_den"], ["s5t_zim"])
            bre = tsb("s5t_bre", [128, 16, 16])
            bim = tsb("s5t_bim", [128, 16, 16])
            self.dma("sp", bre[:], self.w["s5_b_re"][l, d].rearrange("g n q -> (g n) q").rearrange("(j p) q -> p j q", p=128), (), ["s5t_bre"])
            self.dma("sp", bim[:], self.w["s5_b_im"][l, d].rearrange("g n q -> (g n) q").rearrange("(j p) q -> p j q", p=128), (), ["s5t_bim"])
            bbr = tsb("s5t_bbr", [128, 16, 16])
            bbi = tsb("s5t_bbi", [128, 16, 16])
            u1 = tsb("s5t_u1", [128, 16, 16])
            zr_b = zre[:].unsqueeze(2).to_broadcast([128, 16, 16])
            zi_b = zim[:].unsqueeze(2).to_broadcast([128, 16, 16])
            self.tt("dve", bbr[:], bre[:], zr_b, ALU.mult, ["s5t_bre", "s5t_zre"], ["s5t_bbr"])
            self.tt("dve", u1[:], bim[:], zi_b, ALU.mult, ["s5t_bim", "s5t_zim"], ["s5t_u1"])
            self.tt("dve", bbr[:], bbr[:], u1[:], ALU.subtract, ["s5t_bbr", "s5t_u1"], ["s5t_bbr"])
            self.tt("dve", bbi[:], bim[:], zr_b, ALU.mult, ["s5t_bim", "s5t_zre"], ["s5t_bbi"])
            self.tt("dve", u1[:], bre[:], zi_b, ALU.mult, ["s5t_bre", "s5t_zim"], ["s5t_u1"])
            self.tt("dve", bbi[:], bbi[:], u1[:], ALU.add, ["s5t_bbi", "s5t_u1"], ["s5t_bbi"])
            bm = tsb("s5t_bm", [128, 16, 2, 128])
            for j in range(16):
                mk = t[f"mask{j % 4}"]
                for c, src, sname in ((0, bbr, "s5t_bbr"), (1, bbi, "s5t_bbi")):
                    self.tt("dve", bm[:, j, c, :].rearrange("p (r q) -> p r q", r=8),
                            src[:, j, :].unsqueeze(1).to_broadcast([128, 8, 16]),
                            mk[:].rearrange("p (r q) -> p r q", r=8), ALU.mult, [sname, f"mask{j % 4}"], ["s5t_bm"])
            for j in range(16):
                for c in range(2):
                    pn = f"P{k % 4}"
                    k += 1
                    self.tr(t[pn][:, 0:128], bm[:, j, c, :], ["s5t_bm"], [pn])
                    self.cp("act" if k % 2 else "dve", BT[:, j, c * 128:(c + 1) * 128], t[pn][:, 0:128], [pn], ["s5_BT"])
            for c, cname in ((0, "s5_c_re"), (1, "s5_c_im")):
                ctm = tsb(f"s5t_ctm{c}", [128, 4, 2, 64])
                src = self.w[cname][l, d].rearrange("g p n -> (g p) n").rearrange("(b rp) n -> rp b n", rp=128)
                for dup in range(2):
                    self.dma("sp", ctm[:, :, dup, :], src, (), [f"s5t_ctm{c}"])
                for b in range(4):
                    pn = f"P{k % 4}"
                    k += 1
                    self.tr(t[pn][:, 0:128], ctm[:, b, :, :].rearrange("p a n -> p (a n)"), [f"s5t_ctm{c}"], [pn])
                    for kk in range(4):
                        j = 4 * b + kk
                        self.stt(CT[:, j, c, :], t[pn][:, 0:128], 1.0 if c == 0 else -1.0, t[f"mask{kk}"][:], ALU.mult, ALU.mult,
                                 [pn, f"mask{kk}"], ["s5_CT"])


FAMS = {
    "hg": dict(G=4, W=512, g=("GHF", "GHB"), kt=("KHFt", "KHBt"), v="VH", q="QH", kf=("KHF", "KHB"),
               heads=[(h, 0, 128) for h in range(4)], orow=0),
    "gl": dict(G=2, W=256, g=("GG", "GG"), kt=("KGt", "KGt"), v="VG", q="QG", kf=("KG", "KG"),
               heads=[(h // 2, (h % 2) * 64, 64) for h in range(4)], orow=512),
}


class KB3(KB2):
    def rec_pass(self, l, d):
        S, t = self.S, self.t
        T, NT, nct = self.T, self.NT, self.nct
        for nm, rows in ((f"O{d}", 1024), (f"Y5{d}", 512)):
            if nm not in self.scr:
                self.dscr(nm, [rows, T])
        order = list(range(NT)) if d == 0 else (list(range(nct - 1, -1, -1)) + list(range(NT - 1, nct - 1, -1)))
        last = 127 if d == 0 else 0
        with self.phase() as ph:
            psb = lambda name, shape, dt=F32: (self.sb(ph, name, shape, dt) if name not in t else t[name])
            self.s5_setup(ph, l, d)
            minc, mstr, mincb = t[f"minc{d}"], t[f"mstr{d}"], t[f"mincb{d}"]
            mincn, mstrn, mincbn = f"minc{d}", f"mstr{d}", f"mincb{d}"
            W5, V5, BT5, CT5 = t["s5_W"], t["s5_V"], t["s5_BT"], t["s5_CT"]
            for f in ("hg", "gl"):
                psb(f"{f}_Sf", [128, 4, 128])
                psb(f"{f}_Sb", [128, 4, 128], BF16)
                self.memset("pool", t[f"{f}_Sf"][:], 0.0, [f"{f}_Sf"])
                self.memset("pool", t[f"{f}_Sb"][:], 0.0, [f"{f}_Sb"])
            carry = psb("s5_carry", [128, 16, 2])
            self.memset("pool", carry[:], 0.0, ["s5_carry0", "s5_carry1"])
            dbuf = psb("r_D", [128, 2, 8, 128])

            def loads(ti, par):
                rows = slice(ti * 128, (ti + 1) * 128)
                cols = slice(ti * 128, (ti + 1) * 128)
                for f, F_ in FAMS.items():
                    G, W = F_["G"], F_["W"]
                    gt = psb(f"{f}_g{par}", [128, W])
                    gsrc = self.scr[F_["g"][d]]
                    gs = gsrc[rows, :] if f == "hg" else gsrc[rows, d * 256:(d + 1) * 256]
                    self.dma("sp", gt[:], gs, [f"{F_['g'][d]}@{ti}"], [f"{f}_g{par}"])
                    kt = psb(f"{f}_kt{par}", [128, W])
                    self.dma("sp", kt[:], self.scr[F_["kt"][d]][rows, :], [f"{F_['kt'][d]}@{ti}"], [f"{f}_kt{par}"])
                    vt = psb(f"{f}_v{par}", [128, 512], BF16)
                    self.dma("sp", vt[:], self.scr[F_["v"]][rows, :], [f"{F_['v']}@{ti}"], [f"{f}_v{par}"])
                    qf = psb(f"{f}_q{par}", [128, G, 128])
                    self.dma("sp", qf[:], self.scr[F_["q"]].rearrange("(g p) t -> p g t", p=128)[:, :, cols], [f"{F_['q']}@{ti}"], [f"{f}_q{par}"])
                    kf = psb(f"{f}_kf{par}", [128, G, 128])
                    self.dma("sp", kf[:], self.scr[F_["kf"][d]].rearrange("(g p) t -> p g t", p=128)[:, :, cols], [f"{F_['kf'][d]}@{ti}"], [f"{f}_kf{par}"])
                u = psb(f"s5_u{par}", [128, 4, 128])
                self.dma("sp", u[:], self.scr["U5"].rearrange("(g p) t -> p g t", p=128)[:, :, cols], [f"U5@{ti}"], [f"s5_u{par}"])

            def fam_tile(f, ti, par):
                F_ = FAMS[f]
                G, W, heads = F_["G"], F_["W"], F_["heads"]
                gt, kt, vt, qf, kf = (t[f"{f}_{x}{par}"] for x in ("g", "kt", "v", "q", "kf"))
                gn, ktn, vn, qn, kfn = (f"{f}_{x}{par}" for x in ("g", "kt", "v", "q", "kf"))
                Sf, Sb = t[f"{f}_Sf"], t[f"{f}_Sb"]
                PE_, PB, PS, PO = (t[f"P{i}"] for i in range(4))
                PU = t["P0"]
                ex = psb(f"{f}_ex", [128, W])
                if f == "hg":
                    khat = psb(f"{f}_khat", [128, 4, 128], BF16)
                else:
                    fresh = "gl_khat" not in t
                    khat = psb("gl_khat", [128, 4, 128], BF16)
                    if fresh:
                        self.memset("pool", khat[:], 0.0, ["gl_khat"])
                bt = psb(f"{f}_bt", [128, G, 128])
                eb = psb(f"{f}_eb", [128, G, 128])
                qbar = psb(f"{f}_qbar", [128, G, 128], BF16)
                bprev = psb(f"{f}_bprev", [128, G, 8])
                bq = psb(f"{f}_bq", [128, G, 128])
                qt = psb(f"{f}_qt", [128, G, 128], BF16)
                pt = psb(f"{f}_pt", [128, 4, 128], BF16)
                osb = psb(f"{f}_osb", [128, 4, 128])
                ktb = psb(f"{f}_Kt", [128, 4, 8, 128], BF16)
                self.mm(PE_[:, 0:W], mstr[:], gt[:], [mstrn, gn], ["P0"])
                self.act(ex[:], PE_[:, 0:W], AF.Exp, ["P0"], [f"{f}_ex"])
                if f == "hg":
                    self.tt("dve", khat[:].rearrange("p h c -> p (h c)"), ex[:], kt[:], ALU.mult, [f"{f}_ex", ktn], [f"{f}_khat"])
                else:
                    kb_ = khat[:]
                    kout = bass.AP(kb_.tensor, kb_.offset, [list(kb_.ap[0]), [256, 2], [192, 2], [1, 64]])
                    self.tt("dve", kout, ex[:].rearrange("p (g a c) -> p g a c", g=2, a=2), kt[:].rearrange("p (g a c) -> p g a c", g=2, a=2),
                            ALU.mult, [f"{f}_ex", ktn], [f"{f}_khat"])
                yield
                for g in range(G):
                    self.mm(PB[:, g * 128:(g + 1) * 128], gt[:, g * 128:(g + 1) * 128], minc[:], [gn, mincn], ["P1"])
                pb3 = PB[:, 0:G * 128].rearrange("p (g c) -> p g c", g=G)
                self.cp("act", bt[:], pb3, ["P1"], [f"{f}_bt"])
                self.act(eb[:], pb3, AF.Exp, ["P1"], [f"{f}_eb"])
                self.tt("dve", qbar[:], qf[:], eb[:], ALU.mult, [qn, f"{f}_eb"], [f"{f}_qbar"])
                yield
                if d == 0:
                    self.memset("dve", bprev[:, :, 0:1], 0.0, [f"{f}_bprev"])
                    self.cp("dve", bprev[:, :, 1:8], bt[:, :, 15:127:16], [f"{f}_bt"], [f"{f}_bprev"])
                else:
                    self.memset("dve", bprev[:, :, 7:8], 0.0, [f"{f}_bprev"])
                    self.cp("dve", bprev[:, :, 0:7], bt[:, :, 16:128:16], [f"{f}_bt"], [f"{f}_bprev"])
                self.tt("dve", bq[:].rearrange("p g (i j) -> p g i j", i=8), bt[:].rearrange("p g (i j) -> p g i j", i=8),
                        bprev[:].unsqueeze(3).to_broadcast([128, G, 8, 16]), ALU.subtract, [f"{f}_bt", f"{f}_bprev"], [f"{f}_bq"])
                self.act(bq[:], bq[:], AF.Exp, [f"{f}_bq"], [f"{f}_bq"])
                self.tt("dve", qt[:], bq[:], qf[:], ALU.mult, [f"{f}_bq", qn], [f"{f}_qt"])
                yield
                if f == "gl":
                    kfm = psb("gl_kfm", [128, 4, 128])
                    for h, (g, p0, ps_) in enumerate(heads):
                        self.ts("dve", kfm[:, h, :], kf[:, g, :], t[f"hm{h % 2}"][:, 0:1], ALU.mult, [kfn, f"hm{h % 2}"], ["gl_kfm"])
                for g0 in range(0, G, 2):
                    dv = dbuf[:, 0:2, :, :]
                    self.tt("dve", dv, bprev[:, g0:g0 + 2, :].unsqueeze(3).to_broadcast([128, 2, 8, 128]),
                            bt[:, g0:g0 + 2, :].unsqueeze(2).to_broadcast([128, 2, 8, 128]), ALU.subtract, [f"{f}_bprev", f"{f}_bt"], ["r_D"])
                    self.ts("dve", dv, dv, 60.0, ALU.min, ["r_D"], ["r_D"])
                    self.act(dv, dv, AF.Exp, ["r_D"], ["r_D"])
                    if f == "hg":
                        self.tt("dve", ktb[:, g0:g0 + 2, :, :], dv, kf[:, g0:g0 + 2, :].unsqueeze(2).to_broadcast([128, 2, 8, 128]), ALU.mult, ["r_D", kfn], [f"{f}_Kt"])
                    else:
                        for h, (g, p0, ps_) in enumerate(heads):
                            self.tt("dve", ktb[:, h, :, :], dbuf[:, g, :, :], kfm[:, h, :].unsqueeze(1).to_broadcast([128, 8, 128]), ALU.mult,
                                    ["r_D", "gl_kfm"], [f"{f}_Kt"])
                yield
                for h, (g, p0, ps_) in enumerate(heads):
                    for i in range(8):
                        self.mm(PS[:, h * 128 + 16 * i:h * 128 + 16 * i + 16], ktb[:, h, i, :],
                                qt[:, g, 16 * i:16 * i + 16], [f"{f}_Kt", f"{f}_qt"], ["P2"])
                self.tt("dve", pt[:], PS[:].rearrange("p (h c) -> p h c", h=4), minc[:].unsqueeze(1).to_broadcast([128, 4, 128]),
                        ALU.mult, ["P2", mincn], [f"{f}_pt"])
                yield
                for h, (g, p0, ps_) in enumerate(heads):
                    self.mm(PO[:, h * 128:(h + 1) * 128], vt[:, h * 128:(h + 1) * 128], pt[:, h, :], [vn, f"{f}_pt"], ["P3"], start=True, stop=False)
                    self.mm(PO[:, h * 128:(h + 1) * 128], Sb[:, h, :], qbar[:, g, :], [f"{f}_Sb", f"{f}_qbar"], ["P3"], start=False, stop=True)
                self.cp("act", osb[:], PO[:].rearrange("p (h c) -> p h c", h=4), ["P3"], [f"{f}_osb"])
                orow = F_["orow"]
                self.dma("act", self.scr[f"O{d}"][orow:orow + 512, :].rearrange("(h p) t -> p h t", p=128)[:, :, ti * 128:(ti + 1) * 128],
                         osb[:], [f"{f}_osb"], [f"O{d}{f}@{ti}"])
                yield
                for h, (g, p0, ps_) in enumerate(heads):
                    self.mm(PU[:, h * 128:(h + 1) * 128], khat[:, h, :], vt[:, h * 128:(h + 1) * 128], [f"{f}_khat", vn], ["P0"])
                for h, (g, p0, ps_) in enumerate(heads):
                    self.stt(Sf[:, h, :], Sf[:, h, :], eb[:, g, last:last + 1], PU[:, h * 128:(h + 1) * 128], ALU.mult, ALU.add,
                             [f"{f}_Sf", f"{f}_eb", "P0"], [f"{f}_Sf"])
                self.cp("act", Sb[:], Sf[:], [f"{f}_Sf"], [f"{f}_Sb"])
                yield

            def s5_prep(ti, par):
                u = t[f"s5_u{par}"]
                ub = psb("s5_ub", [128, 4, 128], BF16)
                self.cp("act", ub[:], u[:], [f"s5_u{par}"], ["s5_ub"])
                psb("s5_sbf", [128, 16, 2, 128], BF16)

            def s5_half(ti, par, hp):
                ub = t["s5_ub"]
                sbf = t["s5_sbf"]
                PBU, pbun = (t["P5"], "P5") if hp == 0 else (t["P7"], "P7")
                PZ, pzn = (t["P6"], "P6") if hp == 0 else (t["P4"], "P4")
                sbfn, carn = f"s5_sbf{hp}", f"s5_carry{hp}"
                for r in range(hp, 8, 2):
                    for jj in range(2):
                        j = 2 * r + jj
                        self.mm(PBU[:, jj * 256:(jj + 1) * 256], ub[:, j // 4, :], BT5[:, j, :], ["s5_ub", "s5_BT"], [pbun])
                    yield
                    pbu = PBU[:].rearrange("p (a c n) -> p a c n", a=2, c=2)
                    Wv = W5[:, 2 * r:2 * r + 2, :, :]
                    t1 = psb(f"s5_t1_{hp}", [128, 2, 128])
                    t2 = psb(f"s5_t2_{hp}", [128, 2, 128])
                    x = psb(f"s5_x{hp}", [128, 2, 2, 128], BF16)
                    xn, t1n, t2n = f"s5_x{hp}", f"s5_t1_{hp}", f"s5_t2_{hp}"
                    self.tt("dve", t1[:], pbu[:, :, 0, :], Wv[:, :, 0, :], ALU.mult, [pbun, "s5_W"], [t1n])
                    self.tt("dve", t2[:], pbu[:, :, 1, :], Wv[:, :, 1, :], ALU.mult, [pbun, "s5_W"], [t2n])
                    self.tt("dve", x[:, :, 0, :], t1[:], t2[:], ALU.subtract, [t1n, t2n], [xn])
                    self.tt("dve", t1[:], pbu[:, :, 0, :], Wv[:, :, 1, :], ALU.mult, [pbun, "s5_W"], [t1n])
                    self.tt("dve", t2[:], pbu[:, :, 1, :], Wv[:, :, 0, :], ALU.mult, [pbun, "s5_W"], [t2n])
                    self.tt("dve", x[:, :, 1, :], t1[:], t2[:], ALU.add, [t1n, t2n], [xn])
                    yield
                    zs = psb(f"s5_zs_{hp}", [128, 2, 2, 128])
                    zsn = f"s5_zs_{hp}"
                    for jj in range(2):
                        for c in range(2):
                            col = (jj * 2 + c) * 128
                            self.mm(PZ[:, col:col + 128], x[:, jj, c, :], mincb[:], [xn, mincbn], [pzn])
                    for jj in range(2):
                        for c in range(2):
                            col = (jj * 2 + c) * 128
                            self.act(zs[:, jj, c, :], PZ[:, col:col + 128], AF.Identity, [pzn, carn], [zsn],
                                     bias=carry[:, 2 * r + jj, c:c + 1])
                    yield
                    Vv = V5[:, 2 * r:2 * r + 2, :, :]
                    sre = psb(f"s5_sre_{hp}", [128, 2, 128])
                    sim = psb(f"s5_sim_{hp}", [128, 2, 128])
                    m1 = psb(f"s5_m1_{hp}", [128, 2, 128])
                    m2 = psb(f"s5_m2_{hp}", [128, 2, 128])
                    sren, simn, m1n, m2n = f"s5_sre_{hp}", f"s5_sim_{hp}", f"s5_m1_{hp}", f"s5_m2_{hp}"
                    self.tt("dve", m1[:], zs[:, :, 0, :], Vv[:, :, 0, :], ALU.mult, [zsn, "s5_V"], [m1n])
                    self.tt("dve", m2[:], zs[:, :, 1, :], Vv[:, :, 1, :], ALU.mult, [zsn, "s5_V"], [m2n])
                    self.tt("dve", sre[:], m1[:], m2[:], ALU.subtract, [m1n, m2n], [sren])
                    self.tt("dve", m1[:], zs[:, :, 0, :], Vv[:, :, 1, :], ALU.mult, [zsn, "s5_V"], [m1n])
                    self.tt("dve", m2[:], zs[:, :, 1, :], Vv[:, :, 0, :], ALU.mult, [zsn, "s5_V"], [m2n])
                    self.tt("dve", sim[:], m1[:], m2[:], ALU.add, [m1n, m2n], [simn])
                    yield
                    self.cp("act", sbf[:, 2 * r:2 * r + 2, 0, :], sre[:], [sren], [sbfn])
                    self.cp("act", sbf[:, 2 * r:2 * r + 2, 1, :], sim[:], [simn], [sbfn])
                    self.cp("dve", carry[:, 2 * r:2 * r + 2, 0], sre[:, :, last], [sren], [carn])
                    self.cp("dve", carry[:, 2 * r:2 * r + 2, 1], sim[:, :, last], [simn], [carn])
                    yield

            def s5_fin(ti, par):
                sbf = t["s5_sbf"]
                PY = t["P5"]
                for b in range(4):
                    for kk in range(4):
                        j = 4 * b + kk
                        self.mm(PY[:, b * 128:(b + 1) * 128], CT5[:, j, 0, :], sbf[:, j, 0, :], ["s5_CT", "s5_sbf0", "s5_sbf1"], ["P5"], start=(kk == 0), stop=False)
                        self.mm(PY[:, b * 128:(b + 1) * 128], CT5[:, j, 1, :], sbf[:, j, 1, :], ["s5_CT", "s5_sbf0", "s5_sbf1"], ["P5"], start=False, stop=(kk == 3))
                ysb = psb("s5_ysb", [128, 4, 128])
                self.cp("act", ysb[:], PY[:].rearrange("p (b c) -> p b c", b=4), ["P5"], ["s5_ysb"])
                self.dma("act", self.scr[f"Y5{d}"].rearrange("(b p) t -> p b t", p=128)[:, :, ti * 128:(ti + 1) * 128], ysb[:],
                         ["s5_ysb"], [f"Y5{d}@{ti}"])

            import os as _os
            skip = ""
            if "all" in skip:
                return
            loads(order[0], 0)
            for n, ti in enumerate(order):
                par = n % 2
                if n + 1 < len(order):
                    loads(order[n + 1], 1 - par)
                gens = []
                if "hg" not in skip:
                    gens.append(fam_tile("hg", ti, par))
                if "gl" not in skip:
                    gens.append(fam_tile("gl", ti, par))
                if "s5" not in skip:
                    s5_prep(ti, par)
                    gens.append(s5_half(ti, par, 0))
                    gens.append(s5_half(ti, par, 1))
                while gens:
                    for g_ in list(gens):
                        try:
                            next(g_)
                        except StopIteration:
                            gens.remove(g_)
                if "s5" not in skip:
                    s5_fin(ti, par)


class KB4(KB3):
    def load_cast(self, stack, dst, dst_name, src_ap, nk, ncols, kstep):
        t = self.t
        for k0 in range(0, nk, kstep):
            kn = min(kstep, nk - k0)
            sn = f"lc_st{kstep}_{self._lc % 2}"
            self._lc += 1
            stg = self.sb(stack, sn, [128, kstep, 1024]) if sn not in t else t[sn]
            self.dma("sp", stg[:, 0:kn, 0:ncols], src_ap[:, k0:k0 + kn, :], (), [sn])
            self.cp("pool", dst[:, k0:k0 + kn, 0:ncols], stg[:, 0:kn, 0:ncols], [sn], [dst_name])

    def merge_phase(self, l, last):
        S, t = self.S, self.t
        T, NT = self.T, self.NT
        self._lc = 0
        with self.phase() as ph:
            psb = lambda name, shape, dt=F32: (self.sb(ph, name, shape, dt) if name not in t else t[name])
            bp = psb("mg_bp", [128, 12, D], BF16)
            wo = psb("mg_wo", [128, 8, D], BF16)
            glu = psb("mg_glu", [128, 4, 512], BF16)
            with self.phase() as tmpw:
                self.load_cast(tmpw, bp, "mg_bp", self.w["branch_proj"][l].rearrange("b (kc p) n -> p (b kc) n", p=128), 12, D, 4)
                self.load_cast(tmpw, wo, "mg_wo", self.w["w_out"][l].rearrange("(kc p) n -> p kc n", p=128), 8, D, 4)
                self.load_cast(tmpw, glu, "mg_glu", self.w["s5_glu_w"][l].rearrange("(kc p) n -> p kc n", p=128), 4, 512, 4)
            gains = {}
            for nm, src in (("hgn", "hg_norm_g"), ("gln", "gla_norm_g"), ("s5d", "s5_d")):
                g = psb("mg_" + nm, [128, 4])
                self.dma("sp", g[:], self.w[src][l].rearrange("(h p) -> p h", p=128), (), ["mg_" + nm], allow_slow_non_contiguous=True)
                gains[nm] = g
            self.load_mod(ph, l, 2, "mg_g1")
            epsT = psb("mg_eps", [128, 1])
            self.memset("pool", epsT[:], EPS, ["mg_eps"])
            tiles = [ti for ti in range(NT) if not (last and self.is_ctx(ti))]

            def ld(name, par, src_ap, shape, deps, dt=F32):
                nm = f"mg_{name}{par}"
                h = psb(nm, shape, dt)
                self.dma("sp", h[:], src_ap, deps, [nm])
                return h

            def loads(ti, par):
                cols = slice(ti * 128, (ti + 1) * 128)
                fmv = lambda ap: ap.rearrange("(h p) t -> p h t", p=128)[:, :, cols]
                for d in (0, 1):
                    ld(f"ohg{d}", par, fmv(self.scr[f"O{d}"][0:512, :]), [128, 4, 128], [f"O{d}hg@{ti}"])
                    ld(f"ogl{d}", par, fmv(self.scr[f"O{d}"][512:1024, :]), [128, 4, 128], [f"O{d}gl@{ti}"])
                    ld(f"y5{d}", par, fmv(self.scr[f"Y5{d}"]), [128, 4, 128], [f"Y5{d}@{ti}"])
                ld("u5", par, fmv(self.scr["U5"]), [128, 4, 128], [f"U5@{ti}"])
                ld("gh", par, fmv(self.scr["GH"]), [128, 4, 128], [f"GH@{ti}"])
                ld("rg", par, fmv(self.scr["RG"]), [128, 4, 128], [f"RG@{ti}"])
                for nm in ("GA", "GB", "GC"):
                    ld(nm, par, fmv(self.scr[nm]), [128, 8, 128], [f"{nm}@{ti}"])
                ld("x", par, self.scr["X"][ti * 128:(ti + 1) * 128, :], [128, D], [f"X@{ti}"])

            def compute(ti, par):
                v = 1 if self.is_ctx(ti) else 0
                g = lambda name: (t[f"mg_{name}{par}"], f"mg_{name}{par}")
                ybr = {}
                for bi, (fam, gate, gain) in enumerate((("hg", "gh", "hgn"), ("gl", "rg", "gln"))):
                    o0, o0n = g(f"o{fam}0")
                    o1, o1n = g(f"o{fam}1")
                    gt, gtn = g(gate)
                    o = psb(f"mg_o{bi}", [128, 4, 128])
                    osq = psb(f"mg_osq{bi}", [128, 4, 128])
                    rs = psb(f"mg_rs{bi}", [128, 4, 128])
                    yb = psb(f"mg_y{bi}", [128, 4, 128], BF16)
                    on, sqn, rsn, ybn = f"mg_o{bi}", f"mg_osq{bi}", f"mg_rs{bi}", f"mg_y{bi}"
                    self.tt("dve", o[:], o0[:], o1[:], ALU.add, [o0n, o1n], [on])
                    self.act(osq[:], o[:], AF.Square, [on], [sqn])
                    self.mm(t["P0"][:], t["ones"][:], osq[:].rearrange("p h c -> p (h c)"), ["ones", sqn], ["P0"])
                    self.act(rs[:].rearrange("p h c -> p (h c)"), t["P0"][:], AF.Sqrt, ["P0", "mg_eps"], [rsn], bias=epsT[:], scale=1.0 / 128.0)
                    S.op("dve", lambda e, rs=rs: e.reciprocal(out=rs[:], in_=rs[:]), [rsn], [rsn])
                    self.tt("dve", o[:], o[:], rs[:], ALU.mult, [on, rsn], [on])
                    for h in range(4):
                        self.stt(yb[:, h, :], o[:, h, :], gains[gain][:, h:h + 1], gt[:, h, :], ALU.mult, ALU.mult,
                                 [on, "mg_" + gain, gtn], [ybn])
                    ybr[bi] = (yb, ybn)
                u5, u5n = g("u5")
                y50, y50n = g("y50")
                y51, y51n = g("y51")
                yy = psb("mg_yy", [128, 4, 128])
                tq = psb("mg_tq", [128, 4, 128])
                for k in range(4):
                    self.stt(yy[:, k, :], u5[:, k, :], gains["s5d"][:, k:k + 1], y50[:, k, :], ALU.mult, ALU.add, [u5n, "mg_s5d", y50n], ["mg_yy"])
                self.tt("dve", yy[:], yy[:], y51[:], ALU.add, ["mg_yy", y51n], ["mg_yy"])
                self.tt("dve", tq[:], yy[:], yy[:], ALU.mult, ["mg_yy"], ["mg_tq"])
                self.ts("dve", tq[:], tq[:], 0.044715, ALU.mult, ["mg_tq"], ["mg_tq"], s2=1.0, op1=ALU.add)
                self.tt("dve", tq[:], tq[:], yy[:], ALU.mult, ["mg_tq", "mg_yy"], ["mg_tq"])
                self.act(tq[:], tq[:], AF.Tanh, ["mg_tq"], ["mg_tq"], scale=math.sqrt(2.0 / math.pi))
                self.ts("dve", tq[:], tq[:], 1.0, ALU.add, ["mg_tq"], ["mg_tq"], s2=0.5, op1=ALU.mult)
                self.tt("dve", yy[:], yy[:], tq[:], ALU.mult, ["mg_yy", "mg_tq"], ["mg_yy"])
                ygb = psb("mg_ygb", [128, 4, 128], BF16)
                self.cp("act", ygb[:], yy[:], ["mg_yy"], ["mg_ygb"])
                for oc in range(4):
                    for kc in range(4):
                        self.mm(t["P1"][:, oc * 128:(oc + 1) * 128], glu[:, kc, oc * 128:(oc + 1) * 128], ygb[:, kc, :],
                                ["mg_glu", "mg_ygb"], ["P1"], start=(kc == 0), stop=(kc == 3))
                self.act(tq[:].rearrange("p h c -> p (h c)"), t["P1"][:], AF.Sigmoid, ["P1"], ["mg_tq"])
                yc = psb("mg_y2", [128, 4, 128], BF16)
                self.tt("dve", yc[:], yy[:], tq[:], ALU.mult, ["mg_yy", "mg_tq"], ["mg_y2"])
                ybr[2] = (yc, "mg_y2")
                mg = psb("mg_m", [128, 8, 128])
                mt = psb("mg_mt", [128, 8, 128])
                for bi, gname in enumerate(("GA", "GB", "GC")):
                    yb, ybn = ybr[bi]
                    gt, gtn = g(gname)
                    for oc in range(8):
                        pn = f"P{2 + oc // 4}"
                        for kc in range(4):
                            self.mm(t[pn][:, (oc % 4) * 128:(oc % 4 + 1) * 128], bp[:, bi * 4 + kc, oc * 128:(oc + 1) * 128], yb[:, kc, :],
                                    ["mg_bp", ybn], [pn], start=(kc == 0), stop=(kc == 3))
                    for half in range(2):
                        pn = f"P{2 + half}"
                        pv = t[pn][:].rearrange("p (o c) -> p o c", o=4)
                        dst = mg if bi == 0 else mt
                        dn = "mg_m" if bi == 0 else "mg_mt"
                        self.tt("dve", dst[:, half * 4:(half + 1) * 4, :], pv, gt[:, half * 4:(half + 1) * 4, :], ALU.mult, [pn, gtn], [dn])
                    if bi > 0:
                        self.tt("dve", mg[:], mg[:], mt[:], ALU.add, ["mg_m", "mg_mt"], ["mg_m"])
                mgb = psb("mg_mb", [128, 8, 128], BF16)
                self.cp("act", mgb[:], mg[:], ["mg_m"], ["mg_mb"])
                x, xn = g("x")
                xt_ = psb("mg_xt", [128, D])
                for half in range(2):
                    pn = f"P{4 + half}"
                    for kc in range(8):
                        self.mm(t[pn][:], mgb[:, kc, :], wo[:, kc, half * 512:(half + 1) * 512], ["mg_mb", "mg_wo"], [pn],
                                start=(kc == 0), stop=(kc == 7))
                    self.tt("dve", xt_[:, half * 512:(half + 1) * 512], t[pn][:], t[f"mg_g1{v}"][:, half * 512:(half + 1) * 512], ALU.mult,
                            [pn, f"mg_g1{v}"], ["mg_xt"])
                self.tt("dve", x[:], x[:], xt_[:], ALU.add, [xn, "mg_xt"], [xn])
                self.dma("act", self.scr["X"][ti * 128:(ti + 1) * 128, :], x[:], [xn], [f"X@{ti}"])

            loads(tiles[0], 0)
            for n, ti in enumerate(tiles):
                if n + 1 < len(tiles):
                    loads(tiles[n + 1], (n + 1) % 2)
                compute(ti, n % 2)

    def ffn_phase(self, l, moe):
        S, t = self.S, self.t
        T, NT = self.T, self.NT
        self._lc = 0
        GF = 4
        if moe:
            NL = self.NLOC
            tiles = list(range(NL))
            TT_ = NL * 128
            if "XL" not in self.scr:
                self.dscr("XL", [TT_, D])
            xs, xtag = self.scr["XL"], "XL"
        else:
            tiles = list(range(NT))
            TT_ = T
            xs, xtag = self.scr["X"], "X"
        tbs = [(s_, min(512, TT_ - s_)) for s_ in range(0, TT_, 512)]
        with self.phase() as ph:
            psb = lambda name, shape, dt=F32: (self.sb(ph, name, shape, dt) if name not in t else t[name])
            cmbt = psb("ff_cmbt", [128, self.NLOC, NE]) if moe else None
            hT = self.norm_phase(ph, l, True, tiles, router=moe, local=cmbt)
            self.load_mod(ph, l, 5, "ff_g2")
            ag = psb("ff_a", [128, GF, TT_], BF16)
            w2g = psb("ff_w2", [128, GF, D], BF16)
            experts = list(range(NE)) if moe else [None]
            cnt = 0
            for e in experts:
                if moe:
                    w1s, w3s, w2s = self.w["moe_w1"][0, e], self.w["moe_w3"][0, e], self.w["moe_w2"][0, e]
                else:
                    w1s, w3s, w2s = self.w["ffn_w1"][0], self.w["ffn_w3"][0], self.w["ffn_w2"][0]
                w1v = w1s.rearrange("(kc p) f -> p kc f", p=128)
                w3v = w3s.rearrange("(kc p) f -> p kc f", p=128)
                w2v = w2s.rearrange("(fc p) n -> p fc n", p=128)
                for grp in range(FF // 128 // GF):
                    for fi in range(GF):
                        fc = grp * GF + fi
                        i = cnt % 2
                        cnt += 1
                        wf = psb(f"ff_wf{i}", [128, 2, 8, 128])
                        wb = psb(f"ff_wb{i}", [128, 2, 8, 128], BF16)
                        self.dma("sp", wf[:, 0, :, :], w1v[:, :, fc * 128:(fc + 1) * 128], (), [f"ff_wf{i}"])
                        self.dma("sp", wf[:, 1, :, :], w3v[:, :, fc * 128:(fc + 1) * 128], (), [f"ff_wf{i}"])
                        self.cp("pool", wb[:], wf[:], [f"ff_wf{i}"], [f"ff_wb{i}"])
                        for bi_, (s0, sl) in enumerate(tbs):
                            pa, pb_ = f"P{(bi_ % 2) * 2}", f"P{(bi_ % 2) * 2 + 1}"
                            for kc in range(8):
                                self.mm(t[pa][:, 0:sl], wb[:, 0, kc, :], hT[:, kc, s0:s0 + sl], [f"ff_wb{i}", "hT"], [pa], start=(kc == 0), stop=(kc == 7))
                            for kc in range(8):
                                self.mm(t[pb_][:, 0:sl], wb[:, 1, kc, :], hT[:, kc, s0:s0 + sl], [f"ff_wb{i}", "hT"], [pb_], start=(kc == 0), stop=(kc == 7))
                            sg = psb(f"ff_s{bi_ % 2}", [128, 512])
                            sgn = f"ff_s{bi_ % 2}"
                            self.act(sg[:, 0:sl], t[pa][:, 0:sl], AF.Silu, [pa], [sgn])
                            self.tt("dve", ag[:, fi, s0:s0 + sl], sg[:, 0:sl], t[pb_][:, 0:sl], ALU.mult, [sgn, pb_], ["ff_a"])
                    self.load_cast(ph, w2g, "ff_w2", w2v[:, grp * GF:(grp + 1) * GF, :], GF, D, 1)
                    for n, ti in enumerate(tiles):
                        v = 1 if (not moe and self.is_ctx(ti)) else 0
                        xn = f"ff_x{n % 2}"
                        x = psb(xn, [128, D])
                        self.dma("sp", x[:], xs[ti * 128:(ti + 1) * 128, :], [f"{xtag}@{ti}"], [xn])
                        xt_ = psb("ff_xt", [128, D])
                        for half in range(2):
                            pn = f"P{4 + (n % 2) * 2 + half}"
                            for fi in range(GF):
                                self.mm(t[pn][:], ag[:, fi, ti * 128:(ti + 1) * 128], w2g[:, fi, half * 512:(half + 1) * 512], ["ff_a", "ff_w2"], [pn],
                                        start=(fi == 0), stop=(fi == GF - 1))
                            g2 = t[f"ff_g2{v}"][:, half * 512:(half + 1) * 512]
                            if moe:
                                self.stt(xt_[:, half * 512:(half + 1) * 512], t[pn][:], cmbt[:, ti, e:e + 1], g2, ALU.mult, ALU.mult,
                                         [pn, f"ff_g2{v}", "ff_cmbt"], ["ff_xt"])
                            else:
                                self.tt("dve", xt_[:, half * 512:(half + 1) * 512], t[pn][:], g2, ALU.mult, [pn, f"ff_g2{v}"], ["ff_xt"])
                        self.tt("dve", x[:], x[:], xt_[:], ALU.add, [xn, "ff_xt"], [xn])
                        self.dma("pool", xs[ti * 128:(ti + 1) * 128, :], x[:], [xn], [f"{xtag}@{ti}"])

    def final_phase(self):
        S, t = self.S, self.t
        with self.phase() as ph:
            psb = lambda name, shape, dt=F32: (self.sb(ph, name, shape, dt) if name not in t else t[name])
            g = psb("fn_g", [128, D])
            self.dma("sp", g[:], pbc(self.w["final_norm_g"].rearrange("(o d) -> o d", o=1)), (), ["fn_g"])
            if "XL" not in self.scr:
                self.dscr("XL", [self.NLOC * 128, D])
                idxt = psb("fn_idx", [128, self.NLOC], I32)
                self.dma("sp", idxt[:], self.idx_in, (), ["fn_idx"])
                for lt in range(self.NLOC):
                    g_ = psb(f"fn_g{lt % 2}", [128, D])
                    self.S.dma_gather(g_[:], self.scr["X"], idxt[:, lt:lt + 1], ["fn_idx"] + [f"X@{k}" for k in range(self.NT)], [f"fn_g{lt % 2}"])
                    self.dma("sp", self.scr["XL"][lt * 128:(lt + 1) * 128, :], g_[:], [f"fn_g{lt % 2}"], [f"XL@{lt}"])
            for n, ti in enumerate(range(self.NLOC)):
                xn, jn, sn = f"fn_x{n % 2}", f"fn_j{n % 2}", f"fn_s{n % 2}"
                x, junk, ss = psb(xn, [128, D]), psb(jn, [128, D]), psb(sn, [128, 1])
                self.dma("sp", x[:], self.scr["XL"][ti * 128:(ti + 1) * 128, :], [f"XL@{ti}"], [xn])
                self.act(junk[:], x[:], AF.Square, [xn], [jn, sn], accum=ss[:])
                self.ts("dve", ss[:], ss[:], 1.0 / D, ALU.mult, [sn], [sn], s2=EPS, op1=ALU.add)
                self.act(ss[:], ss[:], AF.Sqrt, [sn], [sn])
                S.op("dve", lambda e, ss=ss: e.reciprocal(out=ss[:], in_=ss[:]), [sn], [sn])
                self.stt(junk[:], x[:], ss[:], g[:], ALU.mult, ALU.mult, [xn, sn, "fn_g"], [jn])
                self.dma("pool", self.out[ti * 128:(ti + 1) * 128, :], junk[:], [jn], ["out"])
            self.S.finish(["out"])

    def mixer(self, l, sub=None):
        with self.phase() as ph:
            self.norm_phase(ph, l, False, list(range(self.NT)))
            if sub == "norm":
                return
            self.proj_phase(ph, l)
        if sub == "proj":
            return
        self.rec_pass(l, 0)
        if sub == "rec0":
            return
        self.rec_pass(l, 1)
        if sub == "rec1":
            return
        self.merge_phase(l, last=(l == 1))

    def build_all(self, upto="all"):
        stages = ["setup", "mod0", "mix0", "ffn0", "mod1", "mix1", "ffn1"]
        self.setup()
        if upto != "setup":
            self.mod_phase(0)
            if upto != "mod0":
                self.mixer(0, upto[4:] if upto.startswith("sub0") else None)
                if upto != "mix0" and not upto.startswith("sub0"):
                    self.ffn_phase(0, moe=False)
                    if upto != "ffn0":
                        self.mod_phase(1)
                        self.mixer(1)
                        if upto != "mix1":
                            self.ffn_phase(1, moe=True)
        self.final_phase()
        self.st.close()
        return self.nc


_NC_CACHE = {}


def _build_full():
    if "nc" not in _NC_CACHE:
        k = KB4(nct=2, nlt=32)
        _NC_CACHE["nc"] = k.build_all("all")
    return _NC_CACHE["nc"]


def kernel(**inputs):
    x = np.asarray(inputs["x"], dtype=np.float32)
    c = np.asarray(inputs["c"], dtype=np.float32)
    ctx = np.asarray(inputs["ctx"], dtype=np.float32)
    c_ctx = np.asarray(inputs["c_ctx"], dtype=np.float32)
    nb = x.shape[0]
    shared = {}
    for name, shape in WSPECS:
        shared[name] = np.ascontiguousarray(np.asarray(inputs[name], dtype=np.float32)).reshape(shape)
    cctxcol = np.ascontiguousarray(c_ctx.reshape(8, 128).T)
    in_maps = []
    nloc = 16
    for core in range(8):
        b, half = core % nb, core // nb
        m = dict(shared)
        m["xin"] = np.ascontiguousarray(np.concatenate([ctx[b], x[b]], axis=0))
        m["ccol"] = np.ascontiguousarray(c[b].reshape(8, 128).T)
        m["cctxcol"] = cctxcol
        m["idx"] = (256 + half * nloc * 128 + np.arange(nloc)[None, :] * 128 + np.arange(128)[:, None]).astype(np.int32)
        in_maps.append(m)
    nc = _build_full()
    res = run_bass_kernel_spmd(nc, in_maps, core_ids=list(range(8)))
    out = np.stack([np.concatenate([np.asarray(res.results[b + nb * h]["out"], dtype=np.float32) for h in range(2)], axis=0)
                    for b in range(nb)], axis=0)
    return out
```
